# Optimizing a Trainium2 kernel written in Bass

```python
import math
import jax, jax.numpy as jnp
from jax import lax
import numpy as np

D_MODEL = 4096
BATCH = 1
SEQ = 8192
DEPTH = 4

N_MIXERS = 3
BLOCK_Q = 128
LN_EPS = 1e-5
RMS_EPS = 1e-6

N_SB_LAYERS = (DEPTH + N_MIXERS - 1) // N_MIXERS
N_MLA_LAYERS = (DEPTH + N_MIXERS - 2) // N_MIXERS
N_DIL_LAYERS = DEPTH // N_MIXERS

SB_HEADS = 32
SB_HEAD_DIM = 128

MLA_HEADS = 32
MLA_Q_RANK = 1024
MLA_KV_RANK = 512
MLA_NOPE_DIM = 128
MLA_ROPE_DIM = 64
MLA_V_DIM = 128
ROPE_THETA = 10000.0

DIL_WINDOWS = (128, 512, 2048)
DIL_DILATIONS = (1, 4, 16)
DIL_GROUPS = 3
DIL_HEADS = 16
DIL_HEAD_DIM = 128
DIL_KEYS = DIL_WINDOWS[0] // DIL_DILATIONS[0] + 1

REL_BUCKETS = 32
REL_MAX_DIST = 2048

MOE_GROUPS = 4
MOE_EXPERTS_PER_GROUP = 8
MOE_TOP_K = 2
EXPERT_HIDDEN = 256

DN_ALPHA = (2 * DEPTH) ** 0.25
DN_BETA = (8 * DEPTH) ** -0.25

kernel_name = "hybrid_sb_mla_dilated_hmoe_deepnorm"


def layer_norm(x, g, b):
    xf = x.astype(jnp.float32)
    mu = jnp.mean(xf, axis=-1, keepdims=True)
    var = jnp.mean(jnp.square(xf - mu), axis=-1, keepdims=True)
    return ((xf - mu) * lax.rsqrt(var + LN_EPS) * g + b).astype(x.dtype)


def rms_norm(x, g):
    xf = x.astype(jnp.float32)
    return (xf * lax.rsqrt(jnp.mean(xf * xf, axis=-1, keepdims=True) + RMS_EPS) * g).astype(x.dtype)


def map_query_blocks(fn, n_blocks):
    out = lax.map(fn, jnp.arange(n_blocks))
    out = jnp.moveaxis(out, 0, 1)
    return out.reshape(out.shape[0], -1, *out.shape[3:])


def apply_rope(x, pos):
    half = x.shape[-1] // 2
    inv_freq = ROPE_THETA ** (-jnp.arange(half, dtype=jnp.float32) / half)
    ang = (pos.astype(jnp.float32)[..., None] * inv_freq)[:, :, None, :]
    cos, sin = jnp.cos(ang), jnp.sin(ang)
    x1 = x[..., :half].astype(jnp.float32)
    x2 = x[..., half:].astype(jnp.float32)
    return jnp.concatenate([x1 * cos - x2 * sin, x2 * cos + x1 * sin], axis=-1).astype(x.dtype)


def t5_bucket(rel):
    n = jnp.maximum(rel, 0)
    max_exact = REL_BUCKETS // 2
    nf = jnp.maximum(n, 1).astype(jnp.float32)
    large = max_exact + (jnp.log(nf / max_exact) / math.log(REL_MAX_DIST / max_exact)
                         * (REL_BUCKETS - max_exact)).astype(jnp.int32)
    large = jnp.minimum(large, REL_BUCKETS - 1)
    return jnp.where(n < max_exact, n, large)


def stick_breaking_attention(x, w_qkv, w_o):
    B, S, _ = x.shape
    H, Dh = SB_HEADS, SB_HEAD_DIM
    qkv = (x @ w_qkv).reshape(B, S, 3, H, Dh)
    q, k, v = qkv[:, :, 0], qkv[:, :, 1], qkv[:, :, 2]
    scale = Dh ** -0.5
    key_pos = jnp.arange(S)

    def block(i):
        start = i * BLOCK_Q
        q_blk = lax.dynamic_slice_in_dim(q, start, BLOCK_Q, axis=1)
        z = jnp.einsum('bqhd,bkhd->bhqk', q_blk, k).astype(jnp.float32) * scale
        q_pos = start + jnp.arange(BLOCK_Q)
        strict = key_pos[None, :] < q_pos[:, None]
        log_keep = jnp.where(strict, jax.nn.log_sigmoid(-z), 0.0)
        later = lax.cumsum(log_keep, axis=3, reverse=True) - log_keep
        a = jnp.where(strict, jnp.exp(jax.nn.log_sigmoid(z) + later), 0.0)
        return jnp.einsum('bhqk,bkhd->bqhd', a.astype(v.dtype), v)

    o = map_query_blocks(block, S // BLOCK_Q)
    return o.reshape(B, S, H * Dh) @ w_o


def mla_attention(x, pos, w_q_a, q_a_norm, w_q_b, w_kv_a, kv_a_norm, w_kv_b, w_o):
    B, S, _ = x.shape
    H = MLA_HEADS
    cq = rms_norm(x @ w_q_a, q_a_norm)
    q = (cq @ w_q_b).reshape(B, S, H, MLA_NOPE_DIM + MLA_ROPE_DIM)
    q_nope = q[..., :MLA_NOPE_DIM]
    q_rope = apply_rope(q[..., MLA_NOPE_DIM:], pos)
    kv_a = x @ w_kv_a
    c_kv = rms_norm(kv_a[..., :MLA_KV_RANK], kv_a_norm)
    k_rope = apply_rope(kv_a[..., MLA_KV_RANK:][:, :, None, :], pos)[:, :, 0]
    kv = (c_kv @ w_kv_b).reshape(B, S, H, MLA_NOPE_DIM + MLA_V_DIM)
    k_nope, v = kv[..., :MLA_NOPE_DIM], kv[..., MLA_NOPE_DIM:]
    scale = (MLA_NOPE_DIM + MLA_ROPE_DIM) ** -0.5
    key_pos = jnp.arange(S)

    def block(i):
        start = i * BLOCK_Q
        qn = lax.dynamic_slice_in_dim(q_nope, start, BLOCK_Q, axis=1)
        qr = lax.dynamic_slice_in_dim(q_rope, start, BLOCK_Q, axis=1)
        s = (jnp.einsum('bqhd,bkhd->bhqk', qn, k_nope)
             + jnp.einsum('bqhr,bkr->bhqk', qr, k_rope)).astype(jnp.float32) * scale
        causal = key_pos[None, :] <= (start + jnp.arange(BLOCK_Q))[:, None]
        p = jax.nn.softmax(jnp.where(causal, s, -jnp.inf), axis=-1)
        return jnp.einsum('bhqk,bkhd->bqhd', p.astype(v.dtype), v)

    o = map_query_blocks(block, S // BLOCK_Q)
    return o.reshape(B, S, H * MLA_V_DIM) @ w_o


def dilated_attention(x, pos, rel_bias, w_qkv, w_o):
    B, S, _ = x.shape
    G, H, Dh, K = DIL_GROUPS, DIL_HEADS, DIL_HEAD_DIM, DIL_KEYS
    qkv = (x @ w_qkv).reshape(B, S, 3, G, H, Dh)
    q, k, v = qkv[:, :, 0], qkv[:, :, 1], qkv[:, :, 2]
    dil = jnp.array(DIL_DILATIONS, jnp.int32)
    offsets = dil[:, None] * jnp.arange(K, dtype=jnp.int32)[None, :]
    bias_tab = rel_bias.reshape(REL_BUCKETS, G, H)
    g_ix = jnp.arange(G)[:, None, None]
    g_ix_b = jnp.arange(G)[None, :, None, None]
    scale = Dh ** -0.5

    def block(i):
        start = i * BLOCK_Q
        q_idx = start + jnp.arange(BLOCK_Q)
        key_idx = q_idx[None, :, None] - offsets[:, None, :]
        valid = key_idx >= 0
        kid = jnp.maximum(key_idx, 0)
        k_sel = k[:, kid, g_ix]
        v_sel = v[:, kid, g_ix]
        q_blk = lax.dynamic_slice_in_dim(q, start, BLOCK_Q, axis=1)
        s = jnp.einsum('bqghd,bgqkhd->bghqk', q_blk, k_sel).astype(jnp.float32) * scale
        rel = pos[:, q_idx][:, None, :, None] - pos[:, kid]
        bias = bias_tab[t5_bucket(rel), g_ix_b]
        s = s + jnp.moveaxis(bias, -1, 2).astype(jnp.float32)
        s = jnp.where(valid[None, :, None], s, -jnp.inf)
        lse = jax.nn.logsumexp(s, axis=-1)
        p = jnp.exp(s - lse[..., None])
        o_g = jnp.einsum('bghqk,bgqkhd->bgqhd', p.astype(v.dtype), v_sel)
        w = jax.nn.softmax(lse, axis=1)
        return jnp.einsum('bghq,bgqhd->bqhd', w.astype(v.dtype), o_g)

    o = map_query_blocks(block, S // BLOCK_Q)
    return o.reshape(B, S, H * Dh) @ w_o


def hierarchical_moe(x, w_group_router, b_group_router, w_expert_router, b_expert_router,
                     w_gate, w_up, w_down):
    B, S, _ = x.shape
    G, E = MOE_GROUPS, MOE_EXPERTS_PER_GROUP
    group_logits = (x @ w_group_router + b_group_router).astype(jnp.float32)
    group_prob = jax.nn.softmax(group_logits, axis=-1)
    g_top = jnp.argmax(group_logits, axis=-1)
    g_gate = jnp.take_along_axis(group_prob, g_top[..., None], axis=-1)
    exp_logits = (jnp.einsum('bsd,gde->bsge', x, w_expert_router)
                  + b_expert_router).astype(jnp.float32)
    sel_logits = jnp.take_along_axis(exp_logits, g_top[..., None, None], axis=2)[:, :, 0]
    top_vals, top_idx = lax.top_k(sel_logits, MOE_TOP_K)
    top_w = jax.nn.softmax(top_vals, axis=-1) * g_gate
    within = jnp.sum(jax.nn.one_hot(top_idx, E, dtype=jnp.float32) * top_w[..., None], axis=2)
    combine = jax.nn.one_hot(g_top, G, dtype=jnp.float32)[..., None] * within[:, :, None, :]
    combine = combine.reshape(B, S, G * E).astype(x.dtype)
    h = jax.nn.silu(jnp.einsum('bsd,edf->bsef', x, w_gate)) * jnp.einsum('bsd,edf->bsef', x, w_up)
    return jnp.einsum('bsef,efd->bsd', h * combine[..., None], w_down)


def setup_inputs(seed: int = 0) -> dict:
    key = jax.random.key(seed)
    ks = iter(jax.random.split(key, 32))

    def dense(shape, fan_in, gain=1.0):
        return jax.random.normal(next(ks), shape, jnp.float32) * (gain * fan_in ** -0.5)

    def gain_vec(shape):
        return 1.0 + 0.02 * jax.random.normal(next(ks), shape, jnp.float32)

    D = D_MODEL
    NE = MOE_GROUPS * MOE_EXPERTS_PER_GROUP
    x = jax.random.normal(next(ks), (BATCH, SEQ, D), jnp.float32)
    offset = jax.random.randint(next(ks), (BATCH, 1), 0, 1024, dtype=jnp.int32)
    positions = offset + jnp.arange(SEQ, dtype=jnp.int32)[None, :]
    rel_bias = 0.1 * jax.random.normal(next(ks), (REL_BUCKETS, DIL_GROUPS * DIL_HEADS), jnp.float32)
    sb_w_qkv = dense((N_SB_LAYERS, D, 3 * SB_HEADS * SB_HEAD_DIM), D)
    sb_w_o = dense((N_SB_LAYERS, SB_HEADS * SB_HEAD_DIM, D), SB_HEADS * SB_HEAD_DIM, DN_BETA)
    mla_w_q_a = dense((N_MLA_LAYERS, D, MLA_Q_RANK), D)
    mla_q_a_norm = gain_vec((N_MLA_LAYERS, MLA_Q_RANK))
    mla_w_q_b = dense((N_MLA_LAYERS, MLA_Q_RANK, MLA_HEADS * (MLA_NOPE_DIM + MLA_ROPE_DIM)), MLA_Q_RANK)
    mla_w_kv_a = dense((N_MLA_LAYERS, D, MLA_KV_RANK + MLA_ROPE_DIM), D)
    mla_kv_a_norm = gain_vec((N_MLA_LAYERS, MLA_KV_RANK))
    mla_w_kv_b = dense((N_MLA_LAYERS, MLA_KV_RANK, MLA_HEADS * (MLA_NOPE_DIM + MLA_V_DIM)), MLA_KV_RANK)
    mla_w_o = dense((N_MLA_LAYERS, MLA_HEADS * MLA_V_DIM, D), MLA_HEADS * MLA_V_DIM, DN_BETA)
    dil_w_qkv = dense((N_DIL_LAYERS, D, 3 * DIL_GROUPS * DIL_HEADS * DIL_HEAD_DIM), D)
    dil_w_o = dense((N_DIL_LAYERS, DIL_HEADS * DIL_HEAD_DIM, D), DIL_HEADS * DIL_HEAD_DIM, DN_BETA)
    ln_gain = gain_vec((DEPTH, 2, D))
    ln_bias = 0.02 * jax.random.normal(next(ks), (DEPTH, 2, D), jnp.float32)
    moe_w_group_router = dense((DEPTH, D, MOE_GROUPS), D)
    moe_b_group_router = 0.01 * jax.random.normal(next(ks), (DEPTH, MOE_GROUPS), jnp.float32)
    moe_w_expert_router = dense((DEPTH, MOE_GROUPS, D, MOE_EXPERTS_PER_GROUP), D)
    moe_b_expert_router = 0.01 * jax.random.normal(next(ks), (DEPTH, MOE_GROUPS, MOE_EXPERTS_PER_GROUP), jnp.float32)
    moe_w_gate = dense((DEPTH, NE, D, EXPERT_HIDDEN), D)
    moe_w_up = dense((DEPTH, NE, D, EXPERT_HIDDEN), D)
    moe_w_down = dense((DEPTH, NE, EXPERT_HIDDEN, D), EXPERT_HIDDEN, DN_BETA)
    return {"x": x, "positions": positions, "rel_bias": rel_bias,
            "sb_w_qkv": sb_w_qkv, "sb_w_o": sb_w_o,
            "mla_w_q_a": mla_w_q_a, "mla_q_a_norm": mla_q_a_norm, "mla_w_q_b": mla_w_q_b,
            "mla_w_kv_a": mla_w_kv_a, "mla_kv_a_norm": mla_kv_a_norm, "mla_w_kv_b": mla_w_kv_b,
            "mla_w_o": mla_w_o,
            "dil_w_qkv": dil_w_qkv, "dil_w_o": dil_w_o,
            "ln_gain": ln_gain, "ln_bias": ln_bias,
            "moe_w_group_router": moe_w_group_router, "moe_b_group_router": moe_b_group_router,
            "moe_w_expert_router": moe_w_expert_router, "moe_b_expert_router": moe_b_expert_router,
            "moe_w_gate": moe_w_gate, "moe_w_up": moe_w_up, "moe_w_down": moe_w_down}


def reference(x, positions, rel_bias,
              sb_w_qkv, sb_w_o,
              mla_w_q_a, mla_q_a_norm, mla_w_q_b, mla_w_kv_a, mla_kv_a_norm, mla_w_kv_b, mla_w_o,
              dil_w_qkv, dil_w_o,
              ln_gain, ln_bias,
              moe_w_group_router, moe_b_group_router, moe_w_expert_router, moe_b_expert_router,
              moe_w_gate, moe_w_up, moe_w_down):
    h = x
    for i in range(DEPTH):
        kind, j = i % N_MIXERS, i // N_MIXERS
        if kind == 0:
            mix = stick_breaking_attention(h, sb_w_qkv[j], sb_w_o[j])
        elif kind == 1:
            mix = mla_attention(h, positions, mla_w_q_a[j], mla_q_a_norm[j], mla_w_q_b[j],
                                mla_w_kv_a[j], mla_kv_a_norm[j], mla_w_kv_b[j], mla_w_o[j])
        else:
            mix = dilated_attention(h, positions, rel_bias, dil_w_qkv[j], dil_w_o[j])
        h = layer_norm(DN_ALPHA * h + mix, ln_gain[i, 0], ln_bias[i, 0])
        ffn = hierarchical_moe(h, moe_w_group_router[i], moe_b_group_router[i],
                               moe_w_expert_router[i], moe_b_expert_router[i],
                               moe_w_gate[i], moe_w_up[i], moe_w_down[i])
        h = layer_norm(DN_ALPHA * h + ffn, ln_gain[i, 1], ln_bias[i, 1])
    return h
```

```python
import sys, math, time
import numpy as np
import concourse.bass as bass
import concourse.mybir as mybir

F32 = mybir.dt.float32
BF16 = mybir.dt.bfloat16
I32 = mybir.dt.int32
AF = mybir.ActivationFunctionType
ALU = mybir.AluOpType
AX = mybir.AxisListType


import types


def _snap(fn):
    if fn is None or fn.__closure__ is None:
        return fn
    cells = tuple(types.CellType(c.cell_contents) for c in fn.__closure__)
    g = types.FunctionType(fn.__code__, fn.__globals__, fn.__name__, fn.__defaults__, cells)
    g.__kwdefaults__ = fn.__kwdefaults__
    return g


class Prog:
    CE = ['pe', 'act', 'dve', 'pool']
    NDS = 24

    def __init__(self, nc):
        self.nc = nc
        self.eng = {'pe': nc.tensor, 'act': nc.scalar, 'dve': nc.vector, 'pool': nc.gpsimd, 'sp': nc.sync}
        self.sem = {e: nc.alloc_semaphore('s_' + e) for e in self.CE}
        self.cnt = {e: 0 for e in self.CE}
        self.dsem = [nc.alloc_semaphore('d%d' % i) for i in range(self.NDS)]
        self.dcnt = [0] * self.NDS
        self.dma_i = 0
        self.stream = {e: [] for e in self.eng}
        self.lastw = {}
        self.readers = {}
        self.seen = {e: {} for e in self.eng}
        self.semobj = {}
        for e in self.CE:
            self.semobj[self.sem[e].num] = self.sem[e]
        for s in self.dsem:
            self.semobj[s.num] = s
        self.n_ops = 0

    def _need(self, e, events, waits):
        for ev in events:
            if ev is None:
                continue
            s, v = ev
            if self.seen[e].get(s, 0) >= v:
                continue
            self.seen[e][s] = v
            waits[s] = max(waits.get(s, 0), v)

    def _deps(self, e, reads, writes):
        waits = {}
        for k in reads:
            self._need(e, [self.lastw.get(k)], waits)
        for k in writes:
            self._need(e, [self.lastw.get(k)], waits)
            self._need(e, self.readers.get(k, []), waits)
        return waits

    def _commit(self, ev, reads, writes):
        for k in reads:
            self.readers.setdefault(k, []).append(ev)
        for k in writes:
            self.lastw[k] = ev
            self.readers[k] = []

    def op(self, e, fn, reads=(), writes=()):
        fn = _snap(fn)
        waits = self._deps(e, reads, writes)
        self.cnt[e] += 1
        ev = (self.sem[e].num, self.cnt[e])
        self.stream[e].append((waits, fn, (self.sem[e], 1)))
        self._commit(ev, reads, writes)
        self.n_ops += 1

    def dma(self, q, fn, reads=(), writes=()):
        fn = _snap(fn)
        i = self.dma_i % self.NDS
        self.dma_i += 1
        waits = self._deps(q, reads, writes)
        if self.dcnt[i] > 0:
            self._need(q, [(self.dsem[i].num, self.dcnt[i])], waits)
        self.dcnt[i] += 16
        ev = (self.dsem[i].num, self.dcnt[i])
        self.stream[q].append((waits, fn, (self.dsem[i], 16)))
        self._commit(ev, reads, writes)
        self.n_ops += 1

    def wait_all(self, e, keys):
        waits = {}
        for k in keys:
            self._need(e, [self.lastw.get(k)], waits)
        self.stream[e].append((waits, None, None))

    def emit(self):
        nc = self.nc
        names = {'pe': 'tensor', 'act': 'scalar', 'dve': 'vector', 'pool': 'gpsimd', 'sp': 'sync'}
        with nc.Block() as block:
            for e, lst in self.stream.items():
                if not lst:
                    continue

                def body(engine, lst=lst):
                    for waits, fn, inc in lst:
                        for s, v in waits.items():
                            engine.wait_ge(self.semobj[s], v)
                        if fn is not None:
                            ins = fn(engine)
                            ins.then_inc(inc[0], inc[1])
                getattr(block, names[e])(body)

import ml_dtypes
BF = ml_dtypes.bfloat16
DN_ALPHA = 8 ** 0.25
LN_EPS = 1e-5

def ln_layout(v):
    return np.ascontiguousarray(v.reshape(2, 32, 128).transpose(2, 0, 1))

def wr_layout(wgr, wer):
    w = np.concatenate([wgr] + [wer[g] for g in range(4)], axis=1)
    return np.ascontiguousarray(w.reshape(32, 128, 36).transpose(1, 0, 2))

def post_consts():
    return {"ident": np.eye(128, dtype=np.float32), "onesm": np.full((128, 128), 1.0 / 4096, np.float32)}

class WPool:
    def __init__(self, P, nc, n, nstage=3, cast_engines=('dve', 'pool')):
        self.P = P
        self.bufs = [nc.alloc_sbuf_tensor("wp%d" % i, [128, 16, 256], BF16) for i in range(n)]
        self.stage = [nc.alloc_sbuf_tensor("wst%d" % i, [128, 16, 256], F32) for i in range(nstage)]
        self.i = 0
        self.si = 0
        self.ce = cast_engines
    def load(self, src):
        i = self.i % len(self.bufs); self.i += 1
        si = self.si % len(self.stage); self.si += 1
        b = self.bufs[i]; st = self.stage[si]
        self.P.dma('sp', lambda e: e.dma_start(out=st[:], in_=src), writes=[('wst', si)])
        eng = self.ce[self.si % len(self.ce)]
        self.P.op(eng, lambda e: e.tensor_copy(out=b[:], in_=st[:]), reads=[('wst', si)], writes=[('wp', i)])
        return b, ('wp', i)

def build_post(nc, Fa, T=1024, TP=512, NW=4, mode='std', debug=False):
    KA = Fa // 128
    NP = T // TP
    D = nc.dram_tensor
    if mode == 'dil':
        ogT = D("ogT", [3, Fa, T], BF16, kind="ExternalInput").ap()
        lse = D("lse", [3, Fa // 128, 128, T], F32, kind="ExternalInput").ap()
    else:
        aT = D("aT", [Fa, T], BF16, kind="ExternalInput").ap()
    hT = D("hT", [4096, T], F32, kind="ExternalInput").ap()
    wo = D("wo", [Fa, 4096], F32, kind="ExternalInput").ap()
    lng = D("lng", [128, 2, 32], F32, kind="ExternalInput").ap()
    lnb = D("lnb", [128, 2, 32], F32, kind="ExternalInput").ap()
    wr_d = D("wr", [128, 32, 36], F32, kind="ExternalInput").ap()
    brep = D("brep", [128, 36], F32, kind="ExternalInput").ap()
    wg = D("wg", [32, 4096, 256], F32, kind="ExternalInput").ap()
    wu = D("wu", [32, 4096, 256], F32, kind="ExternalInput").ap()
    wd = D("wd", [32, 256, 4096], F32, kind="ExternalInput").ap()
    ident_d = D("ident", [128, 128], F32, kind="ExternalInput").ap()
    onesm_d = D("onesm", [128, 128], F32, kind="ExternalInput").ap()
    outT = D("outT", [4096, T], F32, kind="ExternalOutput").ap()
    if debug:
        dbg1 = D("dbg1", [4096, T], F32, kind="ExternalOutput").ap()
        dbg2 = D("dbg2", [T, 32], F32, kind="ExternalOutput").ap()
        dbg3 = D("dbg3", [4096, T], F32, kind="ExternalOutput").ap()
    A = nc.alloc_sbuf_tensor
    hf = A("hf", [128, 32, TP], F32)
    ab = A("ab", [128, 32, TP], BF16)
    act = A("act", [128, 16, TP], BF16)
    Wr = A("Wr", [128, 32, 36], F32)
    lg = A("lg", [128, 2, 32], F32); lb = A("lb", [128, 2, 32], F32)
    ident = A("ident_s", [128, 128], F32); onesm = A("onesm_s", [128, 128], F32)
    brs = A("brs", [128, 36], F32)
    tmp = [A("tmp%d" % i, [128, TP], F32) for i in range(3)]
    rstd = A("rstd", [128, TP], F32)
    comb = [A("comb%d" % i, [128, 32], F32) for i in range(TP // 128)]
    sm = A("sm", [128, 128], F32)
    PS = nc.alloc_psum_tensor
    G = [PS("G%d" % i, [128, TP], F32) for i in range(2)]
    U = [PS("U%d" % i, [128, TP], F32) for i in range(2)]
    CB = PS("CB", [128, TP], F32)
    Y = [PS("Y%d" % i, [128, TP], F32) for i in range(3)]
    P = Prog(nc)
    wp = WPool(P, nc, NW)
    P.dma('sp', lambda e: e.dma_start(out=ident[:], in_=ident_d), writes=['ident'])
    P.dma('sp', lambda e: e.dma_start(out=onesm[:], in_=onesm_d), writes=['onesm'])
    P.dma('sp', lambda e: e.dma_start(out=brs[:], in_=brep), writes=['brs'])
    P.dma('sp', lambda e: e.dma_start(out=lg[:], in_=lng), writes=['lg'])
    P.dma('sp', lambda e: e.dma_start(out=lb[:], in_=lnb), writes=['lb'])
    P.dma('sp', lambda e: e.dma_start(out=Wr[:], in_=wr_d), writes=['Wr'])

    tmpi = [0]
    def gettmp():
        i = tmpi[0] % 3; tmpi[0] += 1
        return tmp[i], ('tmp', i)

    def layer_norm(li, make_bf):
        hk = [('hf', c) for c in range(32)]
        for c in range(32):
            P.op('pe', lambda e, c=c: e.matmul(Y[0][:], lhsT=onesm[:], rhs=hf[:, c, :], start=(c == 0), stop=(c == 31)),
                 reads=[hk[c], 'onesm'], writes=['Y0'])
        for c in range(32):
            P.op('dve', lambda e, c=c: e.tensor_tensor(out=hf[:, c, :], in0=hf[:, c, :], in1=Y[0][:], op=ALU.subtract),
                 reads=['Y0', hk[c]], writes=[hk[c]])
            t, tk = gettmp()
            P.op('act', lambda e, c=c, t=t: e.activation(out=t[:], in_=hf[:, c, :], func=AF.Square), reads=[hk[c]], writes=[tk])
            P.op('pe', lambda e, c=c, t=t: e.matmul(Y[1][:], lhsT=onesm[:], rhs=t[:], start=(c == 0), stop=(c == 31)),
                 reads=[tk, 'onesm'], writes=['Y1'])
        t, tk = gettmp()
        P.op('act', lambda e: e.activation(out=t[:], in_=Y[1][:], func=AF.Sqrt, bias=LN_EPS), reads=['Y1'], writes=[tk])
        P.op('dve', lambda e: e.reciprocal(out=rstd[:], in_=t[:]), reads=[tk], writes=['rstd'])
        for c in range(32):
            P.op('dve', lambda e, c=c: e.tensor_tensor(out=hf[:, c, :], in0=hf[:, c, :], in1=rstd[:], op=ALU.mult),
                 reads=['rstd', hk[c]], writes=[hk[c]])
            P.op('act', lambda e, c=c: e.activation(out=hf[:, c, :], in_=hf[:, c, :], func=AF.Identity,
                                                    scale=lg[:, li, c:c + 1], bias=lb[:, li, c:c + 1]),
                 reads=[hk[c], 'lg', 'lb'], writes=[hk[c]])
            if make_bf:
                P.op('pool', lambda e, c=c: e.tensor_copy(out=ab[:, c, :], in_=hf[:, c, :]), reads=[hk[c]], writes=[('ab', c)])

    outkeys = []
    for tp in range(NP):
        ts = slice(tp * TP, (tp + 1) * TP)
        for c4 in range(0, 32, 8):
            P.dma('sp', lambda e, c4=c4, ts=ts: e.dma_start(out=hf[:, c4:c4 + 8, :], in_=hT[c4 * 128:(c4 + 8) * 128, ts].rearrange("(c p) t -> p c t", p=128)),
                  writes=[('hf', c) for c in range(c4, c4 + 8)])
        if mode == 'dil':
            raise NotImplementedError
        else:
            for c4 in range(0, KA, 8):
                P.dma('sp', lambda e, c4=c4, ts=ts: e.dma_start(out=ab[:, c4:c4 + 8, :], in_=aT[c4 * 128:(c4 + 8) * 128, ts].rearrange("(c p) t -> p c t", p=128)),
                      writes=[('ab', c) for c in range(c4, c4 + 8)])
        NKH = KA // 16
        for npair in range(16):
            n0 = npair * 256
            wts = [wp.load(wo[kh * 2048:(kh + 1) * 2048, n0:n0 + 256].rearrange("(c p) n -> p c n", p=128)) for kh in range(NKH)]
            for f in range(2):
                for kh in range(NKH):
                    wt, wk = wts[kh]
                    for c in range(16):
                        P.op('pe', lambda e, wt=wt, f=f, kh=kh, c=c: e.matmul(Y[f][:], lhsT=wt[:, c, f * 128:(f + 1) * 128], rhs=ab[:, kh * 16 + c, :],
                                                                             start=(kh == 0 and c == 0), stop=(kh == NKH - 1 and c == 15)),
                             reads=[wk, ('ab', kh * 16 + c)], writes=['Y%d' % f])
                j = npair * 2 + f
                P.op('dve', lambda e, j=j, f=f: e.scalar_tensor_tensor(out=hf[:, j, :], in0=hf[:, j, :], scalar=DN_ALPHA, in1=Y[f][:], op0=ALU.mult, op1=ALU.add),
                     reads=['Y%d' % f, ('hf', j)], writes=[('hf', j)])
        if debug:
            P.dma('sp', lambda e, ts=ts: e.dma_start(out=dbg3[:, ts].rearrange("(c p) t -> p c t", p=128), in_=hf[:]), reads=[('hf', c) for c in range(32)], writes=[('dbg3', tp)])
            outkeys.append(('dbg3', tp))
        layer_norm(0, True)
        if debug:
            P.dma('sp', lambda e, ts=ts: e.dma_start(out=dbg1[:, ts].rearrange("(c p) t -> p c t", p=128), in_=hf[:]), reads=[('hf', c) for c in range(32)], writes=[('dbg1', tp)])
            outkeys.append(('dbg1', tp))
        for tb in range(TP // 128):
            for c in range(32):
                P.op('pe', lambda e, c=c, tb=tb: e.matmul(CB[:, 0:36], lhsT=hf[:, c, tb * 128:(tb + 1) * 128], rhs=Wr[:, c, :], start=(c == 0), stop=(c == 31)),
                     reads=[('hf', c), 'Wr'], writes=['CB'])
            Ls = sm[:, 0:36]; gmax = sm[:, 36:37]; ngmax = sm[:, 37:38]; ohg = sm[:, 40:44]; gex = sm[:, 44:48]; gsum = sm[:, 48:49]
            ggate = sm[:, 49:50]; sel = sm[:, 52:60]; m1 = sm[:, 60:61]; oh1 = sm[:, 64:72]; sel2 = sm[:, 72:80]; m2 = sm[:, 80:81]
            oh2 = sm[:, 84:92]; dd = sm[:, 92:93]; e2 = sm[:, 93:94]; den = sm[:, 94:95]; w1 = sm[:, 95:96]; w2 = sm[:, 96:97]
            within = sm[:, 100:108]
            cb = comb[tb]
            def dv(fn, r=('sm',), w=('sm',)):
                P.op('dve', fn, reads=list(r), writes=list(w))
            dv(lambda e: e.tensor_tensor(out=Ls, in0=CB[:, 0:36], in1=brs[:], op=ALU.add), r=('CB', 'brs', 'sm'))
            dv(lambda e: e.reduce_max(out=gmax, in_=Ls[:, 0:4], axis=AX.X))
            dv(lambda e: e.tensor_scalar(out=ohg, in0=Ls[:, 0:4], scalar1=gmax, scalar2=None, op0=ALU.is_equal))
            dv(lambda e: e.tensor_scalar(out=ngmax, in0=gmax, scalar1=-1.0, scalar2=None, op0=ALU.mult))
            P.op('act', lambda e: e.activation(out=gex, in_=Ls[:, 0:4], func=AF.Exp, bias=ngmax, scale=1.0, accum_out=gsum), reads=['sm'], writes=['sm'])
            dv(lambda e: e.reciprocal(out=ggate, in_=gsum))
            dv(lambda e: e.tensor_scalar(out=sel, in0=Ls[:, 4:12], scalar1=ohg[:, 0:1], scalar2=None, op0=ALU.mult))
            for g in range(1, 4):
                dv(lambda e, g=g: e.scalar_tensor_tensor(out=sel, in0=Ls[:, 4 + 8 * g:12 + 8 * g], scalar=ohg[:, g:g + 1], in1=sel, op0=ALU.mult, op1=ALU.add))
            dv(lambda e: e.reduce_max(out=m1, in_=sel, axis=AX.X))
            dv(lambda e: e.tensor_scalar(out=oh1, in0=sel, scalar1=m1, scalar2=None, op0=ALU.is_equal))
            dv(lambda e: e.scalar_tensor_tensor(out=sel2, in0=oh1, scalar=-1e30, in1=sel, op0=ALU.mult, op1=ALU.add))
            dv(lambda e: e.reduce_max(out=m2, in_=sel2, axis=AX.X))
            dv(lambda e: e.tensor_scalar(out=oh2, in0=sel2, scalar1=m2, scalar2=None, op0=ALU.is_equal))
            dv(lambda e: e.tensor_tensor(out=dd, in0=m2, in1=m1, op=ALU.subtract))
            P.op('act', lambda e: e.activation(out=e2, in_=dd, func=AF.Exp), reads=['sm'], writes=['sm'])
            dv(lambda e: e.tensor_scalar(out=den, in0=e2, scalar1=1.0, scalar2=None, op0=ALU.add))
            dv(lambda e: e.reciprocal(out=w1, in_=den))
            dv(lambda e: e.tensor_tensor(out=w2, in0=e2, in1=w1, op=ALU.mult))
            dv(lambda e: e.tensor_tensor(out=w1, in0=w1, in1=ggate, op=ALU.mult))
            dv(lambda e: e.tensor_tensor(out=w2, in0=w2, in1=ggate, op=ALU.mult))
            dv(lambda e: e.tensor_scalar(out=within, in0=oh1, scalar1=w1, scalar2=None, op0=ALU.mult))
            dv(lambda e: e.scalar_tensor_tensor(out=within, in0=oh2, scalar=w2, in1=within, op0=ALU.mult, op1=ALU.add))
            for g in range(4):
                dv(lambda e, g=g, cb=cb: e.tensor_scalar(out=cb[:, 8 * g:8 * g + 8], in0=within, scalar1=ohg[:, g:g + 1], scalar2=None, op0=ALU.mult),
                   r=('sm',), w=(('comb', tb),))
        if debug:
            for tb in range(TP // 128):
                P.dma('sp', lambda e, tb=tb, tp=tp: e.dma_start(out=dbg2[tp * TP + tb * 128: tp * TP + (tb + 1) * 128, :], in_=comb[tb][:]), reads=[('comb', tb)], writes=[('dbg2', tp, tb)])
                outkeys.append(('dbg2', tp, tb))
        gi = 0
        for qd in range(4):
            for el in range(8):
                ex = qd * 8 + el
                wgt = [wp.load(wg[ex, kh * 2048:(kh + 1) * 2048, :].rearrange("(c p) n -> p c n", p=128)) for kh in range(2)]
                wut = [wp.load(wu[ex, kh * 2048:(kh + 1) * 2048, :].rearrange("(c p) n -> p c n", p=128)) for kh in range(2)]
                for tb in range(TP // 128):
                    P.op('pe', lambda e, tb=tb, ex=ex: e.matmul(CB[:, tb * 128:(tb + 1) * 128], lhsT=comb[tb][:, ex:ex + 1].broadcast_to([128, 128]), rhs=ident[:], start=True, stop=True),
                         reads=[('comb', tb), 'ident'], writes=['CB'])
                for f in range(2):
                    gb = gi % 2; gi += 1
                    for (bank, bk, wts_) in ((G[gb], 'G%d' % gb, wgt), (U[gb], 'U%d' % gb, wut)):
                        for kh in range(2):
                            wt, wk = wts_[kh]
                            for c in range(16):
                                P.op('pe', lambda e, bank=bank, wt=wt, f=f, kh=kh, c=c: e.matmul(bank[:], lhsT=wt[:, c, f * 128:(f + 1) * 128], rhs=ab[:, kh * 16 + c, :],
                                                                                               start=(kh == 0 and c == 0), stop=(kh == 1 and c == 15)),
                                     reads=[wk, ('ab', kh * 16 + c)], writes=[bk])
                    t, tk = gettmp()
                    P.op('act', lambda e, t=t, gb=gb: e.activation(out=t[:], in_=G[gb][:], func=AF.Silu), reads=['G%d' % gb], writes=[tk])
                    P.op('dve', lambda e, t=t, gb=gb: e.tensor_tensor(out=t[:], in0=t[:], in1=U[gb][:], op=ALU.mult), reads=['U%d' % gb, tk], writes=[tk])
                    ai = el * 2 + f
                    P.op('dve', lambda e, t=t, ai=ai: e.tensor_tensor(out=act[:, ai, :], in0=t[:], in1=CB[:], op=ALU.mult), reads=['CB', tk], writes=[('act', ai)])
            for npair in range(16):
                n0 = npair * 256
                wt, wk = wp.load(wd[qd * 8:(qd + 1) * 8, :, n0:n0 + 256].rearrange("e (fh p) n -> p (e fh) n", p=128))
                for f in range(2):
                    j = npair * 2 + f
                    yb = j % 3
                    for c in range(16):
                        P.op('pe', lambda e, wt=wt, f=f, c=c, yb=yb: e.matmul(Y[yb][:], lhsT=wt[:, c, f * 128:(f + 1) * 128], rhs=act[:, c, :], start=(c == 0), stop=(c == 15)),
                             reads=[wk, ('act', c)], writes=['Y%d' % yb])
                    if qd == 0:
                        P.op('dve', lambda e, j=j, yb=yb: e.scalar_tensor_tensor(out=hf[:, j, :], in0=hf[:, j, :], scalar=DN_ALPHA, in1=Y[yb][:], op0=ALU.mult, op1=ALU.add),
                             reads=['Y%d' % yb, ('hf', j)], writes=[('hf', j)])
                    else:
                        P.op('dve', lambda e, j=j, yb=yb: e.tensor_tensor(out=hf[:, j, :], in0=hf[:, j, :], in1=Y[yb][:], op=ALU.add),
                             reads=['Y%d' % yb, ('hf', j)], writes=[('hf', j)])
        layer_norm(1, False)
        for c4 in range(0, 32, 8):
            P.dma('sp', lambda e, c4=c4, ts=ts: e.dma_start(out=outT[c4 * 128:(c4 + 8) * 128, ts].rearrange("(c p) t -> p c t", p=128), in_=hf[:, c4:c4 + 8, :]),
                  reads=[('hf', c) for c in range(c4, c4 + 8)], writes=[('outT', tp, c4)])
            outkeys.append(('outT', tp, c4))
    P.wait_all('sp', outkeys)
    P.emit()
    return nc, P


def build_proj(nc, K, specs, T=1024, x_dtype=F32):
    KC = K // 128; NKH = K // 2048
    D = nc.dram_tensor
    xT = D("xT", [K, T], x_dtype, kind="ExternalInput").ap()
    wd = {}
    outs = {}
    for (kind, wname, wcols, col0, ncols, scale, oname) in specs:
        if wname not in wd:
            wd[wname] = D(wname, [K, wcols], F32, kind="ExternalInput").ap()
        outs[oname] = D(oname, [ncols, T] if kind == 'fm' else [T, ncols], BF16, kind="ExternalOutput").ap()
    A = nc.alloc_sbuf_tensor
    xb = A("xb", [128, KC, T], BF16)
    ob = [A("ob%d" % i, [128, 512], BF16) for i in range(4)]
    PS = [nc.alloc_psum_tensor("ps%d" % i, [128, 512], F32) for i in range(8)]
    P = Prog(nc)
    wp = WPool(P, nc, 6, nstage=3, cast_engines=('pool', 'dve'))
    for c4 in range(0, KC, 8):
        P.dma('pool', lambda e, c4=c4: e.dma_start(out=xb[:, c4:c4 + 8, :], in_=xT[c4 * 128:(c4 + 8) * 128, :].rearrange("(c p) t -> p c t", p=128)),
              writes=[('xb', c) for c in range(c4, c4 + 8)])
    outkeys = []
    pi = [0]; oi = [0]
    def evac(bank, bk, scale, dst, key):
        i = oi[0] % 4; oi[0] += 1
        o = ob[i]
        n = dst.shape[-1]
        P.op('act', lambda e: e.activation(out=o[:, 0:n], in_=bank, func=AF.Identity, scale=float(scale)), reads=[bk], writes=[('ob', i)])
        P.dma('act', lambda e: e.dma_start(out=dst, in_=o[:, 0:n]), reads=[('ob', i)], writes=[key])
        outkeys.append(key)
    for (kind, wname, wcols, col0, ncols, scale, oname) in specs:
        W = wd[wname]; O = outs[oname]
        for n0 in range(0, ncols, 256):
            nw = min(256, ncols - n0)
            wts = [wp.load(W[kh * 2048:(kh + 1) * 2048, col0 + n0:col0 + n0 + 256].rearrange("(c p) n -> p c n", p=128)) for kh in range(NKH)]
            if kind == 'fm':
                for f in range(0, nw, 128):
                    for th in range(T // 512):
                        b = pi[0] % 8; pi[0] += 1
                        for kh in range(NKH):
                            wt, wk = wts[kh]
                            for c in range(16):
                                P.op('pe', lambda e, b=b, wt=wt, f=f, kh=kh, c=c, th=th: e.matmul(PS[b][:], lhsT=wt[:, c, f:f + 128], rhs=xb[:, kh * 16 + c, th * 512:(th + 1) * 512],
                                                                                             start=(kh == 0 and c == 0), stop=(kh == NKH - 1 and c == 15)),
                                     reads=[wk, ('xb', kh * 16 + c)], writes=[('ps', b)])
                        evac(PS[b][:], ('ps', b), scale, O[n0 + f:n0 + f + 128, th * 512:(th + 1) * 512], (oname, n0, f, th))
            else:
                for tb in range(T // 128):
                    b = pi[0] % 8; pi[0] += 1
                    for kh in range(NKH):
                        wt, wk = wts[kh]
                        for c in range(16):
                            P.op('pe', lambda e, b=b, wt=wt, kh=kh, c=c, tb=tb: e.matmul(PS[b][:, 0:256], lhsT=xb[:, kh * 16 + c, tb * 128:(tb + 1) * 128], rhs=wt[:, c, :],
                                                                                     start=(kh == 0 and c == 0), stop=(kh == NKH - 1 and c == 15)),
                                 reads=[wk, ('xb', kh * 16 + c)], writes=[('ps', b)])
                    evac(PS[b][:, 0:nw], ('ps', b), scale, O[tb * 128:(tb + 1) * 128, n0:n0 + nw], (oname, n0, tb))
    P.wait_all('act', outkeys)
    P.emit()
    return nc, P

import ml_dtypes
BF = ml_dtypes.bfloat16

def sb_consts():
    j = np.arange(128)[:, None]; s = np.arange(128)[None, :]
    uneg = np.where(j >= s, -1.0, 0.0).astype(BF)
    oneg = np.full((128, 128), -1.0).astype(BF)
    t = np.arange(512)[None, :]
    masks = np.stack([((128 * r + j) < t) for r in range(4)]).astype(np.float32).astype(BF)
    return {"uneg": uneg, "oneg": oneg, "masks": masks}

def build_sb_attn(nc, NH, S):
    NB = S // 128; NQC = S // 512
    qT = nc.dram_tensor("qT", [NH, 128, S], BF16, kind="ExternalInput").ap()
    kT = nc.dram_tensor("kT", [NH, 128, S], BF16, kind="ExternalInput").ap()
    v = nc.dram_tensor("v", [NH, S, 128], BF16, kind="ExternalInput").ap()
    uneg_d = nc.dram_tensor("uneg", [128, 128], BF16, kind="ExternalInput").ap()
    oneg_d = nc.dram_tensor("oneg", [128, 128], BF16, kind="ExternalInput").ap()
    masks_d = nc.dram_tensor("masks", [4, 128, 512], BF16, kind="ExternalInput").ap()
    oT = nc.dram_tensor("oT", [NH, 128, S], BF16, kind="ExternalOutput").ap()
    A = nc.alloc_sbuf_tensor
    uneg = A("uneg_s", [128, 128], BF16); oneg = A("oneg_s", [128, 128], BF16)
    masks = A("masks_s", [128, 4, 512], BF16)
    q_s = [A("q_s%d" % i, [128, S], BF16) for i in range(2)]
    k_s = [A("k_s%d" % i, [128, S], BF16) for i in range(2)]
    v_s = [A("v_s%d" % i, [128, NB, 128], BF16) for i in range(2)]
    NE = 3
    ez = [A("ez%d" % i, [128, 512], F32) for i in range(2)]
    Pt = [A("Pt%d" % i, [128, 512], BF16) for i in range(NE)]
    Pm = [A("Pm%d" % i, [128, 512], BF16) for i in range(2)]
    At = [A("At%d" % i, [128, 512], BF16) for i in range(NE)]
    Ps = [A("Ps%d" % i, [128, 512], BF16) for i in range(NE)]
    osb = [A("osb%d" % i, [128, 512], BF16) for i in range(2)]
    E = [nc.alloc_psum_tensor("E%d" % i, [128, 512], F32) for i in range(NE)]
    O = [nc.alloc_psum_tensor("O%d" % i, [128, 512], F32) for i in range(2)]
    P = Prog(nc)
    P.dma('sp', lambda e: e.dma_start(out=uneg[:], in_=uneg_d), writes=['uneg'])
    P.dma('sp', lambda e: e.dma_start(out=oneg[:], in_=oneg_d), writes=['oneg'])
    P.dma('sp', lambda e: e.dma_start(out=masks[:], in_=masks_d.rearrange("r p t -> p r t")), writes=['masks'])
    tiles = []
    for h in range(NH):
        for qc in range(NQC):
            bs = list(range(4 * qc + 3, -1, -1))
            for n, b in enumerate(bs):
                tiles.append(dict(h=h, qc=qc, b=b, first=(n == 0), last=(n == len(bs) - 1), r=b - 4 * qc,
                                  ci=h * NQC + qc))
    loaded = set()
    outkeys = []
    def load_head(h):
        if h in loaded or h >= NH: return
        loaded.add(h)
        i = h % 2
        P.dma('sp', lambda e: e.dma_start(out=q_s[i][:], in_=qT[h]), writes=[('q', i)])
        P.dma('sp', lambda e: e.dma_start(out=k_s[i][:], in_=kT[h]), writes=[('k', i)])
        P.dma('sp', lambda e: e.dma_start(out=v_s[i][:], in_=v[h].rearrange("(b p) d -> p b d", p=128)), writes=[('v', i)])
    load_head(0)
    NT = len(tiles)
    def stageA(i):
        t = tiles[i]; hi = t['h'] % 2; ei = i % NE
        P.op('pe', lambda e: e.matmul(E[ei][:], lhsT=k_s[hi][:, t['b'] * 128:(t['b'] + 1) * 128],
                                      rhs=q_s[hi][:, t['qc'] * 512:(t['qc'] + 1) * 512], start=True, stop=False),
             reads=[('q', hi), ('k', hi)], writes=[('E', ei)])
        P.op('act', lambda e: e.activation(out=ez[i % 2][:], in_=E[ei][:], func=AF.Exp),
             reads=[('E', ei)], writes=[('ez', i % 2)])
        if t['r'] >= 0:
            P.op('act', lambda e: e.activation(out=Pm[i % 2][:], in_=ez[i % 2][:], func=AF.Ln, bias=1.0),
                 reads=[('ez', i % 2)], writes=[('Pm', i % 2)])
            P.op('dve', lambda e: e.tensor_tensor(out=Pt[ei][:], in0=Pm[i % 2][:], in1=masks[:, t['r'], :], op=ALU.mult),
                 reads=[('Pm', i % 2), 'masks'], writes=[('Pt', ei)])
        else:
            P.op('act', lambda e: e.activation(out=Pt[ei][:], in_=ez[i % 2][:], func=AF.Ln, bias=1.0),
                 reads=[('ez', i % 2)], writes=[('Pt', ei)])
    def stageB(i):
        t = tiles[i]; ei = i % NE
        P.op('pe', lambda e: e.matmul(E[ei][:], lhsT=uneg[:], rhs=Pt[ei][:], start=False, stop=t['first']),
             reads=[('Pt', ei), 'uneg'], writes=[('E', ei)])
        if not t['first']:
            P.op('pe', lambda e: e.matmul(E[ei][:], lhsT=oneg[:], rhs=Ps[ei][:], start=False, stop=True),
                 reads=[('Ps', ei), 'oneg'], writes=[('E', ei)])
        if not t['last']:
            ni = (i + 1) % NE
            if t['first']:
                P.op('pool', lambda e: e.tensor_copy(out=Ps[ni][:], in_=Pt[ei][:]), reads=[('Pt', ei)], writes=[('Ps', ni)])
            else:
                P.op('pool', lambda e: e.tensor_tensor(out=Ps[ni][:], in0=Ps[ei][:], in1=Pt[ei][:], op=ALU.add),
                     reads=[('Pt', ei), ('Ps', ei)], writes=[('Ps', ni)])
        if t['r'] >= 0:
            P.op('act', lambda e: e.activation(out=Pm[i % 2][:], in_=E[ei][:], func=AF.Exp),
                 reads=[('E', ei)], writes=[('Pm', i % 2)])
            P.op('dve', lambda e: e.tensor_tensor(out=At[ei][:], in0=Pm[i % 2][:], in1=masks[:, t['r'], :], op=ALU.mult),
                 reads=[('Pm', i % 2), 'masks'], writes=[('At', ei)])
        else:
            P.op('act', lambda e: e.activation(out=At[ei][:], in_=E[ei][:], func=AF.Exp),
                 reads=[('E', ei)], writes=[('At', ei)])
    def stageC(i):
        t = tiles[i]; hi = t['h'] % 2; ei = i % NE; oi = t['ci'] % 2
        P.op('pe', lambda e: e.matmul(O[oi][:], lhsT=v_s[hi][:, t['b'], :], rhs=At[ei][:], start=t['first'], stop=t['last']),
             reads=[('At', ei), ('v', hi)], writes=[('O', oi)])
        if t['last']:
            P.op('dve', lambda e: e.tensor_copy(out=osb[oi][:], in_=O[oi][:]), reads=[('O', oi)], writes=[('osb', oi)])
            P.dma('sp', lambda e: e.dma_start(out=oT[t['h'], :, t['qc'] * 512:(t['qc'] + 1) * 512], in_=osb[oi][:]),
                  reads=[('osb', oi)], writes=[('oT', t['ci'])])
            outkeys.append(('oT', t['ci']))
    pend = None
    for i in range(NT + 2):
        if i < NT:
            if tiles[i]['first'] and tiles[i]['qc'] == 0:
                pend = (i + 4, tiles[i]['h'] + 1)
            stageA(i)
        if pend is not None and i >= pend[0]:
            load_head(pend[1]); pend = None
        if 0 <= i - 1 < NT: stageB(i - 1)
        if 0 <= i - 2 < NT: stageC(i - 2)
    P.wait_all('sp', outkeys)
    P.emit()
    return nc, P


import ml_dtypes
BF = ml_dtypes.bfloat16
RMS_EPS = 1e-6

def mla_consts():
    half = 32
    inv = (10000.0 ** (-np.arange(half, dtype=np.float32) / half)).astype(np.float32)
    R = np.zeros((64, 64), np.float32)
    for m in range(32):
        R[m + 32, m] = -1.0
        R[m, m + 32] = 1.0
    return {"invf": np.concatenate([inv, inv])[:, None].astype(np.float32), "rotm": R, "ones1": np.ones((128, 128), np.float32)}

def build_mla_pre(nc, T=1024):
    D = nc.dram_tensor
    hT = D("hT", [4096, T], F32, kind="ExternalInput").ap()
    wqa = D("wqa", [4096, 1024], F32, kind="ExternalInput").ap()
    wkva = D("wkva", [4096, 576], F32, kind="ExternalInput").ap()
    qg = D("qg", [128, 8], F32, kind="ExternalInput").ap()
    kg = D("kg", [128, 4], F32, kind="ExternalInput").ap()
    wqb = D("wqb", [1024, 6144], F32, kind="ExternalInput").ap()
    wkvb = D("wkvb", [512, 8192], F32, kind="ExternalInput").ap()
    posr = D("posr", [64, T], I32, kind="ExternalInput").ap()
    invf_d = D("invf", [64, 1], F32, kind="ExternalInput").ap()
    rotm_d = D("rotm", [64, 64], F32, kind="ExternalInput").ap()
    ones_d = D("ones1", [128, 128], F32, kind="ExternalInput").ap()
    qnT = D("qnT", [4096, T], BF16, kind="ExternalOutput").ap()
    qrT = D("qrT", [2048, T], BF16, kind="ExternalOutput").ap()
    knT = D("knT", [4096, T], BF16, kind="ExternalOutput").ap()
    krT = D("krT", [64, T], BF16, kind="ExternalOutput").ap()
    vo = D("v", [T, 4096], BF16, kind="ExternalOutput").ap()
    A = nc.alloc_sbuf_tensor
    xb = A("xb", [128, 32, T], BF16)
    cqf = A("cqf", [128, 8, T], F32)
    kvf = A("kvf", [128, 5, T], F32)
    cqb = A("cqb", [128, 8, T], BF16)
    ckb = A("ckb", [128, 4, T], BF16)
    cos2 = A("cos2", [64, T], F32); sin2 = A("sin2", [64, T], F32)
    ang = A("ang", [64, T], F32); kf = A("kf", [64, T], F32); pi_ = A("pi_", [64, T], I32); ki = pi_
    invf = A("invf_s", [64, 1], F32); rotm = A("rotm_s", [64, 64], F32); ones1 = A("ones_s", [128, 128], F32)
    qgs = A("qgs", [128, 8], F32); kgs = A("kgs", [128, 4], F32)
    wbuf = [A("wb%d" % i, [128, 16, 256], BF16) for i in range(3)]
    wrot = [A("wrot%d" % i, [128, 8, 64], BF16) for i in range(2)]
    tmp = [A("tmp%d" % i, [128, 512], F32) for i in range(4)]
    ob = [A("ob%d" % i, [128, 512], BF16) for i in range(4)]
    rstd = A("rstd", [128, 512], F32)
    PS = [nc.alloc_psum_tensor("ps%d" % i, [128, 512], F32) for i in range(8)]
    P = Prog(nc)
    outkeys = []
    cnt = dict(w=0, p=0, t=0, o=0)
    def getw():
        i = cnt['w'] % 3; cnt['w'] += 1; return wbuf[i], ('wb', i)
    def getp():
        i = cnt['p'] % 8; cnt['p'] += 1; return PS[i], ('ps', i)
    def gett():
        i = cnt['t'] % 4; cnt['t'] += 1; return tmp[i], ('tmp', i)
    def geto():
        i = cnt['o'] % 4; cnt['o'] += 1; return ob[i], ('ob', i)
    def store(o, ok, dst, key, n=512, rows=128):
        P.dma('sp', lambda e: e.dma_start(out=dst, in_=o[0:rows, 0:n]), reads=[ok], writes=[key]); outkeys.append(key)
    for c4 in range(0, 32, 8):
        P.dma('pool', lambda e, c4=c4: e.dma_start(out=xb[:, c4:c4 + 8, :], in_=hT[c4 * 128:(c4 + 8) * 128, :].rearrange("(c p) t -> p c t", p=128)),
              writes=[('xb', c) for c in range(c4, c4 + 8)])
    for (dst, src, k) in ((invf, invf_d, 'invf'), (rotm, rotm_d, 'rotm'), (ones1, ones_d, 'ones1'), (qgs, qg, 'qgs'), (kgs, kg, 'kgs'), (pi_, posr, 'pi_')):
        P.dma('sp', lambda e, dst=dst, src=src: e.dma_start(out=dst[:], in_=src), writes=[k])
    TWO_PI = 2 * math.pi
    P.op('dve', lambda e: e.tensor_copy(out=ang[:], in_=pi_[:]), reads=['pi_'], writes=['ang'])
    P.op('dve', lambda e: e.tensor_scalar(out=ang[:], in0=ang[:], scalar1=invf[:, 0:1], scalar2=None, op0=ALU.mult), reads=['ang', 'invf'], writes=['ang'])
    def sin_tab(dst, key, shift):
        P.op('dve', lambda e: e.tensor_scalar(out=kf[:], in0=ang[:], scalar1=shift, scalar2=1.0 / TWO_PI, op0=ALU.add, op1=ALU.mult), reads=['ang'], writes=['kf'])
        P.op('dve', lambda e: e.tensor_copy(out=ki[:], in_=kf[:]), reads=['kf', 'ang'], writes=['pi_'])
        P.op('dve', lambda e: e.tensor_copy(out=kf[:], in_=ki[:]), reads=['pi_'], writes=['kf'])
        P.op('dve', lambda e: e.tensor_scalar(out=dst[:], in0=ang[:], scalar1=shift, scalar2=None, op0=ALU.add), reads=['ang'], writes=[key])
        P.op('dve', lambda e: e.scalar_tensor_tensor(out=dst[:], in0=kf[:], scalar=-TWO_PI, in1=dst[:], op0=ALU.mult, op1=ALU.add), reads=['kf', key], writes=[key])
        P.op('dve', lambda e: e.tensor_scalar(out=kf[:], in0=dst[:], scalar1=math.pi, scalar2=-TWO_PI, op0=ALU.is_gt, op1=ALU.mult), reads=[key], writes=['kf'])
        P.op('dve', lambda e: e.tensor_tensor(out=dst[:], in0=dst[:], in1=kf[:], op=ALU.add), reads=['kf', key], writes=[key])
        P.op('dve', lambda e: e.tensor_scalar(out=kf[:], in0=dst[:], scalar1=-math.pi, scalar2=TWO_PI, op0=ALU.is_lt, op1=ALU.mult), reads=[key], writes=['kf'])
        P.op('dve', lambda e: e.tensor_tensor(out=dst[:], in0=dst[:], in1=kf[:], op=ALU.add), reads=['kf', key], writes=[key])
        P.op('dve', lambda e: e.tensor_scalar(out=dst[:], in0=dst[:], scalar1=-math.pi, scalar2=math.pi, op0=ALU.max, op1=ALU.min), reads=[key], writes=[key])
        P.op('act', lambda e: e.activation(out=dst[:], in_=dst[:], func=AF.Sin), reads=[key], writes=[key])
    sin_tab(sin2, 'sin2', 0.0)
    sin_tab(cos2, 'cos2', 0.5 * math.pi)
    def proj1(W, ncols, dstf, dkey):
        for n0 in range(0, ncols, 256):
            nw = min(256, ncols - n0)
            wts = []
            for kh in range(2):
                wt, wk = getw()
                P.dma('pool', lambda e, wt=wt, kh=kh, n0=n0, nw=nw: e.dma_start(out=wt[:, :, 0:nw], in_=W[kh * 2048:(kh + 1) * 2048, n0:n0 + nw].rearrange("(c p) n -> p c n", p=128)), writes=[wk])
                wts.append((wt, wk))
            for f in range(0, nw, 128):
                m = min(128, nw - f)
                j = (n0 + f) // 128
                for th in range(T // 512):
                    ps, pk = getp()
                    for kh in range(2):
                        wt, wk = wts[kh]
                        for c in range(16):
                            P.op('pe', lambda e, ps=ps, wt=wt, f=f, m=m, kh=kh, c=c, th=th: e.matmul(ps[0:m, :], lhsT=wt[:, c, f:f + m], rhs=xb[:, kh * 16 + c, th * 512:(th + 1) * 512],
                                                                                              start=(kh == 0 and c == 0), stop=(kh == 1 and c == 15)),
                                 reads=[wk, ('xb', kh * 16 + c)], writes=[pk])
                    P.op('act', lambda e, ps=ps, m=m, j=j, th=th: e.activation(out=dstf[0:m, j, th * 512:(th + 1) * 512], in_=ps[0:m, :], func=AF.Identity),
                         reads=[pk], writes=[(dkey, j, th)])
    proj1(wqa, 1024, cqf, 'cqf')
    proj1(wkva, 576, kvf, 'kvf')
    def rms(srcf, skey, nch, gs, gkey, dstb, dkey):
        for th in range(T // 512):
            tsl = slice(th * 512, (th + 1) * 512)
            ps, pk = getp()
            for c in range(nch):
                t, tk = gett()
                P.op('act', lambda e, t=t, c=c: e.activation(out=t[:], in_=srcf[:, c, tsl], func=AF.Square), reads=[(skey, c, th)], writes=[tk])
                P.op('pe', lambda e, t=t, ps=ps, c=c: e.matmul(ps[:], lhsT=ones1[:], rhs=t[:], start=(c == 0), stop=(c == nch - 1)), reads=[tk, 'ones1'], writes=[pk])
            t, tk = gett()
            P.op('act', lambda e, t=t, ps=ps: e.activation(out=t[:], in_=ps[:], func=AF.Sqrt, scale=1.0 / (nch * 128), bias=RMS_EPS), reads=[pk], writes=[tk])
            P.op('dve', lambda e, t=t: e.reciprocal(out=rstd[:], in_=t[:]), reads=[tk], writes=['rstd'])
            for c in range(nch):
                t, tk = gett()
                P.op('dve', lambda e, t=t, c=c: e.tensor_tensor(out=t[:], in0=srcf[:, c, tsl], in1=rstd[:], op=ALU.mult), reads=[(skey, c, th), 'rstd'], writes=[tk])
                P.op('act', lambda e, t=t, c=c: e.activation(out=dstb[:, c, tsl], in_=t[:], func=AF.Identity, scale=gs[:, c:c + 1]), reads=[tk, gkey], writes=[(dkey, c, th)])
    rms(cqf, 'cqf', 8, qgs, 'qgs', cqb, 'cqb')
    rms(kvf, 'kvf', 4, kgs, 'kgs', ckb, 'ckb')
    QS = 192 ** -0.5
    for th in range(T // 512):
        tsl = slice(th * 512, (th + 1) * 512)
        ps, pk = getp()
        P.op('pe', lambda e, ps=ps: e.matmul(ps[0:64, :], lhsT=rotm[:], rhs=kvf[0:64, 4, tsl], start=True, stop=True), reads=[('kvf', 4, th), 'rotm'], writes=[pk])
        t1, k1 = gett(); t2, k2 = gett(); o, ok = geto()
        P.op('dve', lambda e, t1=t1: e.tensor_tensor(out=t1[0:64, :], in0=kvf[0:64, 4, tsl], in1=cos2[:, tsl], op=ALU.mult), reads=[('kvf', 4, th), 'cos2'], writes=[k1])
        P.op('dve', lambda e, t2=t2, ps=ps: e.tensor_tensor(out=t2[0:64, :], in0=ps[0:64, :], in1=sin2[:, tsl], op=ALU.mult), reads=[pk, 'sin2'], writes=[k2])
        P.op('dve', lambda e, t1=t1, t2=t2, o=o: e.tensor_tensor(out=o[0:64, :], in0=t1[0:64, :], in1=t2[0:64, :], op=ALU.add), reads=[k1, k2], writes=[ok])
        store(o, ok, krT[:, tsl], ('krT', th), rows=64)
    for h in range(32):
        wt, wk = getw()
        P.dma('pool', lambda e, wt=wt, h=h: e.dma_start(out=wt[:, 0:8, 0:192], in_=wqb[:, h * 192:(h + 1) * 192].rearrange("(c p) n -> p c n", p=128)), writes=[wk])
        wr = wrot[h % 2]; wrk = ('wrot', h % 2)
        P.op('act', lambda e, wt=wt, wr=wr: e.activation(out=wr[:, :, 0:32], in_=wt[:, 0:8, 160:192], func=AF.Identity, scale=-1.0), reads=[wk], writes=[wrk])
        P.op('act', lambda e, wt=wt, wr=wr: e.activation(out=wr[:, :, 32:64], in_=wt[:, 0:8, 128:160], func=AF.Identity), reads=[wk, wrk], writes=[wrk])
        for th in range(T // 512):
            tsl = slice(th * 512, (th + 1) * 512)
            ps, pk = getp()
            for c in range(8):
                P.op('pe', lambda e, ps=ps, wt=wt, c=c: e.matmul(ps[:], lhsT=wt[:, c, 0:128], rhs=cqb[:, c, tsl], start=(c == 0), stop=(c == 7)), reads=[wk, ('cqb', c, th)], writes=[pk])
            o, ok = geto()
            P.op('act', lambda e, o=o, ps=ps: e.activation(out=o[:], in_=ps[:], func=AF.Identity, scale=QS), reads=[pk], writes=[ok])
            store(o, ok, qnT[h * 128:(h + 1) * 128, tsl], ('qnT', h, th))
            px, pxk = getp(); pr, prk = getp()
            for c in range(8):
                P.op('pe', lambda e, px=px, wt=wt, c=c: e.matmul(px[0:64, :], lhsT=wt[:, c, 128:192], rhs=cqb[:, c, tsl], start=(c == 0), stop=(c == 7)), reads=[wk, ('cqb', c, th)], writes=[pxk])
            for c in range(8):
                P.op('pe', lambda e, pr=pr, wr=wr, c=c: e.matmul(pr[0:64, :], lhsT=wr[:, c, :], rhs=cqb[:, c, tsl], start=(c == 0), stop=(c == 7)), reads=[wrk, ('cqb', c, th)], writes=[prk])
            t1, k1 = gett(); t2, k2 = gett(); o, ok = geto()
            P.op('dve', lambda e, t1=t1, px=px: e.tensor_tensor(out=t1[0:64, :], in0=px[0:64, :], in1=cos2[:, tsl], op=ALU.mult), reads=[pxk, 'cos2'], writes=[k1])
            P.op('dve', lambda e, t2=t2, pr=pr: e.scalar_tensor_tensor(out=t2[0:64, :], in0=pr[0:64, :], scalar=QS, in1=sin2[:, tsl], op0=ALU.mult, op1=ALU.mult), reads=[prk, 'sin2'], writes=[k2])
            P.op('dve', lambda e, t1=t1, t2=t2, o=o: e.scalar_tensor_tensor(out=o[0:64, :], in0=t1[0:64, :], scalar=QS, in1=t2[0:64, :], op0=ALU.mult, op1=ALU.add), reads=[k1, k2], writes=[ok])
            store(o, ok, qrT[h * 64:(h + 1) * 64, tsl], ('qrT', h, th), rows=64)
    for h in range(32):
        wt, wk = getw()
        P.dma('pool', lambda e, wt=wt, h=h: e.dma_start(out=wt[:, 0:4, :], in_=wkvb[:, h * 256:(h + 1) * 256].rearrange("(c p) n -> p c n", p=128)), writes=[wk])
        for th in range(T // 512):
            tsl = slice(th * 512, (th + 1) * 512)
            ps, pk = getp()
            for c in range(4):
                P.op('pe', lambda e, ps=ps, wt=wt, c=c: e.matmul(ps[:], lhsT=wt[:, c, 0:128], rhs=ckb[:, c, tsl], start=(c == 0), stop=(c == 3)), reads=[wk, ('ckb', c, th)], writes=[pk])
            o, ok = geto()
            P.op('act', lambda e, o=o, ps=ps: e.activation(out=o[:], in_=ps[:], func=AF.Identity), reads=[pk], writes=[ok])
            store(o, ok, knT[h * 128:(h + 1) * 128, tsl], ('knT', h, th))
            ps, pk = getp()
            for tb in range(4):
                for c in range(4):
                    P.op('pe', lambda e, ps=ps, wt=wt, c=c, tb=tb, th=th: e.matmul(ps[:, tb * 128:(tb + 1) * 128], lhsT=ckb[:, c, th * 512 + tb * 128: th * 512 + (tb + 1) * 128], rhs=wt[:, c, 128:256], start=(c == 0), stop=(c == 3)),
                         reads=[wk, ('ckb', c, th)], writes=[pk])
            o, ok = geto()
            P.op('dve', lambda e, o=o, ps=ps: e.tensor_copy(out=o[:], in_=ps[:]), reads=[pk], writes=[ok])
            key = ('v', h, th)
            P.dma('sp', lambda e, o=o, h=h, th=th: e.dma_start(out=vo[th * 512:(th + 1) * 512, h * 128:(h + 1) * 128].rearrange("(b p) d -> p b d", p=128), in_=o[:].rearrange("p (b d) -> p b d", d=128)),
                  reads=[ok], writes=[key]); outkeys.append(key)
    P.wait_all('sp', outkeys)
    P.emit()
    return nc, P

import ml_dtypes
BF = ml_dtypes.bfloat16
DILS = (1, 4, 16)

def t5_thresholds():
    n = np.arange(0, 20000)
    nf = np.maximum(n, 1).astype(np.float32)
    large = 16 + (np.log(nf / np.float32(16)) / np.float32(math.log(2048 / 16)) * np.float32(16)).astype(np.int32)
    large = np.minimum(large, 31)
    bucket = np.where(n < 16, n, large)
    return [int(np.argmax(bucket >= b)) for b in range(1, 32)]

def dil_deltas(d):
    return list(range(-3, d + 1))

def attn_consts(mode):
    j = np.arange(128)[:, None]; t = np.arange(512)[None, :]
    c = {"ones": np.ones((128, 128), np.float32).astype(BF)}
    if mode == 'mla':
        c["masks"] = np.stack([((128 * r + j) <= t) for r in range(4)]).astype(np.float32).astype(BF)
    else:
        ms = []
        for d in DILS:
            for dl in dil_deltas(d):
                rel = dl * 128 + t - j
                ms.append(((rel % d) == 0) & (rel >= 0) & (rel <= 128 * d))
        c["masks"] = np.stack(ms).astype(np.float32).astype(BF)
        c["relbase"] = (t - j).astype(np.float32) + np.zeros((128, 1), np.float32)
    return c

def build_attn(nc, NH, S, mode):
    NB = S // 128; NQC = S // 512
    D = nc.dram_tensor
    A = nc.alloc_sbuf_tensor
    NG = 3 if mode == 'dil' else 1
    qT = D("qT", [NG, NH, 128, S], BF16, kind="ExternalInput").ap()
    kT = D("kT", [NG, NH, 128, S], BF16, kind="ExternalInput").ap()
    v = D("v", [NG, NH, S, 128], BF16, kind="ExternalInput").ap()
    ones_d = D("ones", [128, 128], BF16, kind="ExternalInput").ap()
    NM = 4 if mode == 'mla' else 33
    masks_d = D("masks", [NM, 128, 512], BF16, kind="ExternalInput").ap()
    oT = D("oT", [NH, 128, S], BF16, kind="ExternalOutput").ap()
    ones = A("ones_s", [128, 128], BF16)
    P = Prog(nc)
    P.dma('sp', lambda e: e.dma_start(out=ones[:], in_=ones_d), writes=['ones'])
    if mode == 'mla':
        qrT = D("qrT", [NH, 64, S], BF16, kind="ExternalInput").ap()
        krT = D("krT", [64, S], BF16, kind="ExternalInput").ap()
        masks = A("masks_s", [128, 4, 512], BF16)
        P.dma('sp', lambda e: e.dma_start(out=masks[:], in_=masks_d.rearrange("r p t -> p r t")), writes=['masks'])
        kr_s = A("kr_s", [64, S], BF16)
        P.dma('sp', lambda e: e.dma_start(out=kr_s[:], in_=krT), writes=['kr'])
        NBUF = 2
        q_s = [A("q_s%d" % i, [128, S], BF16) for i in range(2)]
        qr_s = [A("qr_s%d" % i, [64, S], BF16) for i in range(2)]
    else:
        tabrep = D("tabrep", [128, 32 * 48], F32, kind="ExternalInput").ap()
        relbase_d = D("relbase", [128, 512], F32, kind="ExternalInput").ap()
        hsel = D("hsel", [1, 2], I32, kind="ExternalInput").ap()
        NBUF = 1
        tab = A("tab", [128, 32 * 48], F32); dtab = A("dtab", [128, 31 * 48], F32)
        relbase = A("relbase_s", [128, 512], F32)
        nrel = A("nrel", [128, 512], F32); acc = A("acc", [128, 512], F32); stp = A("stp", [128, 512], F32)
        mk = [A("mk%d" % i, [128, 512], BF16) for i in range(2)]
        BM = A("BM", [128, 33, 512], BF16)
        qc_s = [A("qc_s%d" % i, [128, 3, 512], BF16) for i in range(2)]
        P.dma('sp', lambda e: e.dma_start(out=tab[:], in_=tabrep), writes=['tab'])
        P.dma('sp', lambda e: e.dma_start(out=relbase[:], in_=relbase_d), writes=['relbase'])
        P.op('dve', lambda e: e.tensor_tensor(out=dtab[:], in0=tab[:, 48:32 * 48], in1=tab[:, 0:31 * 48], op=ALU.subtract), reads=['tab'], writes=['dtab'])
    k_s = [A("k_s%d" % i, [128, NG, S], BF16) for i in range(NBUF)]
    v_s = [A("v_s%d" % i, [128, NG, NB, 128], BF16) for i in range(NBUF)]
    NE = 3
    At = [A("At%d" % i, [128, 512], BF16) for i in range(NE)]
    Am = [A("Am%d" % i, [128, 512], BF16) for i in range(2)]
    osb = [A("osb%d" % i, [128, 512], BF16) for i in range(2)]
    rl = [A("rl%d" % i, [128, 512], F32) for i in range(2)]
    E = [nc.alloc_psum_tensor("E%d" % i, [128, 512], F32) for i in range(NE)]
    O = [nc.alloc_psum_tensor("O%d" % i, [128, 512], F32) for i in range(2)]
    L = [nc.alloc_psum_tensor("L%d" % i, [128, 512], F32) for i in range(2)]
    outkeys = []
    tiles = []
    for h in range(NH):
        for qc in range(NQC):
            lst = []
            if mode == 'mla':
                for b in range(4 * qc + 3, -1, -1):
                    lst.append(dict(g=0, b=b, r=b - 4 * qc, mi=b - 4 * qc))
            else:
                mi0 = 0
                for g, d in enumerate(DILS):
                    for k, dl in enumerate(dil_deltas(d)):
                        b = 4 * qc - dl
                        if b >= 0:
                            lst.append(dict(g=g, b=b, r=0, mi=mi0 + k))
                    mi0 += len(dil_deltas(d))
            for n, t in enumerate(lst):
                t.update(h=h, qc=qc, first=(n == 0), last=(n == len(lst) - 1), ci=h * NQC + qc)
                tiles.append(t)
    thr = t5_thresholds()
    def load_head(h):
        i = h % NBUF
        for g in range(NG):
            P.dma('sp', lambda e: e.dma_start(out=k_s[i][:, g, :], in_=kT[g, h]), writes=[('k', i)])
            P.dma('sp', lambda e: e.dma_start(out=v_s[i][:, g, :, :], in_=v[g, h].rearrange("(b p) d -> p b d", p=128)), writes=[('v', i)])
        if mode == 'mla':
            P.dma('sp', lambda e: e.dma_start(out=q_s[i][:], in_=qT[0, h]), writes=[('q', i)])
            P.dma('sp', lambda e: e.dma_start(out=qr_s[i][:], in_=qrT[h]), writes=[('qr', i)])
        else:
            mi = 0
            for g, d in enumerate(DILS):
                col = g * 16
                for dl in dil_deltas(d):
                    m = mk[mi % 2]
                    P.dma('sp', lambda e: e.dma_start(out=m[:], in_=masks_d[mi]), writes=[('mk', mi % 2)])
                    P.op('dve', lambda e: e.tensor_scalar(out=nrel[:], in0=relbase[:], scalar1=float(dl * 128), scalar2=0.0, op0=ALU.add, op1=ALU.max), reads=['relbase'], writes=['nrel'])
                    for b in range(31):
                        dcol = b * 48 + col + h
                        if b == 0:
                            P.op('dve', lambda e: e.tensor_scalar(out=acc[:], in0=nrel[:], scalar1=float(thr[b]), scalar2=dtab[:, dcol:dcol + 1], op0=ALU.is_ge, op1=ALU.mult), reads=['nrel', 'dtab'], writes=['acc'])
                        else:
                            P.op('dve', lambda e: e.tensor_scalar(out=stp[:], in0=nrel[:], scalar1=float(thr[b]), scalar2=dtab[:, dcol:dcol + 1], op0=ALU.is_ge, op1=ALU.mult), reads=['nrel', 'dtab'], writes=['stp'])
                            P.op('dve', lambda e: e.tensor_tensor(out=acc[:], in0=acc[:], in1=stp[:], op=ALU.add), reads=['stp', 'acc'], writes=['acc'])
                    t0c = col + h
                    P.op('act', lambda e: e.activation(out=acc[:], in_=acc[:], func=AF.Exp, bias=tab[:, t0c:t0c + 1], scale=1.0), reads=['acc', 'tab'], writes=['acc'])
                    P.op('dve', lambda e: e.tensor_tensor(out=BM[:, mi, :], in0=acc[:], in1=m[:], op=ALU.mult), reads=['acc', ('mk', mi % 2)], writes=[('BM', mi)])
                    mi += 1
    loaded = set()
    def ensure(h):
        if h < NH and h not in loaded:
            loaded.add(h); load_head(h)
    ensure(0)
    NT = len(tiles)
    def stageA(i):
        t = tiles[i]; hi = t['h'] % NBUF; ei = i % NE; g = t['g']; b = t['b']; qc = t['qc']
        if mode == 'mla':
            P.op('pe', lambda e: e.matmul(E[ei][:], lhsT=k_s[hi][:, 0, b * 128:(b + 1) * 128], rhs=q_s[hi][:, qc * 512:(qc + 1) * 512], start=True, stop=False),
                 reads=[('q', hi), ('k', hi)], writes=[('E', ei)])
            P.op('pe', lambda e: e.matmul(E[ei][:], lhsT=kr_s[:, b * 128:(b + 1) * 128], rhs=qr_s[hi][:, qc * 512:(qc + 1) * 512], start=False, stop=True),
                 reads=[('qr', hi), 'kr'], writes=[('E', ei)])
            if t['r'] >= 0:
                P.op('act', lambda e: e.activation(out=Am[i % 2][:], in_=E[ei][:], func=AF.Exp), reads=[('E', ei)], writes=[('Am', i % 2)])
                P.op('dve', lambda e: e.tensor_tensor(out=At[ei][:], in0=Am[i % 2][:], in1=masks[:, t['r'], :], op=ALU.mult), reads=[('Am', i % 2), 'masks'], writes=[('At', ei)])
            else:
                P.op('act', lambda e: e.activation(out=At[ei][:], in_=E[ei][:], func=AF.Exp), reads=[('E', ei)], writes=[('At', ei)])
        else:
            ci = t['ci']
            if t['first']:
                P.dma('sp', lambda e: e.dma_start(out=qc_s[ci % 2][:], in_=qT[:, t['h'], :, qc * 512:(qc + 1) * 512].rearrange("g p t -> p g t")), writes=[('qc', ci % 2)])
            P.op('pe', lambda e: e.matmul(E[ei][:], lhsT=k_s[hi][:, g, b * 128:(b + 1) * 128], rhs=qc_s[ci % 2][:, g, :], start=True, stop=True),
                 reads=[('qc', ci % 2), ('k', hi)], writes=[('E', ei)])
            P.op('act', lambda e: e.activation(out=Am[i % 2][:], in_=E[ei][:], func=AF.Exp), reads=[('E', ei)], writes=[('Am', i % 2)])
            P.op('dve', lambda e: e.tensor_tensor(out=At[ei][:], in0=Am[i % 2][:], in1=BM[:, t['mi'], :], op=ALU.mult), reads=[('Am', i % 2), ('BM', t['mi'])], writes=[('At', ei)])
    def stageC(i):
        t = tiles[i]; hi = t['h'] % NBUF; ei = i % NE; oi = t['ci'] % 2; g = t['g']; b = t['b']; qc = t['qc']; h = t['h']
        P.op('pe', lambda e: e.matmul(O[oi][:], lhsT=v_s[hi][:, g, b, :], rhs=At[ei][:], start=t['first'], stop=t['last']),
             reads=[('At', ei), ('v', hi)], writes=[('O', oi)])
        P.op('pe', lambda e: e.matmul(L[oi][:], lhsT=ones[:], rhs=At[ei][:], start=t['first'], stop=t['last']),
             reads=[('At', ei), 'ones'], writes=[('L', oi)])
        if t['last']:
            P.op('dve', lambda e: e.reciprocal(out=rl[oi][:], in_=L[oi][:]), reads=[('L', oi)], writes=[('rl', oi)])
            P.op('dve', lambda e: e.tensor_tensor(out=osb[oi][:], in0=O[oi][:], in1=rl[oi][:], op=ALU.mult), reads=[('O', oi), ('rl', oi)], writes=[('osb', oi)])
            key = ('oT', t['ci'])
            P.dma('sp', lambda e: e.dma_start(out=oT[h, :, qc * 512:(qc + 1) * 512], in_=osb[oi][:]), reads=[('osb', oi)], writes=[key])
            outkeys.append(key)
    pend = None
    for i in range(NT + 1):
        drained = False
        if i < NT:
            t = tiles[i]
            if t['first'] and t['qc'] == 0:
                if NBUF == 1:
                    if i > 0:
                        stageC(i - 1); drained = True
                    ensure(t['h'])
                else:
                    pend = (i + 3, t['h'] + 1)
            stageA(i)
        if pend is not None and i >= pend[0]:
            ensure(pend[1]); pend = None
        if 0 <= i - 1 < NT and not drained: stageC(i - 1)
    P.wait_all('sp', outkeys)
    P.emit()
    return nc, P


from concourse.bass_utils import run_bass_kernel_spmd

NCORES = 8
S_FULL = 8192
TOK = S_FULL // NCORES


def _run(nc, in_maps):
    res = run_bass_kernel_spmd(nc, in_maps, core_ids=list(range(NCORES)))
    return res.results


def _c(a):
    return np.ascontiguousarray(a)


def kernel(x, positions, rel_bias, sb_w_qkv, sb_w_o, mla_w_q_a, mla_q_a_norm, mla_w_q_b, mla_w_kv_a, mla_kv_a_norm,
           mla_w_kv_b, mla_w_o, dil_w_qkv, dil_w_o, ln_gain, ln_bias, moe_w_group_router, moe_b_group_router,
           moe_w_expert_router, moe_b_expert_router, moe_w_gate, moe_w_up, moe_w_down):
    f32 = lambda a: np.asarray(a, dtype=np.float32)
    x = f32(x); S = S_FULL
    positions = np.asarray(positions).astype(np.int32)
    hT = [_c(x[0, c * TOK:(c + 1) * TOK].T) for c in range(NCORES)]
    for li in range(4):
        kind, j = li % 3, li // 3
        if kind == 0 or kind == 2:
            if kind == 0:
                w = f32(sb_w_qkv[j]); nq = 4096; sc = 128 ** -0.5; wo = f32(sb_w_o[j])
            else:
                w = f32(dil_w_qkv[j]); nq = 6144; sc = 128 ** -0.5; wo = f32(dil_w_o[j])
            specs = [('fm', 'w', 3 * nq, 0, nq, sc, 'qT'), ('fm', 'w', 3 * nq, nq, nq, 1.0, 'kT'), ('tm', 'w', 3 * nq, 2 * nq, nq, 1.0, 'v')]
            nc = bass.Bass("TRN2", target_bir_lowering=False)
            nc, _ = build_proj(nc, 4096, specs, T=TOK)
            r = _run(nc, [dict(xT=hT[c], w=w) for c in range(NCORES)])
            qT = np.concatenate([r[c]["qT"] for c in range(NCORES)], axis=1)
            kT = np.concatenate([r[c]["kT"] for c in range(NCORES)], axis=1)
            v = np.concatenate([r[c]["v"] for c in range(NCORES)], axis=0)
            del r
            if kind == 0:
                nc = bass.Bass("TRN2", target_bir_lowering=False)
                nc, _ = build_sb_attn(nc, 4, S)
                cst = sb_consts()
                ims = []
                for c in range(NCORES):
                    d = dict(qT=_c(qT[c * 512:(c + 1) * 512].reshape(4, 128, S)), kT=_c(kT[c * 512:(c + 1) * 512].reshape(4, 128, S)),
                             v=_c(v[:, c * 512:(c + 1) * 512].reshape(S, 4, 128).transpose(1, 0, 2)))
                    d.update(cst); ims.append(d)
                r = _run(nc, ims)
                aT = np.concatenate([r[c]["oT"].reshape(512, S) for c in range(NCORES)], axis=0)
                Fa = 4096
            else:
                nc = bass.Bass("TRN2", target_bir_lowering=False)
                nc, _ = build_attn(nc, 2, S, 'dil')
                cst = attn_consts('dil')
                rb = f32(rel_bias)
                ims = []
                for c in range(NCORES):
                    rows = [g * 2048 + (2 * c + hl) * 128 for g in range(3) for hl in range(2)]
                    qs = np.stack([qT[r0:r0 + 128] for r0 in rows]).reshape(3, 2, 128, S)
                    ks = np.stack([kT[r0:r0 + 128] for r0 in rows]).reshape(3, 2, 128, S)
                    vs = np.stack([v[:, r0:r0 + 128] for r0 in rows]).reshape(3, 2, S, 128)
                    tabc = np.zeros((32, 48), np.float32)
                    for g in range(3):
                        for hl in range(2):
                            tabc[:, g * 16 + hl] = rb[:, g * 16 + 2 * c + hl]
                    d = dict(qT=_c(qs), kT=_c(ks), v=_c(vs), tabrep=_c(np.tile(tabc.reshape(1, -1), (128, 1))), hsel=np.zeros((1, 2), np.int32))
                    d.update(cst); ims.append(d)
                r = _run(nc, ims)
                aT = np.concatenate([r[c]["oT"].reshape(256, S) for c in range(NCORES)], axis=0)
                Fa = 2048
            del qT, kT, v
        else:
            nc = bass.Bass("TRN2", target_bir_lowering=False)
            nc, _ = build_mla_pre(nc, T=TOK)
            cst = mla_consts()
            ims = []
            for c in range(NCORES):
                d = dict(hT=hT[c], wqa=f32(mla_w_q_a[j]), wkva=f32(mla_w_kv_a[j]), qg=_c(f32(mla_q_a_norm[j]).reshape(8, 128).T),
                         kg=_c(f32(mla_kv_a_norm[j]).reshape(4, 128).T), wqb=f32(mla_w_q_b[j]), wkvb=f32(mla_w_kv_b[j]),
                         posr=_c(np.tile(positions[0, c * TOK:(c + 1) * TOK][None, :], (64, 1))))
                d.update(cst); ims.append(d)
            r = _run(nc, ims)
            cat = lambda k, ax: np.concatenate([r[c][k] for c in range(NCORES)], axis=ax)
            qn = cat("qnT", 1); qr = cat("qrT", 1); kn = cat("knT", 1); kr = cat("krT", 1); v = cat("v", 0)
            del r
            nc = bass.Bass("TRN2", target_bir_lowering=False)
            nc, _ = build_attn(nc, 4, S, 'mla')
            cst = attn_consts('mla')
            ims = []
            for c in range(NCORES):
                d = dict(qT=_c(qn[c * 512:(c + 1) * 512].reshape(1, 4, 128, S)), qrT=_c(qr[c * 256:(c + 1) * 256].reshape(4, 64, S)),
                         kT=_c(kn[c * 512:(c + 1) * 512].reshape(1, 4, 128, S)), krT=_c(kr),
                         v=_c(v[:, c * 512:(c + 1) * 512].reshape(S, 4, 128).transpose(1, 0, 2)[None]))
                d.update(cst); ims.append(d)
            r = _run(nc, ims)
            aT = np.concatenate([r[c]["oT"].reshape(512, S) for c in range(NCORES)], axis=0)
            Fa = 4096; wo = f32(mla_w_o[j])
            del qn, qr, kn, kr, v
        nc = bass.Bass("TRN2", target_bir_lowering=False)
        nc, _ = build_post(nc, Fa, T=TOK)
        cst = post_consts()
        brep = np.tile(np.concatenate([f32(moe_b_group_router[li]), f32(moe_b_expert_router[li]).reshape(-1)])[None, :], (128, 1)).astype(np.float32)
        base = dict(wo=wo, lng=ln_layout(f32(ln_gain[li])), lnb=ln_layout(f32(ln_bias[li])),
                    wr=wr_layout(f32(moe_w_group_router[li]), f32(moe_w_expert_router[li])), brep=_c(brep),
                    wg=f32(moe_w_gate[li]), wu=f32(moe_w_up[li]), wd=f32(moe_w_down[li]))
        base.update(cst)
        ims = []
        for c in range(NCORES):
            d = dict(aT=_c(aT[:, c * TOK:(c + 1) * TOK]), hT=hT[c]); d.update(base); ims.append(d)
        r = _run(nc, ims)
        hT = [r[c]["outT"] for c in range(NCORES)]
        del r, aT
    out = np.concatenate([hT[c].T for c in range(NCORES)], axis=0)[None]
    return np.ascontiguousarray(out.astype(np.float32))
```

```python
import sys, math, time
import numpy as np
import concourse.bass as bass
import concourse.mybir as mybir

F32 = mybir.dt.float32
BF16 = mybir.dt.bfloat16
I32 = mybir.dt.int32
AF = mybir.ActivationFunctionType
ALU = mybir.AluOpType
AX = mybir.AxisListType


import types


def _snap(fn):
    if fn is None or fn.__closure__ is None:
        return fn
    cells = tuple(types.CellType(c.cell_contents) for c in fn.__closure__)
    g = types.FunctionType(fn.__code__, fn.__globals__, fn.__name__, fn.__defaults__, cells)
    g.__kwdefaults__ = fn.__kwdefaults__
    return g


class Prog:
    CE = ['pe', 'act', 'dve', 'pool']
    NDS = 24

    def __init__(self, nc):
        self.nc = nc
        self.eng = {'pe': nc.tensor, 'act': nc.scalar, 'dve': nc.vector, 'pool': nc.gpsimd, 'sp': nc.sync}
        self.sem = {e: nc.alloc_semaphore('s_' + e) for e in self.CE}
        self.cnt = {e: 0 for e in self.CE}
        self.dsem = [nc.alloc_semaphore('d%d' % i) for i in range(self.NDS)]
        self.dcnt = [0] * self.NDS
        self.dma_i = 0
        self.stream = {e: [] for e in self.eng}
        self.lastw = {}
        self.readers = {}
        self.seen = {e: {} for e in self.eng}
        self.semobj = {}
        for e in self.CE:
            self.semobj[self.sem[e].num] = self.sem[e]
        for s in self.dsem:
            self.semobj[s.num] = s
        self.n_ops = 0

    def _need(self, e, events, waits):
        for ev in events:
            if ev is None:
                continue
            s, v = ev
            if e == 'pe' and s == self.sem['pe'].num:
                continue
            if self.seen[e].get(s, 0) >= v:
                continue
            self.seen[e][s] = v
            waits[s] = max(waits.get(s, 0), v)

    def _deps(self, e, reads, writes):
        waits = {}
        for k in reads:
            self._need(e, [self.lastw.get(k)], waits)
        for k in writes:
            self._need(e, [self.lastw.get(k)], waits)
            self._need(e, self.readers.get(k, []), waits)
        return waits

    def _commit(self, ev, reads, writes):
        for k in reads:
            self.readers.setdefault(k, []).append(ev)
        for k in writes:
            self.lastw[k] = ev
            self.readers[k] = []

    def op(self, e, fn, reads=(), writes=()):
        fn = _snap(fn)
        waits = self._deps(e, reads, writes)
        self.cnt[e] += 1
        ev = (self.sem[e].num, self.cnt[e])
        self.stream[e].append((waits, fn, (self.sem[e], 1)))
        self._commit(ev, reads, writes)
        self.n_ops += 1

    def dma(self, q, fn, reads=(), writes=()):
        fn = _snap(fn)
        i = self.dma_i % self.NDS
        self.dma_i += 1
        waits = self._deps(q, reads, writes)
        if self.dcnt[i] > 0:
            self._need(q, [(self.dsem[i].num, self.dcnt[i])], waits)
        self.dcnt[i] += 16
        ev = (self.dsem[i].num, self.dcnt[i])
        self.stream[q].append((waits, fn, (self.dsem[i], 16)))
        self._commit(ev, reads, writes)
        self.n_ops += 1

    def wait_all(self, e, keys):
        waits = {}
        for k in keys:
            self._need(e, [self.lastw.get(k)], waits)
        self.stream[e].append((waits, None, None))

    def emit(self):
        nc = self.nc
        names = {'pe': 'tensor', 'act': 'scalar', 'dve': 'vector', 'pool': 'gpsimd', 'sp': 'sync'}
        with nc.Block() as block:
            for e, lst in self.stream.items():
                if not lst:
                    continue

                def body(engine, lst=lst):
                    for waits, fn, inc in lst:
                        for s, v in waits.items():
                            engine.wait_ge(self.semobj[s], v)
                        if fn is not None:
                            ins = fn(engine)
                            ins.then_inc(inc[0], inc[1])
                getattr(block, names[e])(body)

import ml_dtypes
BF = ml_dtypes.bfloat16
DN_ALPHA = 8 ** 0.25
LN_EPS = 1e-5

def ln_layout(v):
    return np.ascontiguousarray(v.reshape(2, 32, 128).transpose(2, 0, 1))

def wr_layout(wgr, wer):
    w = np.concatenate([wgr] + [wer[g] for g in range(4)], axis=1)
    return np.ascontiguousarray(w.reshape(32, 128, 36).transpose(1, 0, 2))

def post_consts():
    return {"ident": np.eye(128, dtype=np.float32), "onesm": np.full((128, 128), 1.0 / 4096, np.float32)}

class WPool:
    def __init__(self, P, nc, n, nstage=3, cast_engines=('dve', 'pool')):
        self.P = P
        self.bufs = [nc.alloc_sbuf_tensor("wp%d" % i, [128, 16, 256], BF16) for i in range(n)]
        self.stage = [nc.alloc_sbuf_tensor("wst%d" % i, [128, 16, 256], F32) for i in range(nstage)]
        self.i = 0
        self.si = 0
        self.ce = cast_engines
    def load(self, src):
        i = self.i % len(self.bufs); self.i += 1
        si = self.si % len(self.stage); self.si += 1
        b = self.bufs[i]; st = self.stage[si]
        self.P.dma('sp', lambda e: e.dma_start(out=st[:], in_=src), writes=[('wst', si)])
        eng = self.ce[self.si % len(self.ce)]
        self.P.op(eng, lambda e: e.tensor_copy(out=b[:], in_=st[:]), reads=[('wst', si)], writes=[('wp', i)])
        return b, ('wp', i)

def build_post(nc, Fa, T=1024, TP=512, NW=4, mode='std', debug=False):
    KA = Fa // 128
    NP = T // TP
    D = nc.dram_tensor
    if mode == 'dil':
        ogT = D("ogT", [3, Fa, T], BF16, kind="ExternalInput").ap()
        lse = D("lse", [3, Fa // 128, 128, T], F32, kind="ExternalInput").ap()
    else:
        aT = D("aT", [Fa, T], BF16, kind="ExternalInput").ap()
    hT = D("hT", [4096, T], F32, kind="ExternalInput").ap()
    wo = D("wo", [Fa, 4096], F32, kind="ExternalInput").ap()
    lng = D("lng", [128, 2, 32], F32, kind="ExternalInput").ap()
    lnb = D("lnb", [128, 2, 32], F32, kind="ExternalInput").ap()
    wr_d = D("wr", [128, 32, 36], F32, kind="ExternalInput").ap()
    brep = D("brep", [128, 36], F32, kind="ExternalInput").ap()
    wg = D("wg", [32, 4096, 256], F32, kind="ExternalInput").ap()
    wu = D("wu", [32, 4096, 256], F32, kind="ExternalInput").ap()
    wd = D("wd", [32, 256, 4096], F32, kind="ExternalInput").ap()
    ident_d = D("ident", [128, 128], F32, kind="ExternalInput").ap()
    onesm_d = D("onesm", [128, 128], F32, kind="ExternalInput").ap()
    outT = D("outT", [4096, T], F32, kind="ExternalOutput").ap()
    if debug:
        dbg1 = D("dbg1", [4096, T], F32, kind="ExternalOutput").ap()
        dbg2 = D("dbg2", [T, 32], F32, kind="ExternalOutput").ap()
        dbg3 = D("dbg3", [4096, T], F32, kind="ExternalOutput").ap()
    A = nc.alloc_sbuf_tensor
    hf = A("hf", [128, 32, TP], F32)
    ab = A("ab", [128, 32, TP], BF16)
    act = A("act", [128, 16, TP], BF16)
    Wr = A("Wr", [128, 32, 36], F32)
    lg = A("lg", [128, 2, 32], F32); lb = A("lb", [128, 2, 32], F32)
    ident = A("ident_s", [128, 128], F32); onesm = A("onesm_s", [128, 128], F32)
    brs = A("brs", [128, 36], F32)
    tmp = [A("tmp%d" % i, [128, TP], F32) for i in range(3)]
    rstd = A("rstd", [128, TP], F32)
    comb = [A("comb%d" % i, [128, 32], F32) for i in range(TP // 128)]
    sm = A("sm", [128, 128], F32)
    PS = nc.alloc_psum_tensor
    G = [PS("G%d" % i, [128, TP], F32) for i in range(2)]
    U = [PS("U%d" % i, [128, TP], F32) for i in range(2)]
    CB = PS("CB", [128, TP], F32)
    Y = [PS("Y%d" % i, [128, TP], F32) for i in range(3)]
    P = Prog(nc)
    wp = WPool(P, nc, NW)
    P.dma('sp', lambda e: e.dma_start(out=ident[:], in_=ident_d), writes=['ident'])
    P.dma('sp', lambda e: e.dma_start(out=onesm[:], in_=onesm_d), writes=['onesm'])
    P.dma('sp', lambda e: e.dma_start(out=brs[:], in_=brep), writes=['brs'])
    P.dma('sp', lambda e: e.dma_start(out=lg[:], in_=lng), writes=['lg'])
    P.dma('sp', lambda e: e.dma_start(out=lb[:], in_=lnb), writes=['lb'])
    P.dma('sp', lambda e: e.dma_start(out=Wr[:], in_=wr_d), writes=['Wr'])

    tmpi = [0]
    def gettmp():
        i = tmpi[0] % 3; tmpi[0] += 1
        return tmp[i], ('tmp', i)

    def layer_norm(li, make_bf):
        hk = [('hf', c) for c in range(32)]
        for c in range(32):
            P.op('pe', lambda e, c=c: e.matmul(Y[0][:], lhsT=onesm[:], rhs=hf[:, c, :], start=(c == 0), stop=(c == 31)),
                 reads=[hk[c], 'onesm'], writes=['Y0'])
        for c in range(32):
            P.op('dve', lambda e, c=c: e.tensor_tensor(out=hf[:, c, :], in0=hf[:, c, :], in1=Y[0][:], op=ALU.subtract),
                 reads=['Y0', hk[c]], writes=[hk[c]])
            t, tk = gettmp()
            P.op('act', lambda e, c=c, t=t: e.activation(out=t[:], in_=hf[:, c, :], func=AF.Square), reads=[hk[c]], writes=[tk])
            P.op('pe', lambda e, c=c, t=t: e.matmul(Y[1][:], lhsT=onesm[:], rhs=t[:], start=(c == 0), stop=(c == 31)),
                 reads=[tk, 'onesm'], writes=['Y1'])
        t, tk = gettmp()
        P.op('act', lambda e: e.activation(out=t[:], in_=Y[1][:], func=AF.Sqrt, bias=LN_EPS), reads=['Y1'], writes=[tk])
        P.op('dve', lambda e: e.reciprocal(out=rstd[:], in_=t[:]), reads=[tk], writes=['rstd'])
        for c in range(32):
            P.op('dve', lambda e, c=c: e.tensor_tensor(out=hf[:, c, :], in0=hf[:, c, :], in1=rstd[:], op=ALU.mult),
                 reads=['rstd', hk[c]], writes=[hk[c]])
            P.op('act', lambda e, c=c: e.activation(out=hf[:, c, :], in_=hf[:, c, :], func=AF.Identity,
                                                    scale=lg[:, li, c:c + 1], bias=lb[:, li, c:c + 1]),
                 reads=[hk[c], 'lg', 'lb'], writes=[hk[c]])
            if make_bf:
                P.op('pool', lambda e, c=c: e.tensor_copy(out=ab[:, c, :], in_=hf[:, c, :]), reads=[hk[c]], writes=[('ab', c)])

    outkeys = []
    for tp in range(NP):
        ts = slice(tp * TP, (tp + 1) * TP)
        for c4 in range(0, 32, 8):
            P.dma('sp', lambda e, c4=c4, ts=ts: e.dma_start(out=hf[:, c4:c4 + 8, :], in_=hT[c4 * 128:(c4 + 8) * 128, ts].rearrange("(c p) t -> p c t", p=128)),
                  writes=[('hf', c) for c in range(c4, c4 + 8)])
        if mode == 'dil':
            raise NotImplementedError
        else:
            for c4 in range(0, KA, 8):
                P.dma('sp', lambda e, c4=c4, ts=ts: e.dma_start(out=ab[:, c4:c4 + 8, :], in_=aT[c4 * 128:(c4 + 8) * 128, ts].rearrange("(c p) t -> p c t", p=128)),
                      writes=[('ab', c) for c in range(c4, c4 + 8)])
        NKH = KA // 16
        for npair in range(16):
            n0 = npair * 256
            wts = [wp.load(wo[kh * 2048:(kh + 1) * 2048, n0:n0 + 256].rearrange("(c p) n -> p c n", p=128)) for kh in range(NKH)]
            for f in range(2):
                for kh in range(NKH):
                    wt, wk = wts[kh]
                    for c in range(16):
                        P.op('pe', lambda e, wt=wt, f=f, kh=kh, c=c: e.matmul(Y[f][:], lhsT=wt[:, c, f * 128:(f + 1) * 128], rhs=ab[:, kh * 16 + c, :],
                                                                             start=(kh == 0 and c == 0), stop=(kh == NKH - 1 and c == 15)),
                             reads=[wk, ('ab', kh * 16 + c)], writes=['Y%d' % f])
                j = npair * 2 + f
                P.op('dve', lambda e, j=j, f=f: e.scalar_tensor_tensor(out=hf[:, j, :], in0=hf[:, j, :], scalar=DN_ALPHA, in1=Y[f][:], op0=ALU.mult, op1=ALU.add),
                     reads=['Y%d' % f, ('hf', j)], writes=[('hf', j)])
        if debug:
            P.dma('sp', lambda e, ts=ts: e.dma_start(out=dbg3[:, ts].rearrange("(c p) t -> p c t", p=128), in_=hf[:]), reads=[('hf', c) for c in range(32)], writes=[('dbg3', tp)])
            outkeys.append(('dbg3', tp))
        layer_norm(0, True)
        if debug:
            P.dma('sp', lambda e, ts=ts: e.dma_start(out=dbg1[:, ts].rearrange("(c p) t -> p c t", p=128), in_=hf[:]), reads=[('hf', c) for c in range(32)], writes=[('dbg1', tp)])
            outkeys.append(('dbg1', tp))
        for tb in range(TP // 128):
            for c in range(32):
                P.op('pe', lambda e, c=c, tb=tb: e.matmul(CB[:, 0:36], lhsT=hf[:, c, tb * 128:(tb + 1) * 128], rhs=Wr[:, c, :], start=(c == 0), stop=(c == 31)),
                     reads=[('hf', c), 'Wr'], writes=['CB'])
            Ls = sm[:, 0:36]; gmax = sm[:, 36:37]; ngmax = sm[:, 37:38]; ohg = sm[:, 40:44]; gex = sm[:, 44:48]; gsum = sm[:, 48:49]
            ggate = sm[:, 49:50]; sel = sm[:, 52:60]; m1 = sm[:, 60:61]; oh1 = sm[:, 64:72]; sel2 = sm[:, 72:80]; m2 = sm[:, 80:81]
            oh2 = sm[:, 84:92]; dd = sm[:, 92:93]; e2 = sm[:, 93:94]; den = sm[:, 94:95]; w1 = sm[:, 95:96]; w2 = sm[:, 96:97]
            within = sm[:, 100:108]
            cb = comb[tb]
            def dv(fn, r=('sm',), w=('sm',)):
                P.op('dve', fn, reads=list(r), writes=list(w))
            dv(lambda e: e.tensor_tensor(out=Ls, in0=CB[:, 0:36], in1=brs[:], op=ALU.add), r=('CB', 'brs', 'sm'))
            dv(lambda e: e.reduce_max(out=gmax, in_=Ls[:, 0:4], axis=AX.X))
            dv(lambda e: e.tensor_scalar(out=ohg, in0=Ls[:, 0:4], scalar1=gmax, scalar2=None, op0=ALU.is_equal))
            dv(lambda e: e.tensor_scalar(out=ngmax, in0=gmax, scalar1=-1.0, scalar2=None, op0=ALU.mult))
            P.op('act', lambda e: e.activation(out=gex, in_=Ls[:, 0:4], func=AF.Exp, bias=ngmax, scale=1.0, accum_out=gsum), reads=['sm'], writes=['sm'])
            dv(lambda e: e.reciprocal(out=ggate, in_=gsum))
            dv(lambda e: e.tensor_scalar(out=sel, in0=Ls[:, 4:12], scalar1=ohg[:, 0:1], scalar2=None, op0=ALU.mult))
            for g in range(1, 4):
                dv(lambda e, g=g: e.scalar_tensor_tensor(out=sel, in0=Ls[:, 4 + 8 * g:12 + 8 * g], scalar=ohg[:, g:g + 1], in1=sel, op0=ALU.mult, op1=ALU.add))
            dv(lambda e: e.reduce_max(out=m1, in_=sel, axis=AX.X))
            dv(lambda e: e.tensor_scalar(out=oh1, in0=sel, scalar1=m1, scalar2=None, op0=ALU.is_equal))
            dv(lambda e: e.scalar_tensor_tensor(out=sel2, in0=oh1, scalar=-1e30, in1=sel, op0=ALU.mult, op1=ALU.add))
            dv(lambda e: e.reduce_max(out=m2, in_=sel2, axis=AX.X))
            dv(lambda e: e.tensor_scalar(out=oh2, in0=sel2, scalar1=m2, scalar2=None, op0=ALU.is_equal))
            dv(lambda e: e.tensor_tensor(out=dd, in0=m2, in1=m1, op=ALU.subtract))
            P.op('act', lambda e: e.activation(out=e2, in_=dd, func=AF.Exp), reads=['sm'], writes=['sm'])
            dv(lambda e: e.tensor_scalar(out=den, in0=e2, scalar1=1.0, scalar2=None, op0=ALU.add))
            dv(lambda e: e.reciprocal(out=w1, in_=den))
            dv(lambda e: e.tensor_tensor(out=w2, in0=e2, in1=w1, op=ALU.mult))
            dv(lambda e: e.tensor_tensor(out=w1, in0=w1, in1=ggate, op=ALU.mult))
            dv(lambda e: e.tensor_tensor(out=w2, in0=w2, in1=ggate, op=ALU.mult))
            dv(lambda e: e.tensor_scalar(out=within, in0=oh1, scalar1=w1, scalar2=None, op0=ALU.mult))
            dv(lambda e: e.scalar_tensor_tensor(out=within, in0=oh2, scalar=w2, in1=within, op0=ALU.mult, op1=ALU.add))
            for g in range(4):
                dv(lambda e, g=g, cb=cb: e.tensor_scalar(out=cb[:, 8 * g:8 * g + 8], in0=within, scalar1=ohg[:, g:g + 1], scalar2=None, op0=ALU.mult),
                   r=('sm',), w=(('comb', tb),))
        if debug:
            for tb in range(TP // 128):
                P.dma('sp', lambda e, tb=tb, tp=tp: e.dma_start(out=dbg2[tp * TP + tb * 128: tp * TP + (tb + 1) * 128, :], in_=comb[tb][:]), reads=[('comb', tb)], writes=[('dbg2', tp, tb)])
                outkeys.append(('dbg2', tp, tb))
        gi = 0
        for qd in range(4):
            for el in range(8):
                ex = qd * 8 + el
                wgt = [wp.load(wg[ex, kh * 2048:(kh + 1) * 2048, :].rearrange("(c p) n -> p c n", p=128)) for kh in range(2)]
                wut = [wp.load(wu[ex, kh * 2048:(kh + 1) * 2048, :].rearrange("(c p) n -> p c n", p=128)) for kh in range(2)]
                for tb in range(TP // 128):
                    P.op('pe', lambda e, tb=tb, ex=ex: e.matmul(CB[:, tb * 128:(tb + 1) * 128], lhsT=comb[tb][:, ex:ex + 1].broadcast_to([128, 128]), rhs=ident[:], start=True, stop=True),
                         reads=[('comb', tb), 'ident'], writes=['CB'])
                for f in range(2):
                    gb = gi % 2; gi += 1
                    for (bank, bk, wts_) in ((G[gb], 'G%d' % gb, wgt), (U[gb], 'U%d' % gb, wut)):
                        for kh in range(2):
                            wt, wk = wts_[kh]
                            for c in range(16):
                                P.op('pe', lambda e, bank=bank, wt=wt, f=f, kh=kh, c=c: e.matmul(bank[:], lhsT=wt[:, c, f * 128:(f + 1) * 128], rhs=ab[:, kh * 16 + c, :],
                                                                                               start=(kh == 0 and c == 0), stop=(kh == 1 and c == 15)),
                                     reads=[wk, ('ab', kh * 16 + c)], writes=[bk])
                    t, tk = gettmp()
                    P.op('act', lambda e, t=t, gb=gb: e.activation(out=t[:], in_=G[gb][:], func=AF.Silu), reads=['G%d' % gb], writes=[tk])
                    P.op('dve', lambda e, t=t, gb=gb: e.tensor_tensor(out=t[:], in0=t[:], in1=U[gb][:], op=ALU.mult), reads=['U%d' % gb, tk], writes=[tk])
                    ai = el * 2 + f
                    P.op('dve', lambda e, t=t, ai=ai: e.tensor_tensor(out=act[:, ai, :], in0=t[:], in1=CB[:], op=ALU.mult), reads=['CB', tk], writes=[('act', ai)])
            for npair in range(16):
                n0 = npair * 256
                wt, wk = wp.load(wd[qd * 8:(qd + 1) * 8, :, n0:n0 + 256].rearrange("e (fh p) n -> p (e fh) n", p=128))
                for f in range(2):
                    j = npair * 2 + f
                    yb = j % 3
                    for c in range(16):
                        P.op('pe', lambda e, wt=wt, f=f, c=c, yb=yb: e.matmul(Y[yb][:], lhsT=wt[:, c, f * 128:(f + 1) * 128], rhs=act[:, c, :], start=(c == 0), stop=(c == 15)),
                             reads=[wk, ('act', c)], writes=['Y%d' % yb])
                    if qd == 0:
                        P.op('dve', lambda e, j=j, yb=yb: e.scalar_tensor_tensor(out=hf[:, j, :], in0=hf[:, j, :], scalar=DN_ALPHA, in1=Y[yb][:], op0=ALU.mult, op1=ALU.add),
                             reads=['Y%d' % yb, ('hf', j)], writes=[('hf', j)])
                    else:
                        P.op('dve', lambda e, j=j, yb=yb: e.tensor_tensor(out=hf[:, j, :], in0=hf[:, j, :], in1=Y[yb][:], op=ALU.add),
                             reads=['Y%d' % yb, ('hf', j)], writes=[('hf', j)])
        layer_norm(1, False)
        for c4 in range(0, 32, 8):
            P.dma('sp', lambda e, c4=c4, ts=ts: e.dma_start(out=outT[c4 * 128:(c4 + 8) * 128, ts].rearrange("(c p) t -> p c t", p=128), in_=hf[:, c4:c4 + 8, :]),
                  reads=[('hf', c) for c in range(c4, c4 + 8)], writes=[('outT', tp, c4)])
            outkeys.append(('outT', tp, c4))
    P.wait_all('sp', outkeys)
    P.emit()
    return nc, P


def build_proj(nc, K, specs, T=1024, x_dtype=F32):
    KC = K // 128; NKH = K // 2048
    D = nc.dram_tensor
    xT = D("xT", [K, T], x_dtype, kind="ExternalInput").ap()
    wd = {}
    outs = {}
    for (kind, wname, wcols, col0, ncols, scale, oname) in specs:
        if wname not in wd:
            wd[wname] = D(wname, [K, wcols], F32, kind="ExternalInput").ap()
        outs[oname] = D(oname, [ncols, T] if kind == 'fm' else [T, ncols], BF16, kind="ExternalOutput").ap()
    A = nc.alloc_sbuf_tensor
    xb = A("xb", [128, KC, T], BF16)
    ob = [A("ob%d" % i, [128, 512], BF16) for i in range(4)]
    PS = [nc.alloc_psum_tensor("ps%d" % i, [128, 512], F32) for i in range(8)]
    P = Prog(nc)
    wp = WPool(P, nc, 6, nstage=3, cast_engines=('pool', 'dve'))
    for c4 in range(0, KC, 8):
        P.dma('pool', lambda e, c4=c4: e.dma_start(out=xb[:, c4:c4 + 8, :], in_=xT[c4 * 128:(c4 + 8) * 128, :].rearrange("(c p) t -> p c t", p=128)),
              writes=[('xb', c) for c in range(c4, c4 + 8)])
    outkeys = []
    pi = [0]; oi = [0]
    def evac(bank, bk, scale, dst, key):
        i = oi[0] % 4; oi[0] += 1
        o = ob[i]
        n = dst.shape[-1]
        P.op('act', lambda e: e.activation(out=o[:, 0:n], in_=bank, func=AF.Identity, scale=float(scale)), reads=[bk], writes=[('ob', i)])
        P.dma('act', lambda e: e.dma_start(out=dst, in_=o[:, 0:n]), reads=[('ob', i)], writes=[key])
        outkeys.append(key)
    for (kind, wname, wcols, col0, ncols, scale, oname) in specs:
        W = wd[wname]; O = outs[oname]
        for n0 in range(0, ncols, 256):
            nw = min(256, ncols - n0)
            wts = [wp.load(W[kh * 2048:(kh + 1) * 2048, col0 + n0:col0 + n0 + 256].rearrange("(c p) n -> p c n", p=128)) for kh in range(NKH)]
            if kind == 'fm':
                for f in range(0, nw, 128):
                    for th in range(T // 512):
                        b = pi[0] % 8; pi[0] += 1
                        for kh in range(NKH):
                            wt, wk = wts[kh]
                            for c in range(16):
                                P.op('pe', lambda e, b=b, wt=wt, f=f, kh=kh, c=c, th=th: e.matmul(PS[b][:], lhsT=wt[:, c, f:f + 128], rhs=xb[:, kh * 16 + c, th * 512:(th + 1) * 512],
                                                                                             start=(kh == 0 and c == 0), stop=(kh == NKH - 1 and c == 15)),
                                     reads=[wk, ('xb', kh * 16 + c)], writes=[('ps', b)])
                        evac(PS[b][:], ('ps', b), scale, O[n0 + f:n0 + f + 128, th * 512:(th + 1) * 512], (oname, n0, f, th))
            else:
                for tb in range(T // 128):
                    b = pi[0] % 8; pi[0] += 1
                    for kh in range(NKH):
                        wt, wk = wts[kh]
                        for c in range(16):
                            P.op('pe', lambda e, b=b, wt=wt, kh=kh, c=c, tb=tb: e.matmul(PS[b][:, 0:256], lhsT=xb[:, kh * 16 + c, tb * 128:(tb + 1) * 128], rhs=wt[:, c, :],
                                                                                     start=(kh == 0 and c == 0), stop=(kh == NKH - 1 and c == 15)),
                                 reads=[wk, ('xb', kh * 16 + c)], writes=[('ps', b)])
                    evac(PS[b][:, 0:nw], ('ps', b), scale, O[tb * 128:(tb + 1) * 128, n0:n0 + nw], (oname, n0, tb))
    P.wait_all('act', outkeys)
    P.emit()
    return nc, P

import ml_dtypes
BF = ml_dtypes.bfloat16

def sb_consts():
    j = np.arange(128)[:, None]; s = np.arange(128)[None, :]
    uneg = np.where(j >= s, -1.0, 0.0).astype(BF)
    oneg = np.full((128, 128), -1.0).astype(BF)
    t = np.arange(512)[None, :]
    masks = np.stack([((128 * r + j) < t) for r in range(4)]).astype(np.float32).astype(BF)
    return {"uneg": uneg, "oneg": oneg, "masks": masks}

def build_sb_attn(nc, NH, S):
    NB = S // 128; NQC = S // 512
    qT = nc.dram_tensor("qT", [NH, 128, S], BF16, kind="ExternalInput").ap()
    kT = nc.dram_tensor("kT", [NH, 128, S], BF16, kind="ExternalInput").ap()
    v = nc.dram_tensor("v", [NH, S, 128], BF16, kind="ExternalInput").ap()
    uneg_d = nc.dram_tensor("uneg", [128, 128], BF16, kind="ExternalInput").ap()
    oneg_d = nc.dram_tensor("oneg", [128, 128], BF16, kind="ExternalInput").ap()
    masks_d = nc.dram_tensor("masks", [4, 128, 512], BF16, kind="ExternalInput").ap()
    oT = nc.dram_tensor("oT", [NH, 128, S], BF16, kind="ExternalOutput").ap()
    A = nc.alloc_sbuf_tensor
    uneg = A("uneg_s", [128, 128], BF16); oneg = A("oneg_s", [128, 128], BF16)
    masks = A("masks_s", [128, 4, 512], BF16)
    q_s = [A("q_s%d" % i, [128, S], BF16) for i in range(2)]
    k_s = [A("k_s%d" % i, [128, S], BF16) for i in range(2)]
    v_s = [A("v_s%d" % i, [128, NB, 128], BF16) for i in range(2)]
    NE = 3
    ez = [A("ez%d" % i, [128, 512], F32) for i in range(2)]
    Pt = [A("Pt%d" % i, [128, 512], BF16) for i in range(NE)]
    Pm = [A("Pm%d" % i, [128, 512], BF16) for i in range(2)]
    At = [A("At%d" % i, [128, 512], BF16) for i in range(NE)]
    Ps = [A("Ps%d" % i, [128, 512], BF16) for i in range(NE)]
    osb = [A("osb%d" % i, [128, 512], BF16) for i in range(2)]
    E = [nc.alloc_psum_tensor("E%d" % i, [128, 512], F32) for i in range(NE)]
    O = [nc.alloc_psum_tensor("O%d" % i, [128, 512], F32) for i in range(2)]
    P = Prog(nc)
    P.dma('sp', lambda e: e.dma_start(out=uneg[:], in_=uneg_d), writes=['uneg'])
    P.dma('sp', lambda e: e.dma_start(out=oneg[:], in_=oneg_d), writes=['oneg'])
    P.dma('sp', lambda e: e.dma_start(out=masks[:], in_=masks_d.rearrange("r p t -> p r t")), writes=['masks'])
    tiles = []
    for h in range(NH):
        for qc in range(NQC):
            bs = list(range(4 * qc + 3, -1, -1))
            for n, b in enumerate(bs):
                tiles.append(dict(h=h, qc=qc, b=b, first=(n == 0), last=(n == len(bs) - 1), r=b - 4 * qc,
                                  ci=h * NQC + qc))
    loaded = set()
    outkeys = []
    def load_head(h):
        if h in loaded or h >= NH: return
        loaded.add(h)
        i = h % 2
        P.dma('sp', lambda e: e.dma_start(out=q_s[i][:], in_=qT[h]), writes=[('q', i)])
        P.dma('sp', lambda e: e.dma_start(out=k_s[i][:], in_=kT[h]), writes=[('k', i)])
        P.dma('sp', lambda e: e.dma_start(out=v_s[i][:], in_=v[h].rearrange("(b p) d -> p b d", p=128)), writes=[('v', i)])
    load_head(0)
    NT = len(tiles)
    def stageA(i):
        t = tiles[i]; hi = t['h'] % 2; ei = i % NE
        P.op('pe', lambda e: e.matmul(E[ei][:], lhsT=k_s[hi][:, t['b'] * 128:(t['b'] + 1) * 128],
                                      rhs=q_s[hi][:, t['qc'] * 512:(t['qc'] + 1) * 512], start=True, stop=False),
             reads=[('q', hi), ('k', hi)], writes=[('E', ei)])
        P.op('act', lambda e: e.activation(out=ez[i % 2][:], in_=E[ei][:], func=AF.Exp),
             reads=[('E', ei)], writes=[('ez', i % 2)])
        if t['r'] >= 0:
            P.op('act', lambda e: e.activation(out=Pm[i % 2][:], in_=ez[i % 2][:], func=AF.Ln, bias=1.0),
                 reads=[('ez', i % 2)], writes=[('Pm', i % 2)])
            P.op('dve', lambda e: e.tensor_tensor(out=Pt[ei][:], in0=Pm[i % 2][:], in1=masks[:, t['r'], :], op=ALU.mult),
                 reads=[('Pm', i % 2), 'masks'], writes=[('Pt', ei)])
        else:
            P.op('act', lambda e: e.activation(out=Pt[ei][:], in_=ez[i % 2][:], func=AF.Ln, bias=1.0),
                 reads=[('ez', i % 2)], writes=[('Pt', ei)])
    def stageB(i):
        t = tiles[i]; ei = i % NE
        P.op('pe', lambda e: e.matmul(E[ei][:], lhsT=uneg[:], rhs=Pt[ei][:], start=False, stop=t['first']),
             reads=[('Pt', ei), 'uneg'], writes=[('E', ei)])
        if not t['first']:
            P.op('pe', lambda e: e.matmul(E[ei][:], lhsT=oneg[:], rhs=Ps[ei][:], start=False, stop=True),
                 reads=[('Ps', ei), 'oneg'], writes=[('E', ei)])
        if not t['last']:
            ni = (i + 1) % NE
            if t['first']:
                P.op('pool', lambda e: e.tensor_copy(out=Ps[ni][:], in_=Pt[ei][:]), reads=[('Pt', ei)], writes=[('Ps', ni)])
            else:
                P.op('pool', lambda e: e.tensor_tensor(out=Ps[ni][:], in0=Ps[ei][:], in1=Pt[ei][:], op=ALU.add),
                     reads=[('Pt', ei), ('Ps', ei)], writes=[('Ps', ni)])
        if t['r'] >= 0:
            P.op('act', lambda e: e.activation(out=Pm[i % 2][:], in_=E[ei][:], func=AF.Exp),
                 reads=[('E', ei)], writes=[('Pm', i % 2)])
            P.op('dve', lambda e: e.tensor_tensor(out=At[ei][:], in0=Pm[i % 2][:], in1=masks[:, t['r'], :], op=ALU.mult),
                 reads=[('Pm', i % 2), 'masks'], writes=[('At', ei)])
        else:
            P.op('act', lambda e: e.activation(out=At[ei][:], in_=E[ei][:], func=AF.Exp),
                 reads=[('E', ei)], writes=[('At', ei)])
    def stageC(i):
        t = tiles[i]; hi = t['h'] % 2; ei = i % NE; oi = t['ci'] % 2
        P.op('pe', lambda e: e.matmul(O[oi][:], lhsT=v_s[hi][:, t['b'], :], rhs=At[ei][:], start=t['first'], stop=t['last']),
             reads=[('At', ei), ('v', hi)], writes=[('O', oi)])
        if t['last']:
            P.op('dve', lambda e: e.tensor_copy(out=osb[oi][:], in_=O[oi][:]), reads=[('O', oi)], writes=[('osb', oi)])
            P.dma('sp', lambda e: e.dma_start(out=oT[t['h'], :, t['qc'] * 512:(t['qc'] + 1) * 512], in_=osb[oi][:]),
                  reads=[('osb', oi)], writes=[('oT', t['ci'])])
            outkeys.append(('oT', t['ci']))
    pend = None
    for i in range(NT + 2):
        if i < NT:
            if tiles[i]['first'] and tiles[i]['qc'] == 0:
                pend = (i + 4, tiles[i]['h'] + 1)
            stageA(i)
        if pend is not None and i >= pend[0]:
            load_head(pend[1]); pend = None
        if 0 <= i - 1 < NT: stageB(i - 1)
        if 0 <= i - 2 < NT: stageC(i - 2)
    P.wait_all('sp', outkeys)
    P.emit()
    return nc, P


import ml_dtypes
BF = ml_dtypes.bfloat16
RMS_EPS = 1e-6

def mla_consts():
    half = 32
    inv = (10000.0 ** (-np.arange(half, dtype=np.float32) / half)).astype(np.float32)
    R = np.zeros((64, 64), np.float32)
    for m in range(32):
        R[m + 32, m] = -1.0
        R[m, m + 32] = 1.0
    return {"invf": np.concatenate([inv, inv])[:, None].astype(np.float32), "rotm": R, "ones1": np.ones((128, 128), np.float32)}

def build_mla_pre(nc, T=1024):
    D = nc.dram_tensor
    hT = D("hT", [4096, T], F32, kind="ExternalInput").ap()
    wqa = D("wqa", [4096, 1024], F32, kind="ExternalInput").ap()
    wkva = D("wkva", [4096, 576], F32, kind="ExternalInput").ap()
    qg = D("qg", [128, 8], F32, kind="ExternalInput").ap()
    kg = D("kg", [128, 4], F32, kind="ExternalInput").ap()
    wqb = D("wqb", [1024, 6144], F32, kind="ExternalInput").ap()
    wkvb = D("wkvb", [512, 8192], F32, kind="ExternalInput").ap()
    posr = D("posr", [64, T], I32, kind="ExternalInput").ap()
    invf_d = D("invf", [64, 1], F32, kind="ExternalInput").ap()
    rotm_d = D("rotm", [64, 64], F32, kind="ExternalInput").ap()
    ones_d = D("ones1", [128, 128], F32, kind="ExternalInput").ap()
    qnT = D("qnT", [4096, T], BF16, kind="ExternalOutput").ap()
    qrT = D("qrT", [2048, T], BF16, kind="ExternalOutput").ap()
    knT = D("knT", [4096, T], BF16, kind="ExternalOutput").ap()
    krT = D("krT", [64, T], BF16, kind="ExternalOutput").ap()
    vo = D("v", [T, 4096], BF16, kind="ExternalOutput").ap()
    A = nc.alloc_sbuf_tensor
    xb = A("xb", [128, 32, T], BF16)
    cqf = A("cqf", [128, 8, T], F32)
    kvf = A("kvf", [128, 5, T], F32)
    cqb = A("cqb", [128, 8, T], BF16)
    ckb = A("ckb", [128, 4, T], BF16)
    cos2 = A("cos2", [64, T], F32); sin2 = A("sin2", [64, T], F32)
    ang = A("ang", [64, T], F32); kf = A("kf", [64, T], F32); pi_ = A("pi_", [64, T], I32); ki = pi_
    invf = A("invf_s", [64, 1], F32); rotm = A("rotm_s", [64, 64], F32); ones1 = A("ones_s", [128, 128], F32)
    qgs = A("qgs", [128, 8], F32); kgs = A("kgs", [128, 4], F32)
    wbuf = [A("wb%d" % i, [128, 16, 256], BF16) for i in range(3)]
    wrot = [A("wrot%d" % i, [128, 8, 64], BF16) for i in range(2)]
    tmp = [A("tmp%d" % i, [128, 512], F32) for i in range(4)]
    ob = [A("ob%d" % i, [128, 512], BF16) for i in range(4)]
    rstd = A("rstd", [128, 512], F32)
    PS = [nc.alloc_psum_tensor("ps%d" % i, [128, 512], F32) for i in range(8)]
    P = Prog(nc)
    outkeys = []
    cnt = dict(w=0, p=0, t=0, o=0)
    def getw():
        i = cnt['w'] % 3; cnt['w'] += 1; return wbuf[i], ('wb', i)
    def getp():
        i = cnt['p'] % 8; cnt['p'] += 1; return PS[i], ('ps', i)
    def gett():
        i = cnt['t'] % 4; cnt['t'] += 1; return tmp[i], ('tmp', i)
    def geto():
        i = cnt['o'] % 4; cnt['o'] += 1; return ob[i], ('ob', i)
    def store(o, ok, dst, key, n=512, rows=128):
        P.dma('sp', lambda e: e.dma_start(out=dst, in_=o[0:rows, 0:n]), reads=[ok], writes=[key]); outkeys.append(key)
    for c4 in range(0, 32, 8):
        P.dma('pool', lambda e, c4=c4: e.dma_start(out=xb[:, c4:c4 + 8, :], in_=hT[c4 * 128:(c4 + 8) * 128, :].rearrange("(c p) t -> p c t", p=128)),
              writes=[('xb', c) for c in range(c4, c4 + 8)])
    for (dst, src, k) in ((invf, invf_d, 'invf'), (rotm, rotm_d, 'rotm'), (ones1, ones_d, 'ones1'), (qgs, qg, 'qgs'), (kgs, kg, 'kgs'), (pi_, posr, 'pi_')):
        P.dma('sp', lambda e, dst=dst, src=src: e.dma_start(out=dst[:], in_=src), writes=[k])
    TWO_PI = 2 * math.pi
    P.op('dve', lambda e: e.tensor_copy(out=ang[:], in_=pi_[:]), reads=['pi_'], writes=['ang'])
    P.op('dve', lambda e: e.tensor_scalar(out=ang[:], in0=ang[:], scalar1=invf[:, 0:1], scalar2=None, op0=ALU.mult), reads=['ang', 'invf'], writes=['ang'])
    def sin_tab(dst, key, shift):
        P.op('dve', lambda e: e.tensor_scalar(out=kf[:], in0=ang[:], scalar1=shift, scalar2=1.0 / TWO_PI, op0=ALU.add, op1=ALU.mult), reads=['ang'], writes=['kf'])
        P.op('dve', lambda e: e.tensor_copy(out=ki[:], in_=kf[:]), reads=['kf', 'ang'], writes=['pi_'])
        P.op('dve', lambda e: e.tensor_copy(out=kf[:], in_=ki[:]), reads=['pi_'], writes=['kf'])
        P.op('dve', lambda e: e.tensor_scalar(out=dst[:], in0=ang[:], scalar1=shift, scalar2=None, op0=ALU.add), reads=['ang'], writes=[key])
        P.op('dve', lambda e: e.scalar_tensor_tensor(out=dst[:], in0=kf[:], scalar=-TWO_PI, in1=dst[:], op0=ALU.mult, op1=ALU.add), reads=['kf', key], writes=[key])
        P.op('dve', lambda e: e.tensor_scalar(out=kf[:], in0=dst[:], scalar1=math.pi, scalar2=-TWO_PI, op0=ALU.is_gt, op1=ALU.mult), reads=[key], writes=['kf'])
        P.op('dve', lambda e: e.tensor_tensor(out=dst[:], in0=dst[:], in1=kf[:], op=ALU.add), reads=['kf', key], writes=[key])
        P.op('dve', lambda e: e.tensor_scalar(out=kf[:], in0=dst[:], scalar1=-math.pi, scalar2=TWO_PI, op0=ALU.is_lt, op1=ALU.mult), reads=[key], writes=['kf'])
        P.op('dve', lambda e: e.tensor_tensor(out=dst[:], in0=dst[:], in1=kf[:], op=ALU.add), reads=['kf', key], writes=[key])
        P.op('dve', lambda e: e.tensor_scalar(out=dst[:], in0=dst[:], scalar1=-math.pi, scalar2=math.pi, op0=ALU.max, op1=ALU.min), reads=[key], writes=[key])
        P.op('act', lambda e: e.activation(out=dst[:], in_=dst[:], func=AF.Sin), reads=[key], writes=[key])
    sin_tab(sin2, 'sin2', 0.0)
    sin_tab(cos2, 'cos2', 0.5 * math.pi)
    def proj1(W, ncols, dstf, dkey):
        for n0 in range(0, ncols, 256):
            nw = min(256, ncols - n0)
            wts = []
            for kh in range(2):
                wt, wk = getw()
                P.dma('pool', lambda e, wt=wt, kh=kh, n0=n0, nw=nw: e.dma_start(out=wt[:, :, 0:nw], in_=W[kh * 2048:(kh + 1) * 2048, n0:n0 + nw].rearrange("(c p) n -> p c n", p=128)), writes=[wk])
                wts.append((wt, wk))
            for f in range(0, nw, 128):
                m = min(128, nw - f)
                j = (n0 + f) // 128
                for th in range(T // 512):
                    ps, pk = getp()
                    for kh in range(2):
                        wt, wk = wts[kh]
                        for c in range(16):
                            P.op('pe', lambda e, ps=ps, wt=wt, f=f, m=m, kh=kh, c=c, th=th: e.matmul(ps[0:m, :], lhsT=wt[:, c, f:f + m], rhs=xb[:, kh * 16 + c, th * 512:(th + 1) * 512],
                                                                                              start=(kh == 0 and c == 0), stop=(kh == 1 and c == 15)),
                                 reads=[wk, ('xb', kh * 16 + c)], writes=[pk])
                    P.op('act', lambda e, ps=ps, m=m, j=j, th=th: e.activation(out=dstf[0:m, j, th * 512:(th + 1) * 512], in_=ps[0:m, :], func=AF.Identity),
                         reads=[pk], writes=[(dkey, j, th)])
    proj1(wqa, 1024, cqf, 'cqf')
    proj1(wkva, 576, kvf, 'kvf')
    def rms(srcf, skey, nch, gs, gkey, dstb, dkey):
        for th in range(T // 512):
            tsl = slice(th * 512, (th + 1) * 512)
            ps, pk = getp()
            for c in range(nch):
                t, tk = gett()
                P.op('act', lambda e, t=t, c=c: e.activation(out=t[:], in_=srcf[:, c, tsl], func=AF.Square), reads=[(skey, c, th)], writes=[tk])
                P.op('pe', lambda e, t=t, ps=ps, c=c: e.matmul(ps[:], lhsT=ones1[:], rhs=t[:], start=(c == 0), stop=(c == nch - 1)), reads=[tk, 'ones1'], writes=[pk])
            t, tk = gett()
            P.op('act', lambda e, t=t, ps=ps: e.activation(out=t[:], in_=ps[:], func=AF.Sqrt, scale=1.0 / (nch * 128), bias=RMS_EPS), reads=[pk], writes=[tk])
            P.op('dve', lambda e, t=t: e.reciprocal(out=rstd[:], in_=t[:]), reads=[tk], writes=['rstd'])
            for c in range(nch):
                t, tk = gett()
                P.op('dve', lambda e, t=t, c=c: e.tensor_tensor(out=t[:], in0=srcf[:, c, tsl], in1=rstd[:], op=ALU.mult), reads=[(skey, c, th), 'rstd'], writes=[tk])
                P.op('act', lambda e, t=t, c=c: e.activation(out=dstb[:, c, tsl], in_=t[:], func=AF.Identity, scale=gs[:, c:c + 1]), reads=[tk, gkey], writes=[(dkey, c, th)])
    rms(cqf, 'cqf', 8, qgs, 'qgs', cqb, 'cqb')
    rms(kvf, 'kvf', 4, kgs, 'kgs', ckb, 'ckb')
    QS = 192 ** -0.5
    for th in range(T // 512):
        tsl = slice(th * 512, (th + 1) * 512)
        ps, pk = getp()
        P.op('pe', lambda e, ps=ps: e.matmul(ps[0:64, :], lhsT=rotm[:], rhs=kvf[0:64, 4, tsl], start=True, stop=True), reads=[('kvf', 4, th), 'rotm'], writes=[pk])
        t1, k1 = gett(); t2, k2 = gett(); o, ok = geto()
        P.op('dve', lambda e, t1=t1: e.tensor_tensor(out=t1[0:64, :], in0=kvf[0:64, 4, tsl], in1=cos2[:, tsl], op=ALU.mult), reads=[('kvf', 4, th), 'cos2'], writes=[k1])
        P.op('dve', lambda e, t2=t2, ps=ps: e.tensor_tensor(out=t2[0:64, :], in0=ps[0:64, :], in1=sin2[:, tsl], op=ALU.mult), reads=[pk, 'sin2'], writes=[k2])
        P.op('dve', lambda e, t1=t1, t2=t2, o=o: e.tensor_tensor(out=o[0:64, :], in0=t1[0:64, :], in1=t2[0:64, :], op=ALU.add), reads=[k1, k2], writes=[ok])
        store(o, ok, krT[:, tsl], ('krT', th), rows=64)
    for h in range(32):
        wt, wk = getw()
        P.dma('pool', lambda e, wt=wt, h=h: e.dma_start(out=wt[:, 0:8, 0:192], in_=wqb[:, h * 192:(h + 1) * 192].rearrange("(c p) n -> p c n", p=128)), writes=[wk])
        wr = wrot[h % 2]; wrk = ('wrot', h % 2)
        P.op('act', lambda e, wt=wt, wr=wr: e.activation(out=wr[:, :, 0:32], in_=wt[:, 0:8, 160:192], func=AF.Identity, scale=-1.0), reads=[wk], writes=[wrk])
        P.op('act', lambda e, wt=wt, wr=wr: e.activation(out=wr[:, :, 32:64], in_=wt[:, 0:8, 128:160], func=AF.Identity), reads=[wk, wrk], writes=[wrk])
        for th in range(T // 512):
            tsl = slice(th * 512, (th + 1) * 512)
            ps, pk = getp()
            for c in range(8):
                P.op('pe', lambda e, ps=ps, wt=wt, c=c: e.matmul(ps[:], lhsT=wt[:, c, 0:128], rhs=cqb[:, c, tsl], start=(c == 0), stop=(c == 7)), reads=[wk, ('cqb', c, th)], writes=[pk])
            o, ok = geto()
            P.op('act', lambda e, o=o, ps=ps: e.activation(out=o[:], in_=ps[:], func=AF.Identity, scale=QS), reads=[pk], writes=[ok])
            store(o, ok, qnT[h * 128:(h + 1) * 128, tsl], ('qnT', h, th))
            px, pxk = getp(); pr, prk = getp()
            for c in range(8):
                P.op('pe', lambda e, px=px, wt=wt, c=c: e.matmul(px[0:64, :], lhsT=wt[:, c, 128:192], rhs=cqb[:, c, tsl], start=(c == 0), stop=(c == 7)), reads=[wk, ('cqb', c, th)], writes=[pxk])
            for c in range(8):
                P.op('pe', lambda e, pr=pr, wr=wr, c=c: e.matmul(pr[0:64, :], lhsT=wr[:, c, :], rhs=cqb[:, c, tsl], start=(c == 0), stop=(c == 7)), reads=[wrk, ('cqb', c, th)], writes=[prk])
            t1, k1 = gett(); t2, k2 = gett(); o, ok = geto()
            P.op('dve', lambda e, t1=t1, px=px: e.tensor_tensor(out=t1[0:64, :], in0=px[0:64, :], in1=cos2[:, tsl], op=ALU.mult), reads=[pxk, 'cos2'], writes=[k1])
            P.op('dve', lambda e, t2=t2, pr=pr: e.scalar_tensor_tensor(out=t2[0:64, :], in0=pr[0:64, :], scalar=QS, in1=sin2[:, tsl], op0=ALU.mult, op1=ALU.mult), reads=[prk, 'sin2'], writes=[k2])
            P.op('dve', lambda e, t1=t1, t2=t2, o=o: e.scalar_tensor_tensor(out=o[0:64, :], in0=t1[0:64, :], scalar=QS, in1=t2[0:64, :], op0=ALU.mult, op1=ALU.add), reads=[k1, k2], writes=[ok])
            store(o, ok, qrT[h * 64:(h + 1) * 64, tsl], ('qrT', h, th), rows=64)
    for h in range(32):
        wt, wk = getw()
        P.dma('pool', lambda e, wt=wt, h=h: e.dma_start(out=wt[:, 0:4, :], in_=wkvb[:, h * 256:(h + 1) * 256].rearrange("(c p) n -> p c n", p=128)), writes=[wk])
        for th in range(T // 512):
            tsl = slice(th * 512, (th + 1) * 512)
            ps, pk = getp()
            for c in range(4):
                P.op('pe', lambda e, ps=ps, wt=wt, c=c: e.matmul(ps[:], lhsT=wt[:, c, 0:128], rhs=ckb[:, c, tsl], start=(c == 0), stop=(c == 3)), reads=[wk, ('ckb', c, th)], writes=[pk])
            o, ok = geto()
            P.op('act', lambda e, o=o, ps=ps: e.activation(out=o[:], in_=ps[:], func=AF.Identity), reads=[pk], writes=[ok])
            store(o, ok, knT[h * 128:(h + 1) * 128, tsl], ('knT', h, th))
            ps, pk = getp()
            for tb in range(4):
                for c in range(4):
                    P.op('pe', lambda e, ps=ps, wt=wt, c=c, tb=tb, th=th: e.matmul(ps[:, tb * 128:(tb + 1) * 128], lhsT=ckb[:, c, th * 512 + tb * 128: th * 512 + (tb + 1) * 128], rhs=wt[:, c, 128:256], start=(c == 0), stop=(c == 3)),
                         reads=[wk, ('ckb', c, th)], writes=[pk])
            o, ok = geto()
            P.op('dve', lambda e, o=o, ps=ps: e.tensor_copy(out=o[:], in_=ps[:]), reads=[pk], writes=[ok])
            key = ('v', h, th)
            P.dma('sp', lambda e, o=o, h=h, th=th: e.dma_start(out=vo[th * 512:(th + 1) * 512, h * 128:(h + 1) * 128].rearrange("(b p) d -> p b d", p=128), in_=o[:].rearrange("p (b d) -> p b d", d=128)),
                  reads=[ok], writes=[key]); outkeys.append(key)
    P.wait_all('sp', outkeys)
    P.emit()
    return nc, P

import ml_dtypes
BF = ml_dtypes.bfloat16
DILS = (1, 4, 16)

def t5_thresholds():
    n = np.arange(0, 20000)
    nf = np.maximum(n, 1).astype(np.float32)
    large = 16 + (np.log(nf / np.float32(16)) / np.float32(math.log(2048 / 16)) * np.float32(16)).astype(np.int32)
    large = np.minimum(large, 31)
    bucket = np.where(n < 16, n, large)
    return [int(np.argmax(bucket >= b)) for b in range(1, 32)]

def dil_deltas(d):
    return list(range(-3, d + 1))

def attn_consts(mode):
    j = np.arange(128)[:, None]; t = np.arange(512)[None, :]
    c = {"ones": np.ones((128, 128), np.float32).astype(BF)}
    if mode == 'mla':
        c["masks"] = np.stack([((128 * r + j) <= t) for r in range(4)]).astype(np.float32).astype(BF)
    else:
        ms = []
        for d in DILS:
            for dl in dil_deltas(d):
                rel = dl * 128 + t - j
                ms.append(((rel % d) == 0) & (rel >= 0) & (rel <= 128 * d))
        c["masks"] = np.stack(ms).astype(np.float32).astype(BF)
        c["relbase"] = (t - j).astype(np.float32) + np.zeros((128, 1), np.float32)
    return c

def build_attn(nc, NH, S, mode):
    NB = S // 128; NQC = S // 512
    D = nc.dram_tensor
    A = nc.alloc_sbuf_tensor
    NG = 3 if mode == 'dil' else 1
    qT = D("qT", [NG, NH, 128, S], BF16, kind="ExternalInput").ap()
    kT = D("kT", [NG, NH, 128, S], BF16, kind="ExternalInput").ap()
    v = D("v", [NG, NH, S, 128], BF16, kind="ExternalInput").ap()
    ones_d = D("ones", [128, 128], BF16, kind="ExternalInput").ap()
    NM = 4 if mode == 'mla' else 33
    masks_d = D("masks", [NM, 128, 512], BF16, kind="ExternalInput").ap()
    oT = D("oT", [NH, 128, S], BF16, kind="ExternalOutput").ap()
    ones = A("ones_s", [128, 128], BF16)
    P = Prog(nc)
    P.dma('sp', lambda e: e.dma_start(out=ones[:], in_=ones_d), writes=['ones'])
    if mode == 'mla':
        qrT = D("qrT", [NH, 64, S], BF16, kind="ExternalInput").ap()
        krT = D("krT", [64, S], BF16, kind="ExternalInput").ap()
        masks = A("masks_s", [128, 4, 512], BF16)
        P.dma('sp', lambda e: e.dma_start(out=masks[:], in_=masks_d.rearrange("r p t -> p r t")), writes=['masks'])
        kr_s = A("kr_s", [64, S], BF16)
        P.dma('sp', lambda e: e.dma_start(out=kr_s[:], in_=krT), writes=['kr'])
        NBUF = 2
        q_s = [A("q_s%d" % i, [128, S], BF16) for i in range(2)]
        qr_s = [A("qr_s%d" % i, [64, S], BF16) for i in range(2)]
    else:
        tabrep = D("tabrep", [128, 32 * 48], F32, kind="ExternalInput").ap()
        relbase_d = D("relbase", [128, 512], F32, kind="ExternalInput").ap()
        hsel = D("hsel", [1, 2], I32, kind="ExternalInput").ap()
        NBUF = 1
        tab = A("tab", [128, 32 * 48], F32); dtab = A("dtab", [128, 31 * 48], F32)
        relbase = A("relbase_s", [128, 512], F32)
        nrel = A("nrel", [128, 512], F32); acc = A("acc", [128, 512], F32); stp = A("stp", [128, 512], F32)
        mk = [A("mk%d" % i, [128, 512], BF16) for i in range(2)]
        BM = A("BM", [128, 33, 512], BF16)
        qc_s = [A("qc_s%d" % i, [128, 3, 512], BF16) for i in range(2)]
        P.dma('sp', lambda e: e.dma_start(out=tab[:], in_=tabrep), writes=['tab'])
        P.dma('sp', lambda e: e.dma_start(out=relbase[:], in_=relbase_d), writes=['relbase'])
        P.op('dve', lambda e: e.tensor_tensor(out=dtab[:], in0=tab[:, 48:32 * 48], in1=tab[:, 0:31 * 48], op=ALU.subtract), reads=['tab'], writes=['dtab'])
    k_s = [A("k_s%d" % i, [128, NG, S], BF16) for i in range(NBUF)]
    v_s = [A("v_s%d" % i, [128, NG, NB, 128], BF16) for i in range(NBUF)]
    NE = 3
    At = [A("At%d" % i, [128, 512], BF16) for i in range(NE)]
    Am = [A("Am%d" % i, [128, 512], BF16) for i in range(2)]
    osb = [A("osb%d" % i, [128, 512], BF16) for i in range(2)]
    rl = [A("rl%d" % i, [128, 512], F32) for i in range(2)]
    E = [nc.alloc_psum_tensor("E%d" % i, [128, 512], F32) for i in range(NE)]
    O = [nc.alloc_psum_tensor("O%d" % i, [128, 512], F32) for i in range(2)]
    L = [nc.alloc_psum_tensor("L%d" % i, [128, 512], F32) for i in range(2)]
    outkeys = []
    tiles = []
    for h in range(NH):
        for qc in range(NQC):
            lst = []
            if mode == 'mla':
                for b in range(4 * qc + 3, -1, -1):
                    lst.append(dict(g=0, b=b, r=b - 4 * qc, mi=b - 4 * qc))
            else:
                mi0 = 0
                for g, d in enumerate(DILS):
                    for k, dl in enumerate(dil_deltas(d)):
                        b = 4 * qc - dl
                        if b >= 0:
                            lst.append(dict(g=g, b=b, r=0, mi=mi0 + k))
                    mi0 += len(dil_deltas(d))
            for n, t in enumerate(lst):
                t.update(h=h, qc=qc, first=(n == 0), last=(n == len(lst) - 1), ci=h * NQC + qc)
                tiles.append(t)
    thr = t5_thresholds()
    def load_head(h):
        i = h % NBUF
        for g in range(NG):
            P.dma('sp', lambda e: e.dma_start(out=k_s[i][:, g, :], in_=kT[g, h]), writes=[('k', i)])
            P.dma('sp', lambda e: e.dma_start(out=v_s[i][:, g, :, :], in_=v[g, h].rearrange("(b p) d -> p b d", p=128)), writes=[('v', i)])
        if mode == 'mla':
            P.dma('sp', lambda e: e.dma_start(out=q_s[i][:], in_=qT[0, h]), writes=[('q', i)])
            P.dma('sp', lambda e: e.dma_start(out=qr_s[i][:], in_=qrT[h]), writes=[('qr', i)])
        else:
            mi = 0
            for g, d in enumerate(DILS):
                col = g * 16
                for dl in dil_deltas(d):
                    m = mk[mi % 2]
                    P.dma('sp', lambda e: e.dma_start(out=m[:], in_=masks_d[mi]), writes=[('mk', mi % 2)])
                    P.op('dve', lambda e: e.tensor_scalar(out=nrel[:], in0=relbase[:], scalar1=float(dl * 128), scalar2=0.0, op0=ALU.add, op1=ALU.max), reads=['relbase'], writes=['nrel'])
                    for b in range(31):
                        dcol = b * 48 + col + h
                        if b == 0:
                            P.op('dve', lambda e: e.tensor_scalar(out=acc[:], in0=nrel[:], scalar1=float(thr[b]), scalar2=dtab[:, dcol:dcol + 1], op0=ALU.is_ge, op1=ALU.mult), reads=['nrel', 'dtab'], writes=['acc'])
                        else:
                            P.op('dve', lambda e: e.tensor_scalar(out=stp[:], in0=nrel[:], scalar1=float(thr[b]), scalar2=dtab[:, dcol:dcol + 1], op0=ALU.is_ge, op1=ALU.mult), reads=['nrel', 'dtab'], writes=['stp'])
                            P.op('dve', lambda e: e.tensor_tensor(out=acc[:], in0=acc[:], in1=stp[:], op=ALU.add), reads=['stp', 'acc'], writes=['acc'])
                    t0c = col + h
                    P.op('act', lambda e: e.activation(out=acc[:], in_=acc[:], func=AF.Exp, bias=tab[:, t0c:t0c + 1], scale=1.0), reads=['acc', 'tab'], writes=['acc'])
                    P.op('dve', lambda e: e.tensor_tensor(out=BM[:, mi, :], in0=acc[:], in1=m[:], op=ALU.mult), reads=['acc', ('mk', mi % 2)], writes=[('BM', mi)])
                    mi += 1
    loaded = set()
    def ensure(h):
        if h < NH and h not in loaded:
            loaded.add(h); load_head(h)
    ensure(0)
    NT = len(tiles)
    def stageA(i):
        t = tiles[i]; hi = t['h'] % NBUF; ei = i % NE; g = t['g']; b = t['b']; qc = t['qc']
        if mode == 'mla':
            P.op('pe', lambda e: e.matmul(E[ei][:], lhsT=k_s[hi][:, 0, b * 128:(b + 1) * 128], rhs=q_s[hi][:, qc * 512:(qc + 1) * 512], start=True, stop=False),
                 reads=[('q', hi), ('k', hi)], writes=[('E', ei)])
            P.op('pe', lambda e: e.matmul(E[ei][:], lhsT=kr_s[:, b * 128:(b + 1) * 128], rhs=qr_s[hi][:, qc * 512:(qc + 1) * 512], start=False, stop=True),
                 reads=[('qr', hi), 'kr'], writes=[('E', ei)])
            if t['r'] >= 0:
                P.op('act', lambda e: e.activation(out=Am[i % 2][:], in_=E[ei][:], func=AF.Exp), reads=[('E', ei)], writes=[('Am', i % 2)])
                P.op('dve', lambda e: e.tensor_tensor(out=At[ei][:], in0=Am[i % 2][:], in1=masks[:, t['r'], :], op=ALU.mult), reads=[('Am', i % 2), 'masks'], writes=[('At', ei)])
            else:
                P.op('act', lambda e: e.activation(out=At[ei][:], in_=E[ei][:], func=AF.Exp), reads=[('E', ei)], writes=[('At', ei)])
        else:
            ci = t['ci']
            if t['first']:
                P.dma('sp', lambda e: e.dma_start(out=qc_s[ci % 2][:], in_=qT[:, t['h'], :, qc * 512:(qc + 1) * 512].rearrange("g p t -> p g t")), writes=[('qc', ci % 2)])
            P.op('pe', lambda e: e.matmul(E[ei][:], lhsT=k_s[hi][:, g, b * 128:(b + 1) * 128], rhs=qc_s[ci % 2][:, g, :], start=True, stop=True),
                 reads=[('qc', ci % 2), ('k', hi)], writes=[('E', ei)])
            P.op('act', lambda e: e.activation(out=Am[i % 2][:], in_=E[ei][:], func=AF.Exp), reads=[('E', ei)], writes=[('Am', i % 2)])
            P.op('dve', lambda e: e.tensor_tensor(out=At[ei][:], in0=Am[i % 2][:], in1=BM[:, t['mi'], :], op=ALU.mult), reads=[('Am', i % 2), ('BM', t['mi'])], writes=[('At', ei)])
    def stageC(i):
        t = tiles[i]; hi = t['h'] % NBUF; ei = i % NE; oi = t['ci'] % 2; g = t['g']; b = t['b']; qc = t['qc']; h = t['h']
        P.op('pe', lambda e: e.matmul(O[oi][:], lhsT=v_s[hi][:, g, b, :], rhs=At[ei][:], start=t['first'], stop=t['last']),
             reads=[('At', ei), ('v', hi)], writes=[('O', oi)])
        P.op('pe', lambda e: e.matmul(L[oi][:], lhsT=ones[:], rhs=At[ei][:], start=t['first'], stop=t['last']),
             reads=[('At', ei), 'ones'], writes=[('L', oi)])
        if t['last']:
            P.op('dve', lambda e: e.reciprocal(out=rl[oi][:], in_=L[oi][:]), reads=[('L', oi)], writes=[('rl', oi)])
            P.op('dve', lambda e: e.tensor_tensor(out=osb[oi][:], in0=O[oi][:], in1=rl[oi][:], op=ALU.mult), reads=[('O', oi), ('rl', oi)], writes=[('osb', oi)])
            key = ('oT', t['ci'])
            P.dma('sp', lambda e: e.dma_start(out=oT[h, :, qc * 512:(qc + 1) * 512], in_=osb[oi][:]), reads=[('osb', oi)], writes=[key])
            outkeys.append(key)
    pend = None
    for i in range(NT + 1):
        drained = False
        if i < NT:
            t = tiles[i]
            if t['first'] and t['qc'] == 0:
                if NBUF == 1:
                    if i > 0:
                        stageC(i - 1); drained = True
                    ensure(t['h'])
                else:
                    pend = (i + 3, t['h'] + 1)
            stageA(i)
        if pend is not None and i >= pend[0]:
            ensure(pend[1]); pend = None
        if 0 <= i - 1 < NT and not drained: stageC(i - 1)
    P.wait_all('sp', outkeys)
    P.emit()
    return nc, P


from concourse.bass_utils import run_bass_kernel_spmd

NCORES = 8
S_FULL = 8192
TOK = S_FULL // NCORES


def _run(nc, in_maps):
    res = run_bass_kernel_spmd(nc, in_maps, core_ids=list(range(NCORES)))
    return res.results


def _c(a):
    return np.ascontiguousarray(a)


def kernel(x, positions, rel_bias, sb_w_qkv, sb_w_o, mla_w_q_a, mla_q_a_norm, mla_w_q_b, mla_w_kv_a, mla_kv_a_norm,
           mla_w_kv_b, mla_w_o, dil_w_qkv, dil_w_o, ln_gain, ln_bias, moe_w_group_router, moe_b_group_router,
           moe_w_expert_router, moe_b_expert_router, moe_w_gate, moe_w_up, moe_w_down):
    f32 = lambda a: np.asarray(a, dtype=np.float32)
    x = f32(x); S = S_FULL
    positions = np.asarray(positions).astype(np.int32)
    hT = [_c(x[0, c * TOK:(c + 1) * TOK].T) for c in range(NCORES)]
    for li in range(4):
        kind, j = li % 3, li // 3
        if kind == 0 or kind == 2:
            if kind == 0:
                w = f32(sb_w_qkv[j]); nq = 4096; sc = 128 ** -0.5; wo = f32(sb_w_o[j])
            else:
                w = f32(dil_w_qkv[j]); nq = 6144; sc = 128 ** -0.5; wo = f32(dil_w_o[j])
            specs = [('fm', 'w', 3 * nq, 0, nq, sc, 'qT'), ('fm', 'w', 3 * nq, nq, nq, 1.0, 'kT'), ('tm', 'w', 3 * nq, 2 * nq, nq, 1.0, 'v')]
            nc = bass.Bass("TRN2", target_bir_lowering=False)
            nc, _ = build_proj(nc, 4096, specs, T=TOK)
            r = _run(nc, [dict(xT=hT[c], w=w) for c in range(NCORES)])
            qT = np.concatenate([r[c]["qT"] for c in range(NCORES)], axis=1)
            kT = np.concatenate([r[c]["kT"] for c in range(NCORES)], axis=1)
            v = np.concatenate([r[c]["v"] for c in range(NCORES)], axis=0)
            del r
            if kind == 0:
                nc = bass.Bass("TRN2", target_bir_lowering=False)
                nc, _ = build_sb_attn(nc, 4, S)
                cst = sb_consts()
                ims = []
                for c in range(NCORES):
                    d = dict(qT=_c(qT[c * 512:(c + 1) * 512].reshape(4, 128, S)), kT=_c(kT[c * 512:(c + 1) * 512].reshape(4, 128, S)),
                             v=_c(v[:, c * 512:(c + 1) * 512].reshape(S, 4, 128).transpose(1, 0, 2)))
                    d.update(cst); ims.append(d)
                r = _run(nc, ims)
                aT = np.concatenate([r[c]["oT"].reshape(512, S) for c in range(NCORES)], axis=0)
                Fa = 4096
            else:
                nc = bass.Bass("TRN2", target_bir_lowering=False)
                nc, _ = build_attn(nc, 2, S, 'dil')
                cst = attn_consts('dil')
                rb = f32(rel_bias)
                ims = []
                for c in range(NCORES):
                    rows = [g * 2048 + (2 * c + hl) * 128 for g in range(3) for hl in range(2)]
                    qs = np.stack([qT[r0:r0 + 128] for r0 in rows]).reshape(3, 2, 128, S)
                    ks = np.stack([kT[r0:r0 + 128] for r0 in rows]).reshape(3, 2, 128, S)
                    vs = np.stack([v[:, r0:r0 + 128] for r0 in rows]).reshape(3, 2, S, 128)
                    tabc = np.zeros((32, 48), np.float32)
                    for g in range(3):
                        for hl in range(2):
                            tabc[:, g * 16 + hl] = rb[:, g * 16 + 2 * c + hl]
                    d = dict(qT=_c(qs), kT=_c(ks), v=_c(vs), tabrep=_c(np.tile(tabc.reshape(1, -1), (128, 1))), hsel=np.zeros((1, 2), np.int32))
                    d.update(cst); ims.append(d)
                r = _run(nc, ims)
                aT = np.concatenate([r[c]["oT"].reshape(256, S) for c in range(NCORES)], axis=0)
                Fa = 2048
            del qT, kT, v
        else:
            nc = bass.Bass("TRN2", target_bir_lowering=False)
            nc, _ = build_mla_pre(nc, T=TOK)
            cst = mla_consts()
            ims = []
            for c in range(NCORES):
                d = dict(hT=hT[c], wqa=f32(mla_w_q_a[j]), wkva=f32(mla_w_kv_a[j]), qg=_c(f32(mla_q_a_norm[j]).reshape(8, 128).T),
                         kg=_c(f32(mla_kv_a_norm[j]).reshape(4, 128).T), wqb=f32(mla_w_q_b[j]), wkvb=f32(mla_w_kv_b[j]),
                         posr=_c(np.tile(positions[0, c * TOK:(c + 1) * TOK][None, :], (64, 1))))
                d.update(cst); ims.append(d)
            r = _run(nc, ims)
            cat = lambda k, ax: np.concatenate([r[c][k] for c in range(NCORES)], axis=ax)
            qn = cat("qnT", 1); qr = cat("qrT", 1); kn = cat("knT", 1); kr = cat("krT", 1); v = cat("v", 0)
            del r
            nc = bass.Bass("TRN2", target_bir_lowering=False)
            nc, _ = build_attn(nc, 4, S, 'mla')
            cst = attn_consts('mla')
            ims = []
            for c in range(NCORES):
                d = dict(qT=_c(qn[c * 512:(c + 1) * 512].reshape(1, 4, 128, S)), qrT=_c(qr[c * 256:(c + 1) * 256].reshape(4, 64, S)),
                         kT=_c(kn[c * 512:(c + 1) * 512].reshape(1, 4, 128, S)), krT=_c(kr),
                         v=_c(v[:, c * 512:(c + 1) * 512].reshape(S, 4, 128).transpose(1, 0, 2)[None]))
                d.update(cst); ims.append(d)
            r = _run(nc, ims)
            aT = np.concatenate([r[c]["oT"].reshape(512, S) for c in range(NCORES)], axis=0)
            Fa = 4096; wo = f32(mla_w_o[j])
            del qn, qr, kn, kr, v
        nc = bass.Bass("TRN2", target_bir_lowering=False)
        nc, _ = build_post(nc, Fa, T=TOK)
        cst = post_consts()
        brep = np.tile(np.concatenate([f32(moe_b_group_router[li]), f32(moe_b_expert_router[li]).reshape(-1)])[None, :], (128, 1)).astype(np.float32)
        base = dict(wo=wo, lng=ln_layout(f32(ln_gain[li])), lnb=ln_layout(f32(ln_bias[li])),
                    wr=wr_layout(f32(moe_w_group_router[li]), f32(moe_w_expert_router[li])), brep=_c(brep),
                    wg=f32(moe_w_gate[li]), wu=f32(moe_w_up[li]), wd=f32(moe_w_down[li]))
        base.update(cst)
        ims = []
        for c in range(NCORES):
            d = dict(aT=_c(aT[:, c * TOK:(c + 1) * TOK]), hT=hT[c]); d.update(base); ims.append(d)
        r = _run(nc, ims)
        hT = [r[c]["outT"] for c in range(NCORES)]
        del r, aT
    out = np.concatenate([hT[c].T for c in range(NCORES)], axis=0)[None]
    return np.ascontiguousarray(out.astype(np.float32))
```

```python
import sys, math, time
import numpy as np
import concourse.bass as bass
import concourse.mybir as mybir

F32 = mybir.dt.float32
BF16 = mybir.dt.bfloat16
I32 = mybir.dt.int32
AF = mybir.ActivationFunctionType
ALU = mybir.AluOpType
AX = mybir.AxisListType


import types


def _snap(fn):
    if fn is None or fn.__closure__ is None:
        return fn
    cells = tuple(types.CellType(c.cell_contents) for c in fn.__closure__)
    g = types.FunctionType(fn.__code__, fn.__globals__, fn.__name__, fn.__defaults__, cells)
    g.__kwdefaults__ = fn.__kwdefaults__
    return g


class Prog:
    CE = ['pe', 'act', 'dve', 'pool']
    NDS = 24

    def __init__(self, nc):
        self.nc = nc
        self.eng = {'pe': nc.tensor, 'act': nc.scalar, 'dve': nc.vector, 'pool': nc.gpsimd, 'sp': nc.sync}
        self.sem = {e: nc.alloc_semaphore('s_' + e) for e in self.CE}
        self.cnt = {e: 0 for e in self.CE}
        self.dsem = [nc.alloc_semaphore('d%d' % i) for i in range(self.NDS + 8)]
        self.dcnt = [0] * (self.NDS + 8)
        self.dma_i = 0
        self.dma_j = 0
        self.stream = {e: [] for e in self.eng}
        self.lastw = {}
        self.readers = {}
        self.seen = {e: {} for e in self.eng}
        self.semobj = {}
        for e in self.CE:
            self.semobj[self.sem[e].num] = self.sem[e]
        for s in self.dsem:
            self.semobj[s.num] = s
        self.n_ops = 0

    def _need(self, e, events, waits):
        for ev in events:
            if ev is None:
                continue
            s, v = ev
            if e == 'pe' and s == self.sem['pe'].num:
                continue
            if self.seen[e].get(s, 0) >= v:
                continue
            self.seen[e][s] = v
            waits[s] = max(waits.get(s, 0), v)

    def _deps(self, e, reads, writes):
        waits = {}
        for k in reads:
            self._need(e, [self.lastw.get(k)], waits)
        for k in writes:
            self._need(e, [self.lastw.get(k)], waits)
            self._need(e, self.readers.get(k, []), waits)
        return waits

    def _commit(self, ev, reads, writes):
        for k in reads:
            self.readers.setdefault(k, []).append(ev)
        for k in writes:
            self.lastw[k] = ev
            self.readers[k] = []

    def op(self, e, fn, reads=(), writes=()):
        fn = _snap(fn)
        waits = self._deps(e, reads, writes)
        self.cnt[e] += 1
        ev = (self.sem[e].num, self.cnt[e])
        self.stream[e].append((waits, fn, (self.sem[e], 1)))
        self._commit(ev, reads, writes)
        self.n_ops += 1

    def dma(self, q, fn, reads=(), writes=(), slow=False):
        fn = _snap(fn)
        if slow:
            i = self.NDS + self.dma_j % 8
            self.dma_j += 1
        else:
            i = self.dma_i % self.NDS
            self.dma_i += 1
        waits = self._deps(q, reads, writes)
        if self.dcnt[i] > 0:
            self._need(q, [(self.dsem[i].num, self.dcnt[i])], waits)
        self.dcnt[i] += 16
        ev = (self.dsem[i].num, self.dcnt[i])
        self.stream[q].append((waits, fn, (self.dsem[i], 16)))
        self._commit(ev, reads, writes)
        self.n_ops += 1

    def wait_all(self, e, keys):
        waits = {}
        for k in keys:
            self._need(e, [self.lastw.get(k)], waits)
        self.stream[e].append((waits, None, None))

    def emit(self):
        nc = self.nc
        names = {'pe': 'tensor', 'act': 'scalar', 'dve': 'vector', 'pool': 'gpsimd', 'sp': 'sync'}
        with nc.Block() as block:
            for e, lst in self.stream.items():
                if not lst:
                    continue

                def body(engine, lst=lst):
                    for waits, fn, inc in lst:
                        for s, v in waits.items():
                            engine.wait_ge(self.semobj[s], v)
                        if fn is not None:
                            ins = fn(engine)
                            ins.then_inc(inc[0], inc[1])
                getattr(block, names[e])(body)

import ml_dtypes
BF = ml_dtypes.bfloat16
DN_ALPHA = 8 ** 0.25
LN_EPS = 1e-5

def ln_layout(v):
    return np.ascontiguousarray(v.reshape(2, 32, 128).transpose(2, 0, 1))

def wr_layout(wgr, wer):
    w = np.concatenate([wgr] + [wer[g] for g in range(4)], axis=1)
    return np.ascontiguousarray(w.reshape(32, 128, 36).transpose(1, 0, 2))

def post_consts():
    return {"ident": np.eye(128, dtype=np.float32), "onesm": np.full((128, 128), 1.0 / 4096, np.float32)}

class WPool:
    def __init__(self, P, nc, n, nstage=3, cast_engines=('dve', 'pool')):
        self.P = P
        self.bufs = [nc.alloc_sbuf_tensor("wp%d" % i, [128, 16, 256], BF16) for i in range(n)]
        self.stage = [nc.alloc_sbuf_tensor("wst%d" % i, [128, 16, 256], F32) for i in range(nstage)]
        self.i = 0
        self.si = 0
        self.ce = cast_engines
    def load(self, src):
        i = self.i % len(self.bufs); self.i += 1
        si = self.si % len(self.stage); self.si += 1
        b = self.bufs[i]; st = self.stage[si]
        self.P.dma('sp', lambda e: e.dma_start(out=st[:], in_=src), writes=[('wst', si)])
        eng = self.ce[self.si % len(self.ce)]
        self.P.op(eng, lambda e: e.tensor_copy(out=b[:], in_=st[:]), reads=[('wst', si)], writes=[('wp', i)])
        return b, ('wp', i)

class WPoolBF:
    def __init__(self, P, nc, n):
        self.P = P
        self.bufs = [nc.alloc_sbuf_tensor("wq%d" % i, [128, 16, 256], BF16) for i in range(n)]
        self.i = 0
    def load(self, src):
        i = self.i % len(self.bufs); self.i += 1
        b = self.bufs[i]
        self.P.dma('sp', lambda e: e.dma_start(out=b[:], in_=src), writes=[('wq', i)])
        return b, ('wq', i)


def build_post(nc, Fa, T=1024, TP=512, NW=8, mode='std', debug=False):
    KA = Fa // 128
    NP = T // TP
    D = nc.dram_tensor
    if mode == 'dil':
        ogT = D("ogT", [3, Fa, T], BF16, kind="ExternalInput").ap()
        lse = D("lse", [3, Fa // 128, 128, T], F32, kind="ExternalInput").ap()
    else:
        aT = D("aT", [Fa, T], BF16, kind="ExternalInput").ap()
    hT = D("hT", [4096, T], F32, kind="ExternalInput").ap()
    wo = D("wo", [Fa, 4096], BF16, kind="ExternalInput").ap()
    lng = D("lng", [128, 2, 32], F32, kind="ExternalInput").ap()
    lnb = D("lnb", [128, 2, 32], F32, kind="ExternalInput").ap()
    wr_d = D("wr", [128, 32, 36], F32, kind="ExternalInput").ap()
    brep = D("brep", [128, 36], F32, kind="ExternalInput").ap()
    wg = D("wg", [32, 4096, 256], BF16, kind="ExternalInput").ap()
    wu = D("wu", [32, 4096, 256], BF16, kind="ExternalInput").ap()
    wd = D("wd", [32, 256, 4096], BF16, kind="ExternalInput").ap()
    ident_d = D("ident", [128, 128], F32, kind="ExternalInput").ap()
    onesm_d = D("onesm", [128, 128], F32, kind="ExternalInput").ap()
    outT = D("outT", [4096, T], F32, kind="ExternalOutput").ap()
    if debug:
        dbg1 = D("dbg1", [4096, T], F32, kind="ExternalOutput").ap()
        dbg2 = D("dbg2", [T, 32], F32, kind="ExternalOutput").ap()
        dbg3 = D("dbg3", [4096, T], F32, kind="ExternalOutput").ap()
    A = nc.alloc_sbuf_tensor
    hf = A("hf", [128, 32, TP], F32)
    ab = A("ab", [128, 32, TP], BF16)
    act = A("act", [128, 16, TP], BF16)
    Wr = A("Wr", [128, 32, 36], F32)
    lg = A("lg", [128, 2, 32], F32); lb = A("lb", [128, 2, 32], F32)
    ident = A("ident_s", [128, 128], F32); onesm = A("onesm_s", [128, 128], F32)
    brs = A("brs", [128, 36], F32)
    tmp = [A("tmp%d" % i, [128, TP], F32) for i in range(3)]
    rstd = A("rstd", [128, TP], F32)
    comb = [A("comb%d" % i, [128, 32], F32) for i in range(TP // 128)]
    sm = A("sm", [128, 128], F32)
    PS = nc.alloc_psum_tensor
    G = [PS("G%d" % i, [128, TP], F32) for i in range(2)]
    U = [PS("U%d" % i, [128, TP], F32) for i in range(2)]
    CB = PS("CB", [128, TP], F32)
    Y = [PS("Y%d" % i, [128, TP], F32) for i in range(3)]
    P = Prog(nc)
    wp = WPoolBF(P, nc, NW)
    P.dma('sp', lambda e: e.dma_start(out=ident[:], in_=ident_d), writes=['ident'])
    P.dma('sp', lambda e: e.dma_start(out=onesm[:], in_=onesm_d), writes=['onesm'])
    P.dma('sp', lambda e: e.dma_start(out=brs[:], in_=brep), writes=['brs'])
    P.dma('sp', lambda e: e.dma_start(out=lg[:], in_=lng), writes=['lg'])
    P.dma('sp', lambda e: e.dma_start(out=lb[:], in_=lnb), writes=['lb'])
    P.dma('sp', lambda e: e.dma_start(out=Wr[:], in_=wr_d), writes=['Wr'])

    tmpi = [0]
    def gettmp():
        i = tmpi[0] % 3; tmpi[0] += 1
        return tmp[i], ('tmp', i)

    def layer_norm(li, make_bf):
        hk = [('hf', c) for c in range(32)]
        for c in range(32):
            P.op('pe', lambda e, c=c: e.matmul(Y[0][:], lhsT=onesm[:], rhs=hf[:, c, :], start=(c == 0), stop=(c == 31)),
                 reads=[hk[c], 'onesm'], writes=['Y0'])
        for c in range(32):
            P.op('dve', lambda e, c=c: e.tensor_tensor(out=hf[:, c, :], in0=hf[:, c, :], in1=Y[0][:], op=ALU.subtract),
                 reads=['Y0', hk[c]], writes=[hk[c]])
            t, tk = gettmp()
            P.op('act', lambda e, c=c, t=t: e.activation(out=t[:], in_=hf[:, c, :], func=AF.Square), reads=[hk[c]], writes=[tk])
            P.op('pe', lambda e, c=c, t=t: e.matmul(Y[1][:], lhsT=onesm[:], rhs=t[:], start=(c == 0), stop=(c == 31)),
                 reads=[tk, 'onesm'], writes=['Y1'])
        t, tk = gettmp()
        P.op('act', lambda e: e.activation(out=t[:], in_=Y[1][:], func=AF.Sqrt, bias=LN_EPS), reads=['Y1'], writes=[tk])
        P.op('dve', lambda e: e.reciprocal(out=rstd[:], in_=t[:]), reads=[tk], writes=['rstd'])
        for c in range(32):
            P.op('dve', lambda e, c=c: e.tensor_tensor(out=hf[:, c, :], in0=hf[:, c, :], in1=rstd[:], op=ALU.mult),
                 reads=['rstd', hk[c]], writes=[hk[c]])
            P.op('act', lambda e, c=c: e.activation(out=hf[:, c, :], in_=hf[:, c, :], func=AF.Identity,
                                                    scale=lg[:, li, c:c + 1], bias=lb[:, li, c:c + 1]),
                 reads=[hk[c], 'lg', 'lb'], writes=[hk[c]])
            if make_bf:
                P.op('pool', lambda e, c=c: e.tensor_copy(out=ab[:, c, :], in_=hf[:, c, :]), reads=[hk[c]], writes=[('ab', c)])

    outkeys = []
    for tp in range(NP):
        ts = slice(tp * TP, (tp + 1) * TP)
        for c4 in range(0, 32, 8):
            P.dma('sp', lambda e, c4=c4, ts=ts: e.dma_start(out=hf[:, c4:c4 + 8, :], in_=hT[c4 * 128:(c4 + 8) * 128, ts].rearrange("(c p) t -> p c t", p=128)),
                  writes=[('hf', c) for c in range(c4, c4 + 8)])
        if mode == 'dil':
            raise NotImplementedError
        else:
            for c4 in range(0, KA, 8):
                P.dma('sp', lambda e, c4=c4, ts=ts: e.dma_start(out=ab[:, c4:c4 + 8, :], in_=aT[c4 * 128:(c4 + 8) * 128, ts].rearrange("(c p) t -> p c t", p=128)),
                      writes=[('ab', c) for c in range(c4, c4 + 8)])
        NKH = KA // 16
        for npair in range(16):
            n0 = npair * 256
            wts = [wp.load(wo[kh * 2048:(kh + 1) * 2048, n0:n0 + 256].rearrange("(c p) n -> p c n", p=128)) for kh in range(NKH)]
            for f in range(2):
                for kh in range(NKH):
                    wt, wk = wts[kh]
                    for c in range(16):
                        P.op('pe', lambda e, wt=wt, f=f, kh=kh, c=c: e.matmul(Y[f][:], lhsT=wt[:, c, f * 128:(f + 1) * 128], rhs=ab[:, kh * 16 + c, :],
                                                                             start=(kh == 0 and c == 0), stop=(kh == NKH - 1 and c == 15)),
                             reads=[wk, ('ab', kh * 16 + c)], writes=['Y%d' % f])
                j = npair * 2 + f
                P.op('dve', lambda e, j=j, f=f: e.scalar_tensor_tensor(out=hf[:, j, :], in0=hf[:, j, :], scalar=DN_ALPHA, in1=Y[f][:], op0=ALU.mult, op1=ALU.add),
                     reads=['Y%d' % f, ('hf', j)], writes=[('hf', j)])
        if debug:
            P.dma('sp', lambda e, ts=ts: e.dma_start(out=dbg3[:, ts].rearrange("(c p) t -> p c t", p=128), in_=hf[:]), reads=[('hf', c) for c in range(32)], writes=[('dbg3', tp)])
            outkeys.append(('dbg3', tp))
        layer_norm(0, True)
        if debug:
            P.dma('sp', lambda e, ts=ts: e.dma_start(out=dbg1[:, ts].rearrange("(c p) t -> p c t", p=128), in_=hf[:]), reads=[('hf', c) for c in range(32)], writes=[('dbg1', tp)])
            outkeys.append(('dbg1', tp))
        for tb in range(TP // 128):
            for c in range(32):
                P.op('pe', lambda e, c=c, tb=tb: e.matmul(CB[:, 0:36], lhsT=hf[:, c, tb * 128:(tb + 1) * 128], rhs=Wr[:, c, :], start=(c == 0), stop=(c == 31)),
                     reads=[('hf', c), 'Wr'], writes=['CB'])
            Ls = sm[:, 0:36]; gmax = sm[:, 36:37]; ngmax = sm[:, 37:38]; ohg = sm[:, 40:44]; gex = sm[:, 44:48]; gsum = sm[:, 48:49]
            ggate = sm[:, 49:50]; sel = sm[:, 52:60]; m1 = sm[:, 60:61]; oh1 = sm[:, 64:72]; sel2 = sm[:, 72:80]; m2 = sm[:, 80:81]
            oh2 = sm[:, 84:92]; dd = sm[:, 92:93]; e2 = sm[:, 93:94]; den = sm[:, 94:95]; w1 = sm[:, 95:96]; w2 = sm[:, 96:97]
            within = sm[:, 100:108]
            cb = comb[tb]
            def dv(fn, r=('sm',), w=('sm',)):
                P.op('dve', fn, reads=list(r), writes=list(w))
            dv(lambda e: e.tensor_tensor(out=Ls, in0=CB[:, 0:36], in1=brs[:], op=ALU.add), r=('CB', 'brs', 'sm'))
            dv(lambda e: e.reduce_max(out=gmax, in_=Ls[:, 0:4], axis=AX.X))
            dv(lambda e: e.tensor_scalar(out=ohg, in0=Ls[:, 0:4], scalar1=gmax, scalar2=None, op0=ALU.is_equal))
            dv(lambda e: e.tensor_scalar(out=ngmax, in0=gmax, scalar1=-1.0, scalar2=None, op0=ALU.mult))
            P.op('act', lambda e: e.activation(out=gex, in_=Ls[:, 0:4], func=AF.Exp, bias=ngmax, scale=1.0, accum_out=gsum), reads=['sm'], writes=['sm'])
            dv(lambda e: e.reciprocal(out=ggate, in_=gsum))
            dv(lambda e: e.tensor_scalar(out=sel, in0=Ls[:, 4:12], scalar1=ohg[:, 0:1], scalar2=None, op0=ALU.mult))
            for g in range(1, 4):
                dv(lambda e, g=g: e.scalar_tensor_tensor(out=sel, in0=Ls[:, 4 + 8 * g:12 + 8 * g], scalar=ohg[:, g:g + 1], in1=sel, op0=ALU.mult, op1=ALU.add))
            dv(lambda e: e.reduce_max(out=m1, in_=sel, axis=AX.X))
            dv(lambda e: e.tensor_scalar(out=oh1, in0=sel, scalar1=m1, scalar2=None, op0=ALU.is_equal))
            dv(lambda e: e.scalar_tensor_tensor(out=sel2, in0=oh1, scalar=-1e30, in1=sel, op0=ALU.mult, op1=ALU.add))
            dv(lambda e: e.reduce_max(out=m2, in_=sel2, axis=AX.X))
            dv(lambda e: e.tensor_scalar(out=oh2, in0=sel2, scalar1=m2, scalar2=None, op0=ALU.is_equal))
            dv(lambda e: e.tensor_tensor(out=dd, in0=m2, in1=m1, op=ALU.subtract))
            P.op('act', lambda e: e.activation(out=e2, in_=dd, func=AF.Exp), reads=['sm'], writes=['sm'])
            dv(lambda e: e.tensor_scalar(out=den, in0=e2, scalar1=1.0, scalar2=None, op0=ALU.add))
            dv(lambda e: e.reciprocal(out=w1, in_=den))
            dv(lambda e: e.tensor_tensor(out=w2, in0=e2, in1=w1, op=ALU.mult))
            dv(lambda e: e.tensor_tensor(out=w1, in0=w1, in1=ggate, op=ALU.mult))
            dv(lambda e: e.tensor_tensor(out=w2, in0=w2, in1=ggate, op=ALU.mult))
            dv(lambda e: e.tensor_scalar(out=within, in0=oh1, scalar1=w1, scalar2=None, op0=ALU.mult))
            dv(lambda e: e.scalar_tensor_tensor(out=within, in0=oh2, scalar=w2, in1=within, op0=ALU.mult, op1=ALU.add))
            for g in range(4):
                dv(lambda e, g=g, cb=cb: e.tensor_scalar(out=cb[:, 8 * g:8 * g + 8], in0=within, scalar1=ohg[:, g:g + 1], scalar2=None, op0=ALU.mult),
                   r=('sm',), w=(('comb', tb),))
        if debug:
            for tb in range(TP // 128):
                P.dma('sp', lambda e, tb=tb, tp=tp: e.dma_start(out=dbg2[tp * TP + tb * 128: tp * TP + (tb + 1) * 128, :], in_=comb[tb][:]), reads=[('comb', tb)], writes=[('dbg2', tp, tb)])
                outkeys.append(('dbg2', tp, tb))
        gi = 0
        for qd in range(4):
            for el in range(8):
                ex = qd * 8 + el
                wgt = [wp.load(wg[ex, kh * 2048:(kh + 1) * 2048, :].rearrange("(c p) n -> p c n", p=128)) for kh in range(2)]
                wut = [wp.load(wu[ex, kh * 2048:(kh + 1) * 2048, :].rearrange("(c p) n -> p c n", p=128)) for kh in range(2)]
                for tb in range(TP // 128):
                    P.op('pe', lambda e, tb=tb, ex=ex: e.matmul(CB[:, tb * 128:(tb + 1) * 128], lhsT=comb[tb][:, ex:ex + 1].broadcast_to([128, 128]), rhs=ident[:], start=True, stop=True),
                         reads=[('comb', tb), 'ident'], writes=['CB'])
                for f in range(2):
                    gb = gi % 2; gi += 1
                    for (bank, bk, wts_) in ((G[gb], 'G%d' % gb, wgt), (U[gb], 'U%d' % gb, wut)):
                        for kh in range(2):
                            wt, wk = wts_[kh]
                            for c in range(16):
                                P.op('pe', lambda e, bank=bank, wt=wt, f=f, kh=kh, c=c: e.matmul(bank[:], lhsT=wt[:, c, f * 128:(f + 1) * 128], rhs=ab[:, kh * 16 + c, :],
                                                                                               start=(kh == 0 and c == 0), stop=(kh == 1 and c == 15)),
                                     reads=[wk, ('ab', kh * 16 + c)], writes=[bk])
                    t, tk = gettmp()
                    P.op('act', lambda e, t=t, gb=gb: e.activation(out=t[:], in_=G[gb][:], func=AF.Silu), reads=['G%d' % gb], writes=[tk])
                    P.op('dve', lambda e, t=t, gb=gb: e.tensor_tensor(out=t[:], in0=t[:], in1=U[gb][:], op=ALU.mult), reads=['U%d' % gb, tk], writes=[tk])
                    ai = el * 2 + f
                    P.op('dve', lambda e, t=t, ai=ai: e.tensor_tensor(out=act[:, ai, :], in0=t[:], in1=CB[:], op=ALU.mult), reads=['CB', tk], writes=[('act', ai)])
            for npair in range(16):
                n0 = npair * 256
                wt, wk = wp.load(wd[qd * 8:(qd + 1) * 8, :, n0:n0 + 256].rearrange("e (fh p) n -> p (e fh) n", p=128))
                for f in range(2):
                    j = npair * 2 + f
                    yb = j % 3
                    for c in range(16):
                        P.op('pe', lambda e, wt=wt, f=f, c=c, yb=yb: e.matmul(Y[yb][:], lhsT=wt[:, c, f * 128:(f + 1) * 128], rhs=act[:, c, :], start=(c == 0), stop=(c == 15)),
                             reads=[wk, ('act', c)], writes=['Y%d' % yb])
                    if qd == 0:
                        P.op('dve', lambda e, j=j, yb=yb: e.scalar_tensor_tensor(out=hf[:, j, :], in0=hf[:, j, :], scalar=DN_ALPHA, in1=Y[yb][:], op0=ALU.mult, op1=ALU.add),
                             reads=['Y%d' % yb, ('hf', j)], writes=[('hf', j)])
                    else:
                        P.op('dve', lambda e, j=j, yb=yb: e.tensor_tensor(out=hf[:, j, :], in0=hf[:, j, :], in1=Y[yb][:], op=ALU.add),
                             reads=['Y%d' % yb, ('hf', j)], writes=[('hf', j)])
        layer_norm(1, False)
        for c4 in range(0, 32, 8):
            P.dma('sp', lambda e, c4=c4, ts=ts: e.dma_start(out=outT[c4 * 128:(c4 + 8) * 128, ts].rearrange("(c p) t -> p c t", p=128), in_=hf[:, c4:c4 + 8, :]),
                  reads=[('hf', c) for c in range(c4, c4 + 8)], writes=[('outT', tp, c4)])
            outkeys.append(('outT', tp, c4))
    P.wait_all('sp', outkeys)
    P.emit()
    return nc, P


def build_proj(nc, K, specs, T=1024, x_dtype=F32):
    KC = K // 128; NKH = K // 2048
    D = nc.dram_tensor
    xT = D("xT", [K, T], x_dtype, kind="ExternalInput").ap()
    wd = {}
    outs = {}
    for (kind, wname, wcols, col0, ncols, scale, oname) in specs:
        if wname not in wd:
            wd[wname] = D(wname, [K, wcols], F32, kind="ExternalInput").ap()
        outs[oname] = D(oname, [ncols, T] if kind == 'fm' else [T, ncols], BF16, kind="ExternalOutput").ap()
    A = nc.alloc_sbuf_tensor
    xb = A("xb", [128, KC, T], BF16)
    ob = [A("ob%d" % i, [128, 512], BF16) for i in range(4)]
    PS = [nc.alloc_psum_tensor("ps%d" % i, [128, 512], F32) for i in range(8)]
    P = Prog(nc)
    wp = WPool(P, nc, 6, nstage=3, cast_engines=('pool', 'dve'))
    for c4 in range(0, KC, 8):
        P.dma('pool', lambda e, c4=c4: e.dma_start(out=xb[:, c4:c4 + 8, :], in_=xT[c4 * 128:(c4 + 8) * 128, :].rearrange("(c p) t -> p c t", p=128)),
              writes=[('xb', c) for c in range(c4, c4 + 8)])
    outkeys = []
    pi = [0]; oi = [0]
    def evac(bank, bk, scale, dst, key):
        i = oi[0] % 4; oi[0] += 1
        o = ob[i]
        n = dst.shape[-1]
        P.op('act', lambda e: e.activation(out=o[:, 0:n], in_=bank, func=AF.Identity, scale=float(scale)), reads=[bk], writes=[('ob', i)])
        P.dma('act', lambda e: e.dma_start(out=dst, in_=o[:, 0:n]), reads=[('ob', i)], writes=[key])
        outkeys.append(key)
    for (kind, wname, wcols, col0, ncols, scale, oname) in specs:
        W = wd[wname]; O = outs[oname]
        for n0 in range(0, ncols, 256):
            nw = min(256, ncols - n0)
            wts = [wp.load(W[kh * 2048:(kh + 1) * 2048, col0 + n0:col0 + n0 + 256].rearrange("(c p) n -> p c n", p=128)) for kh in range(NKH)]
            if kind == 'fm':
                for f in range(0, nw, 128):
                    for th in range(T // 512):
                        b = pi[0] % 8; pi[0] += 1
                        for kh in range(NKH):
                            wt, wk = wts[kh]
                            for c in range(16):
                                P.op('pe', lambda e, b=b, wt=wt, f=f, kh=kh, c=c, th=th: e.matmul(PS[b][:], lhsT=wt[:, c, f:f + 128], rhs=xb[:, kh * 16 + c, th * 512:(th + 1) * 512],
                                                                                             start=(kh == 0 and c == 0), stop=(kh == NKH - 1 and c == 15)),
                                     reads=[wk, ('xb', kh * 16 + c)], writes=[('ps', b)])
                        evac(PS[b][:], ('ps', b), scale, O[n0 + f:n0 + f + 128, th * 512:(th + 1) * 512], (oname, n0, f, th))
            else:
                for tb in range(T // 128):
                    b = pi[0] % 8; pi[0] += 1
                    for kh in range(NKH):
                        wt, wk = wts[kh]
                        for c in range(16):
                            P.op('pe', lambda e, b=b, wt=wt, kh=kh, c=c, tb=tb: e.matmul(PS[b][:, 0:256], lhsT=xb[:, kh * 16 + c, tb * 128:(tb + 1) * 128], rhs=wt[:, c, :],
                                                                                     start=(kh == 0 and c == 0), stop=(kh == NKH - 1 and c == 15)),
                                 reads=[wk, ('xb', kh * 16 + c)], writes=[('ps', b)])
                    evac(PS[b][:, 0:nw], ('ps', b), scale, O[tb * 128:(tb + 1) * 128, n0:n0 + nw], (oname, n0, tb))
    P.wait_all('act', outkeys)
    P.emit()
    return nc, P

import ml_dtypes
BF = ml_dtypes.bfloat16

def sb_consts():
    j = np.arange(128)[:, None]; s = np.arange(128)[None, :]
    uneg = np.where(j >= s, -1.0, 0.0).astype(BF)
    oneg = np.full((128, 128), -1.0).astype(BF)
    t = np.arange(512)[None, :]
    masks = np.stack([((128 * r + j) < t) for r in range(4)]).astype(np.float32).astype(BF)
    return {"uneg": uneg, "oneg": oneg, "masks": masks}

def build_sb_attn(nc, NH, S, casts=()):
    NB = S // 128; NQC = S // 512
    qT = nc.dram_tensor("qT", [NH, 128, S], BF16, kind="ExternalInput").ap()
    kT = nc.dram_tensor("kT", [NH, 128, S], BF16, kind="ExternalInput").ap()
    v = nc.dram_tensor("v", [NH, S, 128], BF16, kind="ExternalInput").ap()
    uneg_d = nc.dram_tensor("uneg", [128, 128], BF16, kind="ExternalInput").ap()
    oneg_d = nc.dram_tensor("oneg", [128, 128], BF16, kind="ExternalInput").ap()
    masks_d = nc.dram_tensor("masks", [4, 128, 512], BF16, kind="ExternalInput").ap()
    oT = nc.dram_tensor("oT", [NH, 128, S], BF16, kind="ExternalOutput").ap()
    A = nc.alloc_sbuf_tensor
    uneg = A("uneg_s", [128, 128], BF16); oneg = A("oneg_s", [128, 128], BF16)
    masks = A("masks_s", [128, 4, 512], BF16)
    q_s = [A("q_s%d" % i, [128, S], BF16) for i in range(2)]
    k_s = [A("k_s%d" % i, [128, S], BF16) for i in range(2)]
    v_s = [A("v_s%d" % i, [128, NB, 128], BF16) for i in range(2)]
    NE = 3
    ez = [A("ez%d" % i, [128, 512], F32) for i in range(2)]
    Pt = [A("Pt%d" % i, [128, 512], BF16) for i in range(NE)]
    Pm = [A("Pm%d" % i, [128, 512], BF16) for i in range(2)]
    At = [A("At%d" % i, [128, 512], BF16) for i in range(NE)]
    Ps = [A("Ps%d" % i, [128, 512], BF16) for i in range(NE)]
    osb = [A("osb%d" % i, [128, 512], BF16) for i in range(2)]
    E = [nc.alloc_psum_tensor("E%d" % i, [128, 512], F32) for i in range(NE)]
    O = [nc.alloc_psum_tensor("O%d" % i, [128, 512], F32) for i in range(2)]
    P = Prog(nc)
    P.dma('sp', lambda e: e.dma_start(out=uneg[:], in_=uneg_d), writes=['uneg'])
    P.dma('sp', lambda e: e.dma_start(out=oneg[:], in_=oneg_d), writes=['oneg'])
    P.dma('sp', lambda e: e.dma_start(out=masks[:], in_=masks_d.rearrange("r p t -> p r t")), writes=['masks'])
    tiles = []
    for h in range(NH):
        for qc in range(NQC):
            bs = list(range(4 * qc + 3, -1, -1))
            for n, b in enumerate(bs):
                tiles.append(dict(h=h, qc=qc, b=b, first=(n == 0), last=(n == len(bs) - 1), r=b - 4 * qc,
                                  ci=h * NQC + qc))
    loaded = set()
    outkeys = []
    emit_casts(nc, P, casts, outkeys)
    def load_head(h):
        if h in loaded or h >= NH: return
        loaded.add(h)
        i = h % 2
        P.dma('sp', lambda e: e.dma_start(out=q_s[i][:], in_=qT[h]), writes=[('q', i)])
        P.dma('sp', lambda e: e.dma_start(out=k_s[i][:], in_=kT[h]), writes=[('k', i)])
        P.dma('sp', lambda e: e.dma_start(out=v_s[i][:], in_=v[h].rearrange("(b p) d -> p b d", p=128)), writes=[('v', i)])
    load_head(0)
    NT = len(tiles)
    def stageA(i):
        t = tiles[i]; hi = t['h'] % 2; ei = i % NE
        P.op('pe', lambda e: e.matmul(E[ei][:], lhsT=k_s[hi][:, t['b'] * 128:(t['b'] + 1) * 128],
                                      rhs=q_s[hi][:, t['qc'] * 512:(t['qc'] + 1) * 512], start=True, stop=False),
             reads=[('q', hi), ('k', hi)], writes=[('E', ei)])
        P.op('act', lambda e: e.activation(out=ez[i % 2][:], in_=E[ei][:], func=AF.Exp),
             reads=[('E', ei)], writes=[('ez', i % 2)])
        if t['r'] >= 0:
            P.op('act', lambda e: e.activation(out=Pm[i % 2][:], in_=ez[i % 2][:], func=AF.Ln, bias=1.0),
                 reads=[('ez', i % 2)], writes=[('Pm', i % 2)])
            P.op('dve', lambda e: e.tensor_tensor(out=Pt[ei][:], in0=Pm[i % 2][:], in1=masks[:, t['r'], :], op=ALU.mult),
                 reads=[('Pm', i % 2), 'masks'], writes=[('Pt', ei)])
        else:
            P.op('act', lambda e: e.activation(out=Pt[ei][:], in_=ez[i % 2][:], func=AF.Ln, bias=1.0),
                 reads=[('ez', i % 2)], writes=[('Pt', ei)])
    def stageB(i):
        t = tiles[i]; ei = i % NE
        P.op('pe', lambda e: e.matmul(E[ei][:], lhsT=uneg[:], rhs=Pt[ei][:], start=False, stop=t['first']),
             reads=[('Pt', ei), 'uneg'], writes=[('E', ei)])
        if not t['first']:
            P.op('pe', lambda e: e.matmul(E[ei][:], lhsT=oneg[:], rhs=Ps[ei][:], start=False, stop=True),
                 reads=[('Ps', ei), 'oneg'], writes=[('E', ei)])
        if not t['last']:
            ni = (i + 1) % NE
            if t['first']:
                P.op('pool', lambda e: e.tensor_copy(out=Ps[ni][:], in_=Pt[ei][:]), reads=[('Pt', ei)], writes=[('Ps', ni)])
            else:
                P.op('pool', lambda e: e.tensor_tensor(out=Ps[ni][:], in0=Ps[ei][:], in1=Pt[ei][:], op=ALU.add),
                     reads=[('Pt', ei), ('Ps', ei)], writes=[('Ps', ni)])
        if t['r'] >= 0:
            P.op('act', lambda e: e.activation(out=Pm[i % 2][:], in_=E[ei][:], func=AF.Exp),
                 reads=[('E', ei)], writes=[('Pm', i % 2)])
            P.op('dve', lambda e: e.tensor_tensor(out=At[ei][:], in0=Pm[i % 2][:], in1=masks[:, t['r'], :], op=ALU.mult),
                 reads=[('Pm', i % 2), 'masks'], writes=[('At', ei)])
        else:
            P.op('act', lambda e: e.activation(out=At[ei][:], in_=E[ei][:], func=AF.Exp),
                 reads=[('E', ei)], writes=[('At', ei)])
    def stageC(i):
        t = tiles[i]; hi = t['h'] % 2; ei = i % NE; oi = t['ci'] % 2
        P.op('pe', lambda e: e.matmul(O[oi][:], lhsT=v_s[hi][:, t['b'], :], rhs=At[ei][:], start=t['first'], stop=t['last']),
             reads=[('At', ei), ('v', hi)], writes=[('O', oi)])
        if t['last']:
            P.op('dve', lambda e: e.tensor_copy(out=osb[oi][:], in_=O[oi][:]), reads=[('O', oi)], writes=[('osb', oi)])
            P.dma('sp', lambda e: e.dma_start(out=oT[t['h'], :, t['qc'] * 512:(t['qc'] + 1) * 512], in_=osb[oi][:]),
                  reads=[('osb', oi)], writes=[('oT', t['ci'])])
            outkeys.append(('oT', t['ci']))
    pend = None
    for i in range(NT + 2):
        if i < NT:
            if tiles[i]['first'] and tiles[i]['qc'] == 0:
                pend = (i + 4, tiles[i]['h'] + 1)
            stageA(i)
        if pend is not None and i >= pend[0]:
            load_head(pend[1]); pend = None
        if 0 <= i - 1 < NT: stageB(i - 1)
        if 0 <= i - 2 < NT: stageC(i - 2)
    P.wait_all('sp', outkeys)
    P.emit()
    return nc, P


import ml_dtypes
BF = ml_dtypes.bfloat16
RMS_EPS = 1e-6

def mla_consts():
    half = 32
    inv = (10000.0 ** (-np.arange(half, dtype=np.float32) / half)).astype(np.float32)
    R = np.zeros((64, 64), np.float32)
    for m in range(32):
        R[m + 32, m] = -1.0
        R[m, m + 32] = 1.0
    return {"invf": np.concatenate([inv, inv])[:, None].astype(np.float32), "rotm": R, "ones1": np.ones((128, 128), np.float32)}

def build_mla_pre(nc, T=1024):
    D = nc.dram_tensor
    hT = D("hT", [4096, T], F32, kind="ExternalInput").ap()
    wqa = D("wqa", [4096, 1024], F32, kind="ExternalInput").ap()
    wkva = D("wkva", [4096, 576], F32, kind="ExternalInput").ap()
    qg = D("qg", [128, 8], F32, kind="ExternalInput").ap()
    kg = D("kg", [128, 4], F32, kind="ExternalInput").ap()
    wqb = D("wqb", [1024, 6144], F32, kind="ExternalInput").ap()
    wkvb = D("wkvb", [512, 8192], F32, kind="ExternalInput").ap()
    posr = D("posr", [64, T], I32, kind="ExternalInput").ap()
    invf_d = D("invf", [64, 1], F32, kind="ExternalInput").ap()
    rotm_d = D("rotm", [64, 64], F32, kind="ExternalInput").ap()
    ones_d = D("ones1", [128, 128], F32, kind="ExternalInput").ap()
    qnT = D("qnT", [4096, T], BF16, kind="ExternalOutput").ap()
    qrT = D("qrT", [2048, T], BF16, kind="ExternalOutput").ap()
    knT = D("knT", [4096, T], BF16, kind="ExternalOutput").ap()
    krT = D("krT", [64, T], BF16, kind="ExternalOutput").ap()
    vo = D("v", [T, 4096], BF16, kind="ExternalOutput").ap()
    A = nc.alloc_sbuf_tensor
    xb = A("xb", [128, 32, T], BF16)
    cqf = A("cqf", [128, 8, T], F32)
    kvf = A("kvf", [128, 5, T], F32)
    cqb = A("cqb", [128, 8, T], BF16)
    ckb = A("ckb", [128, 4, T], BF16)
    cos2 = A("cos2", [64, T], F32); sin2 = A("sin2", [64, T], F32)
    ang = A("ang", [64, T], F32); kf = A("kf", [64, T], F32); pi_ = A("pi_", [64, T], I32); ki = pi_
    invf = A("invf_s", [64, 1], F32); rotm = A("rotm_s", [64, 64], F32); ones1 = A("ones_s", [128, 128], F32)
    qgs = A("qgs", [128, 8], F32); kgs = A("kgs", [128, 4], F32)
    wbuf = [A("wb%d" % i, [128, 16, 256], BF16) for i in range(3)]
    wrot = [A("wrot%d" % i, [128, 8, 64], BF16) for i in range(2)]
    tmp = [A("tmp%d" % i, [128, 512], F32) for i in range(4)]
    ob = [A("ob%d" % i, [128, 512], BF16) for i in range(4)]
    rstd = A("rstd", [128, 512], F32)
    PS = [nc.alloc_psum_tensor("ps%d" % i, [128, 512], F32) for i in range(8)]
    P = Prog(nc)
    outkeys = []
    cnt = dict(w=0, p=0, t=0, o=0)
    def getw():
        i = cnt['w'] % 3; cnt['w'] += 1; return wbuf[i], ('wb', i)
    def getp():
        i = cnt['p'] % 8; cnt['p'] += 1; return PS[i], ('ps', i)
    def gett():
        i = cnt['t'] % 4; cnt['t'] += 1; return tmp[i], ('tmp', i)
    def geto():
        i = cnt['o'] % 4; cnt['o'] += 1; return ob[i], ('ob', i)
    def store(o, ok, dst, key, n=512, rows=128):
        P.dma('sp', lambda e: e.dma_start(out=dst, in_=o[0:rows, 0:n]), reads=[ok], writes=[key]); outkeys.append(key)
    for c4 in range(0, 32, 8):
        P.dma('pool', lambda e, c4=c4: e.dma_start(out=xb[:, c4:c4 + 8, :], in_=hT[c4 * 128:(c4 + 8) * 128, :].rearrange("(c p) t -> p c t", p=128)),
              writes=[('xb', c) for c in range(c4, c4 + 8)])
    for (dst, src, k) in ((invf, invf_d, 'invf'), (rotm, rotm_d, 'rotm'), (ones1, ones_d, 'ones1'), (qgs, qg, 'qgs'), (kgs, kg, 'kgs'), (pi_, posr, 'pi_')):
        P.dma('sp', lambda e, dst=dst, src=src: e.dma_start(out=dst[:], in_=src), writes=[k])
    TWO_PI = 2 * math.pi
    P.op('dve', lambda e: e.tensor_copy(out=ang[:], in_=pi_[:]), reads=['pi_'], writes=['ang'])
    P.op('dve', lambda e: e.tensor_scalar(out=ang[:], in0=ang[:], scalar1=invf[:, 0:1], scalar2=None, op0=ALU.mult), reads=['ang', 'invf'], writes=['ang'])
    def sin_tab(dst, key, shift):
        P.op('dve', lambda e: e.tensor_scalar(out=kf[:], in0=ang[:], scalar1=shift, scalar2=1.0 / TWO_PI, op0=ALU.add, op1=ALU.mult), reads=['ang'], writes=['kf'])
        P.op('dve', lambda e: e.tensor_copy(out=ki[:], in_=kf[:]), reads=['kf', 'ang'], writes=['pi_'])
        P.op('dve', lambda e: e.tensor_copy(out=kf[:], in_=ki[:]), reads=['pi_'], writes=['kf'])
        P.op('dve', lambda e: e.tensor_scalar(out=dst[:], in0=ang[:], scalar1=shift, scalar2=None, op0=ALU.add), reads=['ang'], writes=[key])
        P.op('dve', lambda e: e.scalar_tensor_tensor(out=dst[:], in0=kf[:], scalar=-TWO_PI, in1=dst[:], op0=ALU.mult, op1=ALU.add), reads=['kf', key], writes=[key])
        P.op('dve', lambda e: e.tensor_scalar(out=kf[:], in0=dst[:], scalar1=math.pi, scalar2=-TWO_PI, op0=ALU.is_gt, op1=ALU.mult), reads=[key], writes=['kf'])
        P.op('dve', lambda e: e.tensor_tensor(out=dst[:], in0=dst[:], in1=kf[:], op=ALU.add), reads=['kf', key], writes=[key])
        P.op('dve', lambda e: e.tensor_scalar(out=kf[:], in0=dst[:], scalar1=-math.pi, scalar2=TWO_PI, op0=ALU.is_lt, op1=ALU.mult), reads=[key], writes=['kf'])
        P.op('dve', lambda e: e.tensor_tensor(out=dst[:], in0=dst[:], in1=kf[:], op=ALU.add), reads=['kf', key], writes=[key])
        P.op('dve', lambda e: e.tensor_scalar(out=dst[:], in0=dst[:], scalar1=-math.pi, scalar2=math.pi, op0=ALU.max, op1=ALU.min), reads=[key], writes=[key])
        P.op('act', lambda e: e.activation(out=dst[:], in_=dst[:], func=AF.Sin), reads=[key], writes=[key])
    sin_tab(sin2, 'sin2', 0.0)
    sin_tab(cos2, 'cos2', 0.5 * math.pi)
    def proj1(W, ncols, dstf, dkey):
        for n0 in range(0, ncols, 256):
            nw = min(256, ncols - n0)
            wts = []
            for kh in range(2):
                wt, wk = getw()
                P.dma('pool', lambda e, wt=wt, kh=kh, n0=n0, nw=nw: e.dma_start(out=wt[:, :, 0:nw], in_=W[kh * 2048:(kh + 1) * 2048, n0:n0 + nw].rearrange("(c p) n -> p c n", p=128)), writes=[wk])
                wts.append((wt, wk))
            for f in range(0, nw, 128):
                m = min(128, nw - f)
                j = (n0 + f) // 128
                for th in range(T // 512):
                    ps, pk = getp()
                    for kh in range(2):
                        wt, wk = wts[kh]
                        for c in range(16):
                            P.op('pe', lambda e, ps=ps, wt=wt, f=f, m=m, kh=kh, c=c, th=th: e.matmul(ps[0:m, :], lhsT=wt[:, c, f:f + m], rhs=xb[:, kh * 16 + c, th * 512:(th + 1) * 512],
                                                                                              start=(kh == 0 and c == 0), stop=(kh == 1 and c == 15)),
                                 reads=[wk, ('xb', kh * 16 + c)], writes=[pk])
                    P.op('act', lambda e, ps=ps, m=m, j=j, th=th: e.activation(out=dstf[0:m, j, th * 512:(th + 1) * 512], in_=ps[0:m, :], func=AF.Identity),
                         reads=[pk], writes=[(dkey, j, th)])
    proj1(wqa, 1024, cqf, 'cqf')
    proj1(wkva, 576, kvf, 'kvf')
    def rms(srcf, skey, nch, gs, gkey, dstb, dkey):
        for th in range(T // 512):
            tsl = slice(th * 512, (th + 1) * 512)
            ps, pk = getp()
            for c in range(nch):
                t, tk = gett()
                P.op('act', lambda e, t=t, c=c: e.activation(out=t[:], in_=srcf[:, c, tsl], func=AF.Square), reads=[(skey, c, th)], writes=[tk])
                P.op('pe', lambda e, t=t, ps=ps, c=c: e.matmul(ps[:], lhsT=ones1[:], rhs=t[:], start=(c == 0), stop=(c == nch - 1)), reads=[tk, 'ones1'], writes=[pk])
            t, tk = gett()
            P.op('act', lambda e, t=t, ps=ps: e.activation(out=t[:], in_=ps[:], func=AF.Sqrt, scale=1.0 / (nch * 128), bias=RMS_EPS), reads=[pk], writes=[tk])
            P.op('dve', lambda e, t=t: e.reciprocal(out=rstd[:], in_=t[:]), reads=[tk], writes=['rstd'])
            for c in range(nch):
                t, tk = gett()
                P.op('dve', lambda e, t=t, c=c: e.tensor_tensor(out=t[:], in0=srcf[:, c, tsl], in1=rstd[:], op=ALU.mult), reads=[(skey, c, th), 'rstd'], writes=[tk])
                P.op('act', lambda e, t=t, c=c: e.activation(out=dstb[:, c, tsl], in_=t[:], func=AF.Identity, scale=gs[:, c:c + 1]), reads=[tk, gkey], writes=[(dkey, c, th)])
    rms(cqf, 'cqf', 8, qgs, 'qgs', cqb, 'cqb')
    rms(kvf, 'kvf', 4, kgs, 'kgs', ckb, 'ckb')
    QS = 192 ** -0.5
    for th in range(T // 512):
        tsl = slice(th * 512, (th + 1) * 512)
        ps, pk = getp()
        P.op('pe', lambda e, ps=ps: e.matmul(ps[0:64, :], lhsT=rotm[:], rhs=kvf[0:64, 4, tsl], start=True, stop=True), reads=[('kvf', 4, th), 'rotm'], writes=[pk])
        t1, k1 = gett(); t2, k2 = gett(); o, ok = geto()
        P.op('dve', lambda e, t1=t1: e.tensor_tensor(out=t1[0:64, :], in0=kvf[0:64, 4, tsl], in1=cos2[:, tsl], op=ALU.mult), reads=[('kvf', 4, th), 'cos2'], writes=[k1])
        P.op('dve', lambda e, t2=t2, ps=ps: e.tensor_tensor(out=t2[0:64, :], in0=ps[0:64, :], in1=sin2[:, tsl], op=ALU.mult), reads=[pk, 'sin2'], writes=[k2])
        P.op('dve', lambda e, t1=t1, t2=t2, o=o: e.tensor_tensor(out=o[0:64, :], in0=t1[0:64, :], in1=t2[0:64, :], op=ALU.add), reads=[k1, k2], writes=[ok])
        store(o, ok, krT[:, tsl], ('krT', th), rows=64)
    for h in range(32):
        wt, wk = getw()
        P.dma('pool', lambda e, wt=wt, h=h: e.dma_start(out=wt[:, 0:8, 0:192], in_=wqb[:, h * 192:(h + 1) * 192].rearrange("(c p) n -> p c n", p=128)), writes=[wk])
        wr = wrot[h % 2]; wrk = ('wrot', h % 2)
        P.op('act', lambda e, wt=wt, wr=wr: e.activation(out=wr[:, :, 0:32], in_=wt[:, 0:8, 160:192], func=AF.Identity, scale=-1.0), reads=[wk], writes=[wrk])
        P.op('act', lambda e, wt=wt, wr=wr: e.activation(out=wr[:, :, 32:64], in_=wt[:, 0:8, 128:160], func=AF.Identity), reads=[wk, wrk], writes=[wrk])
        for th in range(T // 512):
            tsl = slice(th * 512, (th + 1) * 512)
            ps, pk = getp()
            for c in range(8):
                P.op('pe', lambda e, ps=ps, wt=wt, c=c: e.matmul(ps[:], lhsT=wt[:, c, 0:128], rhs=cqb[:, c, tsl], start=(c == 0), stop=(c == 7)), reads=[wk, ('cqb', c, th)], writes=[pk])
            o, ok = geto()
            P.op('act', lambda e, o=o, ps=ps: e.activation(out=o[:], in_=ps[:], func=AF.Identity, scale=QS), reads=[pk], writes=[ok])
            store(o, ok, qnT[h * 128:(h + 1) * 128, tsl], ('qnT', h, th))
            px, pxk = getp(); pr, prk = getp()
            for c in range(8):
                P.op('pe', lambda e, px=px, wt=wt, c=c: e.matmul(px[0:64, :], lhsT=wt[:, c, 128:192], rhs=cqb[:, c, tsl], start=(c == 0), stop=(c == 7)), reads=[wk, ('cqb', c, th)], writes=[pxk])
            for c in range(8):
                P.op('pe', lambda e, pr=pr, wr=wr, c=c: e.matmul(pr[0:64, :], lhsT=wr[:, c, :], rhs=cqb[:, c, tsl], start=(c == 0), stop=(c == 7)), reads=[wrk, ('cqb', c, th)], writes=[prk])
            t1, k1 = gett(); t2, k2 = gett(); o, ok = geto()
            P.op('dve', lambda e, t1=t1, px=px: e.tensor_tensor(out=t1[0:64, :], in0=px[0:64, :], in1=cos2[:, tsl], op=ALU.mult), reads=[pxk, 'cos2'], writes=[k1])
            P.op('dve', lambda e, t2=t2, pr=pr: e.scalar_tensor_tensor(out=t2[0:64, :], in0=pr[0:64, :], scalar=QS, in1=sin2[:, tsl], op0=ALU.mult, op1=ALU.mult), reads=[prk, 'sin2'], writes=[k2])
            P.op('dve', lambda e, t1=t1, t2=t2, o=o: e.scalar_tensor_tensor(out=o[0:64, :], in0=t1[0:64, :], scalar=QS, in1=t2[0:64, :], op0=ALU.mult, op1=ALU.add), reads=[k1, k2], writes=[ok])
            store(o, ok, qrT[h * 64:(h + 1) * 64, tsl], ('qrT', h, th), rows=64)
    for h in range(32):
        wt, wk = getw()
        P.dma('pool', lambda e, wt=wt, h=h: e.dma_start(out=wt[:, 0:4, :], in_=wkvb[:, h * 256:(h + 1) * 256].rearrange("(c p) n -> p c n", p=128)), writes=[wk])
        for th in range(T // 512):
            tsl = slice(th * 512, (th + 1) * 512)
            ps, pk = getp()
            for c in range(4):
                P.op('pe', lambda e, ps=ps, wt=wt, c=c: e.matmul(ps[:], lhsT=wt[:, c, 0:128], rhs=ckb[:, c, tsl], start=(c == 0), stop=(c == 3)), reads=[wk, ('ckb', c, th)], writes=[pk])
            o, ok = geto()
            P.op('act', lambda e, o=o, ps=ps: e.activation(out=o[:], in_=ps[:], func=AF.Identity), reads=[pk], writes=[ok])
            store(o, ok, knT[h * 128:(h + 1) * 128, tsl], ('knT', h, th))
            ps, pk = getp()
            for tb in range(4):
                for c in range(4):
                    P.op('pe', lambda e, ps=ps, wt=wt, c=c, tb=tb, th=th: e.matmul(ps[:, tb * 128:(tb + 1) * 128], lhsT=ckb[:, c, th * 512 + tb * 128: th * 512 + (tb + 1) * 128], rhs=wt[:, c, 128:256], start=(c == 0), stop=(c == 3)),
                         reads=[wk, ('ckb', c, th)], writes=[pk])
            o, ok = geto()
            P.op('dve', lambda e, o=o, ps=ps: e.tensor_copy(out=o[:], in_=ps[:]), reads=[pk], writes=[ok])
            key = ('v', h, th)
            P.dma('sp', lambda e, o=o, h=h, th=th: e.dma_start(out=vo[th * 512:(th + 1) * 512, h * 128:(h + 1) * 128].rearrange("(b p) d -> p b d", p=128), in_=o[:].rearrange("p (b d) -> p b d", d=128)),
                  reads=[ok], writes=[key]); outkeys.append(key)
    P.wait_all('sp', outkeys)
    P.emit()
    return nc, P

import ml_dtypes
BF = ml_dtypes.bfloat16
DILS = (1, 4, 16)

def t5_thresholds():
    n = np.arange(0, 20000)
    nf = np.maximum(n, 1).astype(np.float32)
    large = 16 + (np.log(nf / np.float32(16)) / np.float32(math.log(2048 / 16)) * np.float32(16)).astype(np.int32)
    large = np.minimum(large, 31)
    bucket = np.where(n < 16, n, large)
    return [int(np.argmax(bucket >= b)) for b in range(1, 32)]

def dil_deltas(d):
    return list(range(-3, d + 1))

def attn_consts(mode):
    j = np.arange(128)[:, None]; t = np.arange(512)[None, :]
    c = {"ones": np.ones((128, 128), np.float32).astype(BF)}
    if mode == 'mla':
        c["masks"] = np.stack([((128 * r + j) <= t) for r in range(4)]).astype(np.float32).astype(BF)
    else:
        ms = []
        for d in DILS:
            for dl in dil_deltas(d):
                rel = dl * 128 + t - j
                ms.append(((rel % d) == 0) & (rel >= 0) & (rel <= 128 * d))
        c["masks"] = np.stack(ms).astype(np.float32).astype(BF)
        c["relbase"] = (t - j).astype(np.float32) + np.zeros((128, 1), np.float32)
    return c

def emit_casts(nc, P, casts, outkeys):
    for (name, rows, cols) in casts:
        src = nc.dram_tensor(name, [rows, cols], F32, kind="ExternalInput").ap()
        dst = nc.dram_tensor(name + "_bf", [rows, cols], BF16, kind="ExternalOutput").ap()
        step = 4096
        for r0 in range(0, rows, step):
            r1 = min(rows, r0 + step)
            key = (name, r0)
            P.dma('pool', lambda e: e.dma_start(out=dst[r0:r1, :], in_=src[r0:r1, :]), writes=[key], slow=True)
            outkeys.append(key)


def build_attn(nc, NH, S, mode, casts=()):
    NB = S // 128; NQC = S // 512
    D = nc.dram_tensor
    A = nc.alloc_sbuf_tensor
    NG = 3 if mode == 'dil' else 1
    qT = D("qT", [NG, NH, 128, S], BF16, kind="ExternalInput").ap()
    kT = D("kT", [NG, NH, 128, S], BF16, kind="ExternalInput").ap()
    v = D("v", [NG, NH, S, 128], BF16, kind="ExternalInput").ap()
    ones_d = D("ones", [128, 128], BF16, kind="ExternalInput").ap()
    NM = 4 if mode == 'mla' else 33
    masks_d = D("masks", [NM, 128, 512], BF16, kind="ExternalInput").ap()
    oT = D("oT", [NH, 128, S], BF16, kind="ExternalOutput").ap()
    ones = A("ones_s", [128, 128], BF16)
    P = Prog(nc)
    P.dma('sp', lambda e: e.dma_start(out=ones[:], in_=ones_d), writes=['ones'])
    if mode == 'mla':
        qrT = D("qrT", [NH, 64, S], BF16, kind="ExternalInput").ap()
        krT = D("krT", [64, S], BF16, kind="ExternalInput").ap()
        masks = A("masks_s", [128, 4, 512], BF16)
        P.dma('sp', lambda e: e.dma_start(out=masks[:], in_=masks_d.rearrange("r p t -> p r t")), writes=['masks'])
        kr_s = A("kr_s", [64, S], BF16)
        P.dma('sp', lambda e: e.dma_start(out=kr_s[:], in_=krT), writes=['kr'])
        NBUF = 2
        q_s = [A("q_s%d" % i, [128, S], BF16) for i in range(2)]
        qr_s = [A("qr_s%d" % i, [64, S], BF16) for i in range(2)]
    else:
        tabrep = D("tabrep", [128, 32 * 48], F32, kind="ExternalInput").ap()
        relbase_d = D("relbase", [128, 512], F32, kind="ExternalInput").ap()
        hsel = D("hsel", [1, 2], I32, kind="ExternalInput").ap()
        NBUF = 1
        tab = A("tab", [128, 32 * 48], F32); dtab = A("dtab", [128, 31 * 48], F32)
        relbase = A("relbase_s", [128, 512], F32)
        nrel = A("nrel", [128, 512], F32); acc = A("acc", [128, 512], F32); stp = A("stp", [128, 512], F32)
        mk = [A("mk%d" % i, [128, 512], BF16) for i in range(2)]
        BM = A("BM", [128, 33, 512], BF16)
        qc_s = [A("qc_s%d" % i, [128, 3, 512], BF16) for i in range(2)]
        P.dma('sp', lambda e: e.dma_start(out=tab[:], in_=tabrep), writes=['tab'])
        P.dma('sp', lambda e: e.dma_start(out=relbase[:], in_=relbase_d), writes=['relbase'])
        P.op('dve', lambda e: e.tensor_tensor(out=dtab[:], in0=tab[:, 48:32 * 48], in1=tab[:, 0:31 * 48], op=ALU.subtract), reads=['tab'], writes=['dtab'])
        dhalf = A("dhalf", [128, 31 * 48], F32); tmid = A("tmid", [128, 48], F32); nthr = A("nthr", [128, 31], F32)
        stp2 = [stp, A("stpb", [128, 512], F32)]
        P.op('dve', lambda e: e.tensor_scalar(out=dhalf[:], in0=dtab[:], scalar1=0.5, scalar2=None, op0=ALU.mult), reads=['dtab'], writes=['dhalf'])
        P.op('dve', lambda e: e.tensor_tensor(out=tmid[:], in0=tab[:, 0:48], in1=tab[:, 31 * 48:32 * 48], op=ALU.add), reads=['tab'], writes=['tmid'])
        P.op('dve', lambda e: e.tensor_scalar(out=tmid[:], in0=tmid[:], scalar1=0.5, scalar2=None, op0=ALU.mult), reads=['tmid'], writes=['tmid'])
        thr_ = t5_thresholds()
        for b in range(31):
            P.op('dve', lambda e: e.memset(nthr[:, b:b + 1], float(0.5 - thr_[b])), writes=['nthr'])
    k_s = [A("k_s%d" % i, [128, NG, S], BF16) for i in range(NBUF)]
    v_s = [A("v_s%d" % i, [128, NG, NB, 128], BF16) for i in range(NBUF)]
    NE = 3
    At = [A("At%d" % i, [128, 512], BF16) for i in range(NE)]
    Am = [A("Am%d" % i, [128, 512], BF16) for i in range(2)]
    osb = [A("osb%d" % i, [128, 512], BF16) for i in range(2)]
    rl = [A("rl%d" % i, [128, 512], F32) for i in range(2)]
    E = [nc.alloc_psum_tensor("E%d" % i, [128, 512], F32) for i in range(NE)]
    O = [nc.alloc_psum_tensor("O%d" % i, [128, 512], F32) for i in range(2)]
    L = [nc.alloc_psum_tensor("L%d" % i, [128, 512], F32) for i in range(2)]
    outkeys = []
    emit_casts(nc, P, casts, outkeys)
    tiles = []
    for h in range(NH):
        for qc in range(NQC):
            lst = []
            if mode == 'mla':
                for b in range(4 * qc + 3, -1, -1):
                    lst.append(dict(g=0, b=b, r=b - 4 * qc, mi=b - 4 * qc))
            else:
                mi0 = 0
                for g, d in enumerate(DILS):
                    for k, dl in enumerate(dil_deltas(d)):
                        b = 4 * qc - dl
                        if b >= 0:
                            lst.append(dict(g=g, b=b, r=0, mi=mi0 + k))
                    mi0 += len(dil_deltas(d))
            for n, t in enumerate(lst):
                t.update(h=h, qc=qc, first=(n == 0), last=(n == len(lst) - 1), ci=h * NQC + qc)
                tiles.append(t)
    thr = t5_thresholds()
    def load_head(h):
        i = h % NBUF
        for g in range(NG):
            P.dma('sp', lambda e: e.dma_start(out=k_s[i][:, g, :], in_=kT[g, h]), writes=[('k', i)])
            P.dma('sp', lambda e: e.dma_start(out=v_s[i][:, g, :, :], in_=v[g, h].rearrange("(b p) d -> p b d", p=128)), writes=[('v', i)])
        if mode == 'mla':
            P.dma('sp', lambda e: e.dma_start(out=q_s[i][:], in_=qT[0, h]), writes=[('q', i)])
            P.dma('sp', lambda e: e.dma_start(out=qr_s[i][:], in_=qrT[h]), writes=[('qr', i)])
        else:
            mi = 0
            for g, d in enumerate(DILS):
                col = g * 16
                for dl in dil_deltas(d):
                    m = mk[mi % 2]
                    P.dma('sp', lambda e: e.dma_start(out=m[:], in_=masks_d[mi]), writes=[('mk', mi % 2)])
                    P.op('dve', lambda e: e.tensor_scalar(out=nrel[:], in0=relbase[:], scalar1=float(dl * 128), scalar2=0.0, op0=ALU.add, op1=ALU.max), reads=['relbase'], writes=['nrel'])
                    for b in range(31):
                        dcol = b * 48 + col + h
                        sb_ = stp2[b % 2]
                        P.op('act', lambda e: e.activation(out=sb_[:], in_=nrel[:], func=AF.Sign, bias=nthr[:, b:b + 1], scale=1.0), reads=['nrel', 'nthr'], writes=[('stp', b % 2)])
                        if b == 0:
                            P.op('dve', lambda e: e.tensor_scalar(out=acc[:], in0=sb_[:], scalar1=dhalf[:, dcol:dcol + 1], scalar2=None, op0=ALU.mult), reads=[('stp', b % 2), 'dhalf'], writes=['acc'])
                        else:
                            P.op('dve', lambda e: e.scalar_tensor_tensor(out=acc[:], in0=sb_[:], scalar=dhalf[:, dcol:dcol + 1], in1=acc[:], op0=ALU.mult, op1=ALU.add), reads=[('stp', b % 2), 'dhalf', 'acc'], writes=['acc'])
                    t0c = col + h
                    P.op('act', lambda e: e.activation(out=acc[:], in_=acc[:], func=AF.Exp, bias=tmid[:, t0c:t0c + 1], scale=1.0), reads=['acc', 'tmid'], writes=['acc'])
                    P.op('dve', lambda e: e.tensor_tensor(out=BM[:, mi, :], in0=acc[:], in1=m[:], op=ALU.mult), reads=['acc', ('mk', mi % 2)], writes=[('BM', mi)])
                    mi += 1
    loaded = set()
    def ensure(h):
        if h < NH and h not in loaded:
            loaded.add(h); load_head(h)
    ensure(0)
    NT = len(tiles)
    def stageA(i):
        t = tiles[i]; hi = t['h'] % NBUF; ei = i % NE; g = t['g']; b = t['b']; qc = t['qc']
        if mode == 'mla':
            P.op('pe', lambda e: e.matmul(E[ei][:], lhsT=k_s[hi][:, 0, b * 128:(b + 1) * 128], rhs=q_s[hi][:, qc * 512:(qc + 1) * 512], start=True, stop=False),
                 reads=[('q', hi), ('k', hi)], writes=[('E', ei)])
            P.op('pe', lambda e: e.matmul(E[ei][:], lhsT=kr_s[:, b * 128:(b + 1) * 128], rhs=qr_s[hi][:, qc * 512:(qc + 1) * 512], start=False, stop=True),
                 reads=[('qr', hi), 'kr'], writes=[('E', ei)])
            if t['r'] >= 0:
                P.op('act', lambda e: e.activation(out=Am[i % 2][:], in_=E[ei][:], func=AF.Exp), reads=[('E', ei)], writes=[('Am', i % 2)])
                P.op('dve', lambda e: e.tensor_tensor(out=At[ei][:], in0=Am[i % 2][:], in1=masks[:, t['r'], :], op=ALU.mult), reads=[('Am', i % 2), 'masks'], writes=[('At', ei)])
            else:
                P.op('act', lambda e: e.activation(out=At[ei][:], in_=E[ei][:], func=AF.Exp), reads=[('E', ei)], writes=[('At', ei)])
        else:
            ci = t['ci']
            if t['first']:
                P.dma('sp', lambda e: e.dma_start(out=qc_s[ci % 2][:], in_=qT[:, t['h'], :, qc * 512:(qc + 1) * 512].rearrange("g p t -> p g t")), writes=[('qc', ci % 2)])
            P.op('pe', lambda e: e.matmul(E[ei][:], lhsT=k_s[hi][:, g, b * 128:(b + 1) * 128], rhs=qc_s[ci % 2][:, g, :], start=True, stop=True),
                 reads=[('qc', ci % 2), ('k', hi)], writes=[('E', ei)])
            P.op('act', lambda e: e.activation(out=Am[i % 2][:], in_=E[ei][:], func=AF.Exp), reads=[('E', ei)], writes=[('Am', i % 2)])
            P.op('dve', lambda e: e.tensor_tensor(out=At[ei][:], in0=Am[i % 2][:], in1=BM[:, t['mi'], :], op=ALU.mult), reads=[('Am', i % 2), ('BM', t['mi'])], writes=[('At', ei)])
    def stageC(i):
        t = tiles[i]; hi = t['h'] % NBUF; ei = i % NE; oi = t['ci'] % 2; g = t['g']; b = t['b']; qc = t['qc']; h = t['h']
        P.op('pe', lambda e: e.matmul(O[oi][:], lhsT=v_s[hi][:, g, b, :], rhs=At[ei][:], start=t['first'], stop=t['last']),
             reads=[('At', ei), ('v', hi)], writes=[('O', oi)])
        P.op('pe', lambda e: e.matmul(L[oi][:], lhsT=ones[:], rhs=At[ei][:], start=t['first'], stop=t['last']),
             reads=[('At', ei), 'ones'], writes=[('L', oi)])
        if t['last']:
            P.op('dve', lambda e: e.reciprocal(out=rl[oi][:], in_=L[oi][:]), reads=[('L', oi)], writes=[('rl', oi)])
            P.op('dve', lambda e: e.tensor_tensor(out=osb[oi][:], in0=O[oi][:], in1=rl[oi][:], op=ALU.mult), reads=[('O', oi), ('rl', oi)], writes=[('osb', oi)])
            key = ('oT', t['ci'])
            P.dma('sp', lambda e: e.dma_start(out=oT[h, :, qc * 512:(qc + 1) * 512], in_=osb[oi][:]), reads=[('osb', oi)], writes=[key])
            outkeys.append(key)
    pend = None
    for i in range(NT + 1):
        drained = False
        if i < NT:
            t = tiles[i]
            if t['first'] and t['qc'] == 0:
                if NBUF == 1:
                    if i > 0:
                        stageC(i - 1); drained = True
                    ensure(t['h'])
                else:
                    pend = (i + 3, t['h'] + 1)
            stageA(i)
        if pend is not None and i >= pend[0]:
            ensure(pend[1]); pend = None
        if 0 <= i - 1 < NT and not drained: stageC(i - 1)
    P.wait_all('sp', outkeys)
    P.emit()
    return nc, P


from concourse.bass_utils import run_bass_kernel_spmd

NCORES = 8
S_FULL = 8192
TOK = S_FULL // NCORES


def _run(nc, in_maps):
    res = run_bass_kernel_spmd(nc, in_maps, core_ids=list(range(NCORES)))
    return res.results


def _c(a):
    return np.ascontiguousarray(a)


def _cast_specs(Fa):
    return [('cw_o', Fa // 4, 2048), ('cw_g', 16384, 256), ('cw_u', 16384, 256), ('cw_d', 2048, 2048)]


def _cast_inputs(c, wo, wg, wu, wd):
    Fa = wo.shape[0]
    wo2 = wo.reshape(Fa * 2, 2048)
    n = Fa * 2 // NCORES
    return dict(cw_o=_c(wo2[c * n:(c + 1) * n]), cw_g=_c(wg[4 * c:4 * c + 4].reshape(16384, 256)),
                cw_u=_c(wu[4 * c:4 * c + 4].reshape(16384, 256)), cw_d=_c(wd[4 * c:4 * c + 4].reshape(2048, 2048)))


def _cast_outputs(r, Fa):
    cat = lambda k: np.concatenate([r[c][k] for c in range(NCORES)], axis=0)
    return (cat('cw_o_bf').reshape(Fa, 4096), cat('cw_g_bf').reshape(32, 4096, 256),
            cat('cw_u_bf').reshape(32, 4096, 256), cat('cw_d_bf').reshape(32, 256, 4096))


def kernel(x, positions, rel_bias, sb_w_qkv, sb_w_o, mla_w_q_a, mla_q_a_norm, mla_w_q_b, mla_w_kv_a, mla_kv_a_norm,
           mla_w_kv_b, mla_w_o, dil_w_qkv, dil_w_o, ln_gain, ln_bias, moe_w_group_router, moe_b_group_router,
           moe_w_expert_router, moe_b_expert_router, moe_w_gate, moe_w_up, moe_w_down):
    f32 = lambda a: np.asarray(a, dtype=np.float32)
    x = f32(x); S = S_FULL
    positions = np.asarray(positions).astype(np.int32)
    hT = [_c(x[0, c * TOK:(c + 1) * TOK].T) for c in range(NCORES)]
    for li in range(4):
        kind, j = li % 3, li // 3
        wgf, wuf, wdf = f32(moe_w_gate[li]), f32(moe_w_up[li]), f32(moe_w_down[li])
        if kind == 0 or kind == 2:
            if kind == 0:
                w = f32(sb_w_qkv[j]); nq = 4096; sc = 128 ** -0.5; wo = f32(sb_w_o[j])
            else:
                w = f32(dil_w_qkv[j]); nq = 6144; sc = 128 ** -0.5; wo = f32(dil_w_o[j])
            specs = [('fm', 'w', 3 * nq, 0, nq, sc, 'qT'), ('fm', 'w', 3 * nq, nq, nq, 1.0, 'kT'), ('tm', 'w', 3 * nq, 2 * nq, nq, 1.0, 'v')]
            nc = bass.Bass("TRN2", target_bir_lowering=False)
            nc, _ = build_proj(nc, 4096, specs, T=TOK)
            r = _run(nc, [dict(xT=hT[c], w=w) for c in range(NCORES)])
            qT = np.concatenate([r[c]["qT"] for c in range(NCORES)], axis=1)
            kT = np.concatenate([r[c]["kT"] for c in range(NCORES)], axis=1)
            v = np.concatenate([r[c]["v"] for c in range(NCORES)], axis=0)
            del r
            if kind == 0:
                nc = bass.Bass("TRN2", target_bir_lowering=False)
                nc, _ = build_sb_attn(nc, 4, S, casts=_cast_specs(4096))
                cst = sb_consts()
                ims = []
                for c in range(NCORES):
                    d = dict(qT=_c(qT[c * 512:(c + 1) * 512].reshape(4, 128, S)), kT=_c(kT[c * 512:(c + 1) * 512].reshape(4, 128, S)),
                             v=_c(v[:, c * 512:(c + 1) * 512].reshape(S, 4, 128).transpose(1, 0, 2)))
                    d.update(cst); d.update(_cast_inputs(c, wo, wgf, wuf, wdf)); ims.append(d)
                r = _run(nc, ims)
                aT = np.concatenate([r[c]["oT"].reshape(512, S) for c in range(NCORES)], axis=0)
                Fa = 4096
                wbf = _cast_outputs(r, Fa)
            else:
                nc = bass.Bass("TRN2", target_bir_lowering=False)
                nc, _ = build_attn(nc, 2, S, 'dil', casts=_cast_specs(2048))
                cst = attn_consts('dil')
                rb = f32(rel_bias)
                ims = []
                for c in range(NCORES):
                    rows = [g * 2048 + (2 * c + hl) * 128 for g in range(3) for hl in range(2)]
                    qs = np.stack([qT[r0:r0 + 128] for r0 in rows]).reshape(3, 2, 128, S)
                    ks = np.stack([kT[r0:r0 + 128] for r0 in rows]).reshape(3, 2, 128, S)
                    vs = np.stack([v[:, r0:r0 + 128] for r0 in rows]).reshape(3, 2, S, 128)
                    tabc = np.zeros((32, 48), np.float32)
                    for g in range(3):
                        for hl in range(2):
                            tabc[:, g * 16 + hl] = rb[:, g * 16 + 2 * c + hl]
                    d = dict(qT=_c(qs), kT=_c(ks), v=_c(vs), tabrep=_c(np.tile(tabc.reshape(1, -1), (128, 1))), hsel=np.zeros((1, 2), np.int32))
                    d.update(cst); d.update(_cast_inputs(c, wo, wgf, wuf, wdf)); ims.append(d)
                r = _run(nc, ims)
                aT = np.concatenate([r[c]["oT"].reshape(256, S) for c in range(NCORES)], axis=0)
                Fa = 2048
                wbf = _cast_outputs(r, Fa)
            del qT, kT, v
        else:
            nc = bass.Bass("TRN2", target_bir_lowering=False)
            nc, _ = build_mla_pre(nc, T=TOK)
            cst = mla_consts()
            ims = []
            for c in range(NCORES):
                d = dict(hT=hT[c], wqa=f32(mla_w_q_a[j]), wkva=f32(mla_w_kv_a[j]), qg=_c(f32(mla_q_a_norm[j]).reshape(8, 128).T),
                         kg=_c(f32(mla_kv_a_norm[j]).reshape(4, 128).T), wqb=f32(mla_w_q_b[j]), wkvb=f32(mla_w_kv_b[j]),
                         posr=_c(np.tile(positions[0, c * TOK:(c + 1) * TOK][None, :], (64, 1))))
                d.update(cst); ims.append(d)
            r = _run(nc, ims)
            cat = lambda k, ax: np.concatenate([r[c][k] for c in range(NCORES)], axis=ax)
            qn = cat("qnT", 1); qr = cat("qrT", 1); kn = cat("knT", 1); kr = cat("krT", 1); v = cat("v", 0)
            del r
            nc = bass.Bass("TRN2", target_bir_lowering=False)
            wo = f32(mla_w_o[j])
            nc, _ = build_attn(nc, 4, S, 'mla', casts=_cast_specs(4096))
            cst = attn_consts('mla')
            ims = []
            for c in range(NCORES):
                d = dict(qT=_c(qn[c * 512:(c + 1) * 512].reshape(1, 4, 128, S)), qrT=_c(qr[c * 256:(c + 1) * 256].reshape(4, 64, S)),
                         kT=_c(kn[c * 512:(c + 1) * 512].reshape(1, 4, 128, S)), krT=_c(kr),
                         v=_c(v[:, c * 512:(c + 1) * 512].reshape(S, 4, 128).transpose(1, 0, 2)[None]))
                d.update(cst); d.update(_cast_inputs(c, wo, wgf, wuf, wdf)); ims.append(d)
            r = _run(nc, ims)
            aT = np.concatenate([r[c]["oT"].reshape(512, S) for c in range(NCORES)], axis=0)
            Fa = 4096
            wbf = _cast_outputs(r, Fa)
            del qn, qr, kn, kr, v
        nc = bass.Bass("TRN2", target_bir_lowering=False)
        nc, _ = build_post(nc, Fa, T=TOK)
        cst = post_consts()
        brep = np.tile(np.concatenate([f32(moe_b_group_router[li]), f32(moe_b_expert_router[li]).reshape(-1)])[None, :], (128, 1)).astype(np.float32)
        base = dict(wo=_c(wbf[0]), lng=ln_layout(f32(ln_gain[li])), lnb=ln_layout(f32(ln_bias[li])),
                    wr=wr_layout(f32(moe_w_group_router[li]), f32(moe_w_expert_router[li])), brep=_c(brep),
                    wg=_c(wbf[1]), wu=_c(wbf[2]), wd=_c(wbf[3]))
        base.update(cst)
        ims = []
        for c in range(NCORES):
            d = dict(aT=_c(aT[:, c * TOK:(c + 1) * TOK]), hT=hT[c]); d.update(base); ims.append(d)
        r = _run(nc, ims)
        hT = [r[c]["outT"] for c in range(NCORES)]
        del r, aT
    out = np.concatenate([hT[c].T for c in range(NCORES)], axis=0)[None]
    return np.ascontiguousarray(out.astype(np.float32))
```
